# Optimizing a Trainium2 kernel written in Bass

```python
import jax
import jax.numpy as jnp
from jax import lax
import numpy as np

D_MODEL = 4096
BATCH = 2
SEQ = 4096
DEPTH = 2

N_MIXERS = 2
N_MLSTM = (DEPTH + 1) // 2
N_RWKV = DEPTH // 2

ML_HEADS = 8
ML_DV = D_MODEL // ML_HEADS
ML_DQK = ML_DV // 2
ML_CHUNK = 128
ML_GATE_CAP = 15.0
ML_QK = ML_HEADS * ML_DQK
ML_V = ML_HEADS * ML_DV
ML_IN = 2 * ML_QK + 2 * ML_V + 2 * ML_HEADS
ML_NORM_EPS = 1e-6

RW_HEAD = 64
RW_HEADS = D_MODEL // RW_HEAD
RW_DECAY_LORA = max(32, int(round(1.8 * D_MODEL ** 0.5 / 32)) * 32)
RW_AAA_LORA = max(32, int(round(1.8 * D_MODEL ** 0.5 / 32)) * 32)
RW_GATE_LORA = max(32, int(round(0.6 * D_MODEL ** 0.8 / 32)) * 32)
RW_GN_EPS = 64e-5

FFN_HIDDEN = -(-8 * D_MODEL // (3 * 256)) * 256

ALPHA = (2 * DEPTH) ** 0.25
BETA = (8 * DEPTH) ** -0.25
LN_EPS = 1e-5

kernel_name = 'hybrid_mlstm_rwkv7_adaln_deepnorm'


def layer_norm(x, g, b):
    xf = x.astype(jnp.float32)
    mu = jnp.mean(xf, axis=-1, keepdims=True)
    var = jnp.mean(jnp.square(xf - mu), axis=-1, keepdims=True)
    return ((xf - mu) * lax.rsqrt(var + LN_EPS) * g + b).astype(x.dtype)


def swiglu_ffn(u, w_in, w_out):
    gate, up = jnp.split(u @ w_in, [FFN_HIDDEN], axis=-1)
    return (jax.nn.silu(gate) * up) @ w_out


def _to_chunks(t):
    b, h, s = t.shape[:3]
    t = t.reshape((b, h, s // ML_CHUNK, ML_CHUNK) + t.shape[3:])
    return jnp.moveaxis(t, 2, 0)


def mlstm_chunkwise(q, k, v, i_pre, log_f):
    b, h, s, _ = q.shape
    causal = jnp.tril(jnp.ones((ML_CHUNK, ML_CHUNK), dtype=bool))
    g = jnp.cumsum(_to_chunks(log_f), axis=-1)
    xs = (_to_chunks(q), _to_chunks(k), _to_chunks(v), _to_chunks(i_pre), g)

    def step(carry, inp):
        c_st, n_st, m_st = carry
        qc, kc, vc, ic, gc = inp
        log_d = jnp.where(causal, gc[..., :, None] - gc[..., None, :] + ic[..., None, :], -jnp.inf)
        log_inter = gc + m_st[..., None]
        m_row = jnp.maximum(jnp.max(log_d, axis=-1), log_inter)
        scores = jnp.einsum('bhld,bhmd->bhlm', qc, kc) * jnp.exp(log_d - m_row[..., None])
        inter = jnp.exp(log_inter - m_row)
        num = jnp.einsum('bhlm,bhmv->bhlv', scores, vc) + inter[..., None] * jnp.einsum('bhld,bhdv->bhlv', qc, c_st)
        den = jnp.sum(scores, axis=-1) + inter * jnp.einsum('bhld,bhd->bhl', qc, n_st)
        h_out = num / jnp.maximum(jnp.abs(den), jnp.exp(-m_row))[..., None]
        g_last = gc[..., -1]
        log_w = g_last[..., None] - gc + ic
        m_new = jnp.maximum(g_last + m_st, jnp.max(log_w, axis=-1))
        wk = kc * jnp.exp(log_w - m_new[..., None])[..., None]
        carry_decay = jnp.exp(g_last + m_st - m_new)
        c_new = carry_decay[..., None, None] * c_st + jnp.einsum('bhld,bhlv->bhdv', wk, vc)
        n_new = carry_decay[..., None] * n_st + jnp.sum(wk, axis=2)
        return (c_new, n_new, m_new), h_out

    init = (jnp.zeros((b, h, ML_DQK, ML_DV), jnp.float32),
            jnp.zeros((b, h, ML_DQK), jnp.float32),
            jnp.zeros((b, h), jnp.float32))
    _, hs = lax.scan(step, init, xs)
    return jnp.moveaxis(hs, 0, 2).reshape(b, h, s, ML_DV)


def mlstm_mixer(u, w_in, b_i, b_f, norm_g, w_out):
    b, s, _ = u.shape
    proj = u @ w_in

    def heads(t, d):
        return t.reshape(b, s, ML_HEADS, d).transpose(0, 2, 1, 3).astype(jnp.float32)

    q = heads(proj[..., :ML_QK], ML_DQK)
    k = heads(proj[..., ML_QK:2 * ML_QK], ML_DQK) * ML_DQK ** -0.5
    v = heads(proj[..., 2 * ML_QK:2 * ML_QK + ML_V], ML_DV)
    o = proj[..., 2 * ML_QK + ML_V:2 * ML_QK + 2 * ML_V]
    gates = proj[..., 2 * ML_QK + 2 * ML_V:].astype(jnp.float32)
    i_pre = ML_GATE_CAP * jnp.tanh((gates[..., :ML_HEADS] + b_i) / ML_GATE_CAP)
    log_f = jax.nn.log_sigmoid(gates[..., ML_HEADS:] + b_f)
    h = mlstm_chunkwise(q, k, v, i_pre.transpose(0, 2, 1), log_f.transpose(0, 2, 1))
    h = h * lax.rsqrt(jnp.mean(jnp.square(h), axis=-1, keepdims=True) + ML_NORM_EPS)
    h = h.transpose(0, 2, 1, 3).reshape(b, s, ML_V) * norm_g * jax.nn.sigmoid(o.astype(jnp.float32))
    return h.astype(u.dtype) @ w_out


def rwkv7_scan(r, decay, k, v, kk, a):
    b, s, h, n = r.shape

    def step(state, inp):
        r_t, w_t, k_t, v_t, kk_t, a_t = inp
        sa = jnp.einsum('bhvk,bhk->bhv', state, -kk_t)
        state = (state * w_t[:, :, None, :] + sa[..., None] * (kk_t * a_t)[:, :, None, :]
                 + v_t[..., None] * k_t[:, :, None, :])
        return state, jnp.einsum('bhvk,bhk->bhv', state, r_t)

    xs = tuple(jnp.moveaxis(t, 1, 0) for t in (r, decay, k, v, kk, a))
    _, ys = lax.scan(step, jnp.zeros((b, h, n, n), jnp.float32), xs)
    return jnp.moveaxis(ys, 0, 1)


def rwkv7_mixer(u, mu, w_r, w_k, w_v, w0, w1, w2, a0, a1, a2, g1, g2, k_k, k_a, r_k, lnx_g, lnx_b, w_o):
    b, s, d = u.shape
    xx = jnp.pad(u, ((0, 0), (1, 0), (0, 0)))[:, :s] - u
    xr, xw, xk, xv, xa, xg = [u + xx * mu[j] for j in range(6)]
    r = xr @ w_r
    k = xk @ w_k
    v = xv @ w_v
    w = -jax.nn.softplus(-(w0 + jnp.tanh(xw @ w1) @ w2)) - 0.5
    a = jax.nn.sigmoid(a0 + (xa @ a1) @ a2)
    g = jax.nn.sigmoid(xg @ g1) @ g2

    def heads(t):
        return t.astype(jnp.float32).reshape(b, s, RW_HEADS, RW_HEAD)

    kk = heads(k * k_k)
    kk = kk / jnp.maximum(jnp.linalg.norm(kk, axis=-1, keepdims=True), 1e-12)
    k = heads(k * (1 + (a - 1) * k_a))
    r, v, a = heads(r), heads(v), heads(a)
    decay = jnp.exp(-jnp.exp(heads(w)))
    y = rwkv7_scan(r, decay, k, v, kk, a)
    mean = jnp.mean(y, axis=-1, keepdims=True)
    var = jnp.mean(jnp.square(y - mean), axis=-1, keepdims=True)
    y = ((y - mean) * lax.rsqrt(var + RW_GN_EPS)).reshape(b, s, d) * lnx_g + lnx_b
    bonus = (jnp.sum(r * k * r_k, axis=-1, keepdims=True) * v).reshape(b, s, d)
    return ((y + bonus) * g.astype(jnp.float32)).astype(u.dtype) @ w_o


def setup_inputs(seed: int = 0) -> dict:
    key = jax.random.key(seed)
    ks = iter(jax.random.split(key, 40))
    D = D_MODEL

    def nrm(shape, scale):
        return jax.random.normal(next(ks), shape, jnp.float32) * scale

    x = nrm((BATCH, SEQ, D), 1.0)
    c = nrm((BATCH, D), 1.0)
    ada_w = nrm((DEPTH, D, 6 * D), D ** -0.5)
    ada_b = nrm((DEPTH, 6 * D), 0.02)
    mix_ln_g = 1.0 + nrm((DEPTH, D), 0.02)
    mix_ln_b = nrm((DEPTH, D), 0.02)
    ffn_ln_g = 1.0 + nrm((DEPTH, D), 0.02)
    ffn_ln_b = nrm((DEPTH, D), 0.02)
    ffn_w_in = nrm((DEPTH, D, 2 * FFN_HIDDEN), D ** -0.5)
    ffn_w_out = nrm((DEPTH, FFN_HIDDEN, D), FFN_HIDDEN ** -0.5 * BETA)

    ml_w_in = jnp.concatenate([
        nrm((N_MLSTM, D, 2 * ML_QK), D ** -0.5),
        nrm((N_MLSTM, D, ML_V), D ** -0.5 * BETA),
        nrm((N_MLSTM, D, ML_V + 2 * ML_HEADS), D ** -0.5)], axis=-1)
    ml_b_i = nrm((N_MLSTM, ML_HEADS), 0.1)
    ml_b_f = jnp.linspace(3.0, 6.0, ML_HEADS)[None, :] + nrm((N_MLSTM, ML_HEADS), 0.1)
    ml_norm_g = 1.0 + nrm((N_MLSTM, ML_V), 0.02)
    ml_w_out = nrm((N_MLSTM, ML_V, D), ML_V ** -0.5 * BETA)

    rw_mu = jax.random.uniform(next(ks), (N_RWKV, 6, D), jnp.float32)
    rw_w_r = nrm((N_RWKV, D, D), D ** -0.5)
    rw_w_k = nrm((N_RWKV, D, D), D ** -0.5)
    rw_w_v = nrm((N_RWKV, D, D), D ** -0.5 * BETA)
    lin = jnp.linspace(0.0, 1.0, D)
    rw_w0 = (-6.5 + 5.0 * lin ** 0.9)[None, :] + nrm((N_RWKV, D), 0.1)
    rw_w1 = nrm((N_RWKV, D, RW_DECAY_LORA), D ** -0.5)
    rw_w2 = nrm((N_RWKV, RW_DECAY_LORA, D), 0.1 * RW_DECAY_LORA ** -0.5)
    rw_a0 = nrm((N_RWKV, D), 0.1)
    rw_a1 = nrm((N_RWKV, D, RW_AAA_LORA), D ** -0.5)
    rw_a2 = nrm((N_RWKV, RW_AAA_LORA, D), 0.5 * RW_AAA_LORA ** -0.5)
    rw_g1 = nrm((N_RWKV, D, RW_GATE_LORA), D ** -0.5)
    rw_g2 = nrm((N_RWKV, RW_GATE_LORA, D), RW_GATE_LORA ** -0.5)
    rw_k_k = 0.85 + nrm((N_RWKV, D), 0.02)
    rw_k_a = 1.0 + nrm((N_RWKV, D), 0.02)
    rw_r_k = nrm((N_RWKV, RW_HEADS, RW_HEAD), 0.1)
    rw_lnx_g = 1.0 + nrm((N_RWKV, D), 0.02)
    rw_lnx_b = nrm((N_RWKV, D), 0.02)
    rw_w_o = nrm((N_RWKV, D, D), D ** -0.5 * BETA)
    return {'x': x, 'c': c, 'ada_w': ada_w, 'ada_b': ada_b,
            'mix_ln_g': mix_ln_g, 'mix_ln_b': mix_ln_b, 'ffn_ln_g': ffn_ln_g, 'ffn_ln_b': ffn_ln_b,
            'ffn_w_in': ffn_w_in, 'ffn_w_out': ffn_w_out,
            'ml_w_in': ml_w_in, 'ml_b_i': ml_b_i, 'ml_b_f': ml_b_f, 'ml_norm_g': ml_norm_g, 'ml_w_out': ml_w_out,
            'rw_mu': rw_mu, 'rw_w_r': rw_w_r, 'rw_w_k': rw_w_k, 'rw_w_v': rw_w_v,
            'rw_w0': rw_w0, 'rw_w1': rw_w1, 'rw_w2': rw_w2,
            'rw_a0': rw_a0, 'rw_a1': rw_a1, 'rw_a2': rw_a2, 'rw_g1': rw_g1, 'rw_g2': rw_g2,
            'rw_k_k': rw_k_k, 'rw_k_a': rw_k_a, 'rw_r_k': rw_r_k,
            'rw_lnx_g': rw_lnx_g, 'rw_lnx_b': rw_lnx_b, 'rw_w_o': rw_w_o}


def reference(x, c, ada_w, ada_b, mix_ln_g, mix_ln_b, ffn_ln_g, ffn_ln_b, ffn_w_in, ffn_w_out,
              ml_w_in, ml_b_i, ml_b_f, ml_norm_g, ml_w_out,
              rw_mu, rw_w_r, rw_w_k, rw_w_v, rw_w0, rw_w1, rw_w2, rw_a0, rw_a1, rw_a2, rw_g1, rw_g2,
              rw_k_k, rw_k_a, rw_r_k, rw_lnx_g, rw_lnx_b, rw_w_o):
    c_act = jax.nn.silu(c)
    for layer in range(DEPTH):
        mod = c_act @ ada_w[layer] + ada_b[layer]
        sh_m, sc_m, gt_m, sh_f, sc_f, gt_f = [t[:, None, :] for t in jnp.split(mod, 6, axis=-1)]
        u = x * (1 + sc_m) + sh_m
        j = layer // N_MIXERS
        if layer % N_MIXERS == 0:
            y = mlstm_mixer(u, ml_w_in[j], ml_b_i[j], ml_b_f[j], ml_norm_g[j], ml_w_out[j])
        else:
            y = rwkv7_mixer(u, rw_mu[j], rw_w_r[j], rw_w_k[j], rw_w_v[j], rw_w0[j], rw_w1[j], rw_w2[j],
                            rw_a0[j], rw_a1[j], rw_a2[j], rw_g1[j], rw_g2[j], rw_k_k[j], rw_k_a[j],
                            rw_r_k[j], rw_lnx_g[j], rw_lnx_b[j], rw_w_o[j])
        x = layer_norm(ALPHA * x + gt_m * y, mix_ln_g[layer], mix_ln_b[layer])
        u = x * (1 + sc_f) + sh_f
        x = layer_norm(ALPHA * x + gt_f * swiglu_ffn(u, ffn_w_in[layer], ffn_w_out[layer]),
                       ffn_ln_g[layer], ffn_ln_b[layer])
    return x
```

```python
import numpy as np
import ml_dtypes
from contextlib import ExitStack
import concourse.bass as bass
import concourse.mybir as mybir
from concourse.bass_utils import run_bass_kernel_spmd

F32 = mybir.dt.float32
BF16 = mybir.dt.bfloat16
AF = mybir.ActivationFunctionType
ALU = mybir.AluOpType
AX = mybir.AxisListType
NPBF = ml_dtypes.bfloat16

D = 4096
KC = D // 128
NCORE = 8
NTOK = 1024
TT = 512
NT = NTOK // TT
FF = 11008
ML_IN = 12304
ALPHA = 4 ** 0.25
LN_EPS = 1e-5
ENGS = ("tensor", "vector", "scalar", "gpsimd", "sync")


class Sem:
    def __init__(self, h, name):
        self.h = h
        self.name = name
        self.n = 0


class Prog:
    def __init__(self, nc):
        self.nc = nc
        self.q = {e: [] for e in ENGS}
        self.waited = {e: {} for e in ENGS}
        self.stack = ExitStack()
        self.serial = set()
        self.last = {}
        self.chain_sem = {}

    def sem(self, name):
        return Sem(self.stack.enter_context(self.nc.semaphore(name)), name)

    def sbuf(self, name, shape, dt):
        return self.stack.enter_context(self.nc.sbuf_tensor(name, shape, dt))

    def psum(self, name, shape, dt):
        return self.stack.enter_context(self.nc.psum_tensor(name, shape, dt))

    def wait(self, eng, sem, val):
        if val <= 0:
            return
        w = self.waited[eng]
        if w.get(sem.name, 0) >= val:
            return
        w[sem.name] = val
        self.q[eng].append(lambda e, s=sem.h, v=val: e.wait_ge(s, v))

    def op(self, eng, fn, waits=(), inc=None, dma=False, selfwait=True):
        for s, v in waits:
            self.wait(eng, s, v)
        if eng in self.serial and not dma:
            if eng in self.last and selfwait:
                self.wait(eng, *self.last[eng])
            if inc is None:
                if eng not in self.chain_sem:
                    self.chain_sem[eng] = self.sem("chain_" + eng)
                inc = self.chain_sem[eng]
            self.last[eng] = (inc, inc.n + 1)
        if inc is None:
            self.q[eng].append(lambda e, f=fn: f(e))
            return None
        k = 16 if dma else 1
        inc.n += k
        self.q[eng].append(lambda e, f=fn, sh=inc.h, kk=k: f(e).then_inc(sh, kk))
        return inc.n

    def tok(self, eng):
        return self.last[eng]

    def run(self):
        with self.nc.Block() as block:
            for ename in ENGS:
                lst = self.q[ename]

                def body(e, lst=lst):
                    for f in lst:
                        f(e)

                getattr(block, ename)(body)
        self.stack.close()


class Ring:
    def __init__(self, P, name, n, shape=None, dt=None, tiles=None):
        self.n = n
        self.tiles = tiles if tiles is not None else [P.sbuf(f"{name}{i}", shape, dt) for i in range(n)]
        self.filled = [P.sem(f"{name}_f{i}") for i in range(n)]
        self.freed = [P.sem(f"{name}_e{i}") for i in range(n)]
        self.i = 0

    def next(self):
        s = self.i % self.n
        self.i += 1
        return s

    def free_wait(self, s):
        return (self.freed[s], self.freed[s].n)

    def fill_wait(self, s):
        return (self.filled[s], self.filled[s].n)


class Slots:
    def __init__(self, P, name, n, shape, dt):
        self.n = n
        self.tiles = [P.sbuf(f"{name}{i}", shape, dt) for i in range(n)]
        self.rel = [[] for _ in range(n)]
        self.sem_in = [P.sem(f"{name}_i{i}") for i in range(n)]
        self.sem_out = [P.sem(f"{name}_o{i}") for i in range(n)]
        self.i = 0

    def get(self):
        s = self.i % self.n
        self.i += 1
        return s, self.tiles[s], list(self.rel[s])

    def release(self, s, toks):
        self.rel[s] = [t for t in toks if t is not None]


class Tok:
    WB_ELEMS = 8192
    NBUF = 3
    KG = 16

    def __init__(self, nc, P, hid_chunks=86, rings=True, kg=None):
        self.nc, self.P = nc, P
        if kg is not None:
            self.KG = kg
            self.WB_ELEMS = kg * 512
        self.wb = [P.sbuf(f"wb{i}", [128, self.WB_ELEMS], BF16) for i in range(self.NBUF)]
        self.act = P.sbuf("act", [128, KC, TT], BF16)
        self.hid = P.sbuf("hid", [128, hid_chunks, TT], BF16) if hid_chunks else None
        self.ps = [P.psum(f"ps{i}", [128, 512], F32) for i in range(8)]
        self.pbank = Ring(P, "pb", 8, tiles=self.ps)
        self.gi = 0
        self.mi = 0
        self.s_wl = [P.sem(f"s_wl{i}") for i in range(self.NBUF)]
        self.s_wf = P.sem("s_wf")
        self.wcount = 0
        self.wrel = {}
        if rings:
            self.ostf = Ring(P, "ostf", 2, [128, TT], F32)
            self.ostb = Ring(P, "ostb", 2, [128, TT], BF16)
            self.xs = Ring(P, "xs", 3, [128, TT], F32)
        self.act_rel = []
        self.hid_rel = []
        self.evi = 0

    def gbank(self):
        b = self.gi % 6
        self.gi += 1
        return b

    def mbank(self):
        b = 6 + self.mi % 2
        self.mi += 1
        return b

    def ev_eng(self):
        self.evi += 1
        return "vector" if self.evi % 2 else "scalar"

    def load_w(self, Wv, k0, kn, segs):
        P = self.P
        idx = self.wcount
        self.wcount += 1
        b = idx % self.NBUF
        ncols = sum(n for _, n in segs)
        assert kn * ncols <= self.WB_ELEMS
        view = self.wb[b][:, 0:kn * ncols].rearrange("p (k c) -> p k c", c=ncols)
        waits = []
        if idx - self.NBUF in self.wrel:
            waits.append(self.wrel[idx - self.NBUF])
        off = 0
        val = None
        for (c0, n) in segs:
            src = Wv[k0 * 128:(k0 + kn) * 128, c0:c0 + n].rearrange("(k p) n -> p k n", p=128)
            dst = view[:, :, off:off + n]
            val = P.op("gpsimd", lambda e, dst=dst, src=src: e.dma_start(out=dst, in_=src),
                       waits=waits, inc=self.s_wl[b], dma=True)
            waits = []
            off += n
        return idx, view, (self.s_wl[b], val)

    def gemm(self, Wv, K, blocks, ep, act=None, m_last=128, act_waits=()):
        P = self.P
        act = self.act if act is None else act
        kct = K // 128
        kgs = []
        k0 = 0
        while k0 < kct:
            kn = min(self.KG, kct - k0)
            kgs.append((k0, kn))
            k0 += kn
        loads = [(bi, gi) for bi in range(len(blocks)) for gi in range(len(kgs))]
        pending = {}

        def issue(li):
            bi, gi = loads[li]
            k0, kn = kgs[gi]
            pending[li] = self.load_w(Wv, k0, kn, blocks[bi])

        issue(0)
        if len(loads) > 1:
            issue(1)
        li = 0
        last_full = None
        for bi, segs in enumerate(blocks):
            ncols = sum(n for _, n in segs)
            nch = (ncols + 127) // 128
            banks = [self.gbank() for _ in range(nch)]
            for gi, (k0, kn) in enumerate(kgs):
                if li + 2 < len(loads):
                    issue(li + 2)
                idx, view, wwait = pending.pop(li)
                li += 1
                for oc in range(nch):
                    bank = banks[oc]
                    M = min(128, ncols - oc * 128)
                    for kk in range(kn):
                        first = gi == 0 and kk == 0
                        last = gi == len(kgs) - 1 and kk == kn - 1
                        waits = []
                        if kk == 0:
                            waits.append(wwait)
                            waits.extend(act_waits)
                            if first:
                                waits.append(self.pbank.free_wait(bank))
                        fn = lambda e, bank=bank, M=M, view=view, kk=kk, oc=oc, k0=k0, first=first, last=last: e.matmul(
                            self.ps[bank][0:M, 0:TT], view[:, kk, oc * 128:oc * 128 + M], act[:, k0 + kk, :],
                            start=first, stop=last)
                        if last:
                            P.op("tensor", fn, waits=waits, inc=self.pbank.filled[bank])
                            if oc == nch - 1:
                                self.wrel[idx] = self.pbank.fill_wait(bank)
                        elif kk == kn - 1 and oc == nch - 1:
                            v = P.op("tensor", fn, waits=waits, inc=self.s_wf)
                            self.wrel[idx] = (self.s_wf, v)
                        else:
                            P.op("tensor", fn, waits=waits)
            last_full = self.pbank.fill_wait(banks[-1])
            for oc in range(nch):
                bank = banks[oc]
                ep(bi, oc, bank, self.pbank.fill_wait(bank))
        return last_full

    def gemm_multi(self, Wv, K, blocks, ep, acts, act_waits=()):
        P = self.P
        kct = K // 128
        assert kct <= self.KG
        pending = {}

        def issue(li):
            pending[li] = self.load_w(Wv, 0, kct, blocks[li])

        issue(0)
        if len(blocks) > 1:
            issue(1)
        last_full = None
        for bi, segs in enumerate(blocks):
            if bi + 2 < len(blocks):
                issue(bi + 2)
            idx, view, wwait = pending.pop(bi)
            ncols = sum(n for _, n in segs)
            nch = (ncols + 127) // 128
            for ti, act in enumerate(acts):
                banks = [self.gbank() for _ in range(nch)]
                for oc in range(nch):
                    bank = banks[oc]
                    M = min(128, ncols - oc * 128)
                    for kk in range(kct):
                        waits = []
                        if kk == 0:
                            waits = [wwait] + list(act_waits) + [self.pbank.free_wait(bank)]
                        fn = lambda e, bank=bank, M=M, view=view, kk=kk, oc=oc, act=act: e.matmul(
                            self.ps[bank][0:M, 0:TT], view[:, kk, oc * 128:oc * 128 + M], act[:, kk, :],
                            start=(kk == 0), stop=(kk == kct - 1))
                        if kk == kct - 1:
                            P.op("tensor", fn, waits=waits, inc=self.pbank.filled[bank])
                        else:
                            P.op("tensor", fn, waits=waits)
                last_full = self.pbank.fill_wait(banks[-1])
                if ti == len(acts) - 1:
                    self.wrel[idx] = last_full
                for oc in range(nch):
                    ep(bi, oc, banks[oc], self.pbank.fill_wait(banks[oc]), ti)
        return last_full


def _dram(nc, name, shape, dt, kind):
    return nc.dram_tensor(name, list(shape), dt, kind=kind).ap()


def _finish(P, out_waits):
    for s, v in out_waits:
        P.wait("sync", s, v)


class OutTracker:
    def __init__(self):
        self.sems = {}

    def add(self, sem):
        self.sems[sem.name] = sem

    def waits(self):
        return [(s, s.n) for s in self.sems.values()]


def modulate(T, xT, t, onepsc, sh, extra_waits=(), act=None):
    P = T.P
    act = T.act if act is None else act
    for c in range(KC):
        s = T.xs.next()
        xt = T.xs.tiles[s]
        P.op("sync", lambda e, xt=xt, c=c: e.dma_start(out=xt[:], in_=xT[c * 128:(c + 1) * 128, t * TT:(t + 1) * TT]),
             waits=[T.xs.free_wait(s)], inc=T.xs.filled[s], dma=True)
        waits = [T.xs.fill_wait(s)]
        if c == 0:
            waits += list(T.act_rel) + list(extra_waits)
        P.op("scalar", lambda e, xt=xt, c=c, act=act: e.activation(out=act[:, c, :], in_=xt[:], func=AF.Identity,
                                                          bias=sh[:, c:c + 1], scale=onepsc[:, c:c + 1]),
             waits=waits, inc=T.xs.freed[s])
    T.act_ready = (T.xs.freed[s], T.xs.freed[s].n)


def build_phase_b():
    nc = bass.Bass("TRN2", target_bir_lowering=False)
    xT = _dram(nc, "xT", [D, NTOK], F32, "ExternalInput")
    W = _dram(nc, "w_in", [D, ML_IN], F32, "ExternalInput")
    modv = _dram(nc, "modv", [128, 2 * KC], F32, "ExternalInput")
    qkvT = _dram(nc, "qkvT", [8192, NTOK], BF16, "ExternalOutput")
    soT = _dram(nc, "soT", [D, NTOK], F32, "ExternalOutput")
    gT = _dram(nc, "gT", [16, NTOK], F32, "ExternalOutput")
    P = Prog(nc)
    T = Tok(nc, P, hid_chunks=0, kg=32)
    act1 = P.sbuf("act1", [128, KC, TT], BF16)
    acts = [T.act, act1]
    mod_sb = P.sbuf("mod_sb", [128, 2 * KC], F32)
    onepsc = P.sbuf("onepsc", [128, KC], F32)
    s_c = P.sem("s_const")
    v = P.op("sync", lambda e: e.dma_start(out=mod_sb[:], in_=modv), inc=s_c, dma=True)
    s_c2 = P.sem("s_const2")
    v2 = P.op("vector", lambda e: e.tensor_scalar_add(onepsc[:], mod_sb[:, KC:2 * KC], 1.0), waits=[(s_c, v)], inc=s_c2)
    sh = mod_sb
    outs = OutTracker()
    for t in range(NT):
        modulate(T, xT, t, onepsc, sh, extra_waits=[(s_c2, v2)], act=acts[t])
    blocks = [[(c0, 512)] for c0 in range(0, 12288, 512)] + [[(12288, 16)]]

    def ep(bi, oc, bank, fw, t):
        col = bi * 512 + oc * 128
        ps = T.ps[bank]
        eng = T.ev_eng()
        if bi < 16:
            ring = T.ostb
            s = ring.next()
            st = ring.tiles[s]
            if bi < 4 or bi >= 8:
                if eng == "vector":
                    fn = lambda e: e.tensor_copy(st[:], ps[:, 0:TT])
                else:
                    fn = lambda e: e.copy(st[:], ps[:, 0:TT])
            else:
                if eng == "vector":
                    fn = lambda e: e.tensor_scalar_mul(st[:], ps[:, 0:TT], 0.0625)
                else:
                    fn = lambda e: e.mul(st[:], ps[:, 0:TT], 0.0625)
            P.op(eng, fn, waits=[fw, ring.free_wait(s)], inc=T.pbank.freed[bank])
            P.op("sync", lambda e: e.dma_start(out=qkvT[col:col + 128, t * TT:(t + 1) * TT], in_=st[:]),
                 waits=[(T.pbank.freed[bank], T.pbank.freed[bank].n)], inc=ring.freed[s], dma=True)
            outs.add(ring.freed[s])
        elif bi < 24:
            ring = T.ostf
            s = ring.next()
            st = ring.tiles[s]
            P.op("scalar", lambda e: e.activation(out=st[:], in_=ps[:, 0:TT], func=AF.Sigmoid),
                 waits=[fw, ring.free_wait(s)], inc=T.pbank.freed[bank])
            c2 = col - 8192
            P.op("sync", lambda e: e.dma_start(out=soT[c2:c2 + 128, t * TT:(t + 1) * TT], in_=st[:]),
                 waits=[(T.pbank.freed[bank], T.pbank.freed[bank].n)], inc=ring.freed[s], dma=True)
            outs.add(ring.freed[s])
        else:
            ring = T.ostf
            s = ring.next()
            st = ring.tiles[s]
            P.op("vector", lambda e: e.tensor_copy(st[0:16, :], ps[0:16, 0:TT]),
                 waits=[fw, ring.free_wait(s)], inc=T.pbank.freed[bank])
            P.op("sync", lambda e: e.dma_start(out=gT[:, t * TT:(t + 1) * TT], in_=st[0:16, :]),
                 waits=[(T.pbank.freed[bank], T.pbank.freed[bank].n)], inc=ring.freed[s], dma=True)
            outs.add(ring.freed[s])

    T.gemm_multi(W, D, blocks, ep, acts, act_waits=[T.act_ready])
    _finish(P, outs.waits())
    P.run()
    return nc


ML_S = 4096
ML_NCH = ML_S // 128
ML_EPS = 1e-6


def build_phase_c(debug=False):
    nc = bass.Bass("TRN2", target_bir_lowering=False)
    if debug:
        dbg1 = _dram(nc, "dbg1", [128, 96], F32, "ExternalOutput")
        dbg2 = _dram(nc, "dbg2", [128, 512], F32, "ExternalOutput")
        dbg3 = _dram(nc, "dbg3", [128, 512], F32, "ExternalOutput")
        dbg4 = _dram(nc, "dbg4", [128, 128], BF16, "ExternalOutput")
        dbg5 = _dram(nc, "dbg5", [128, 8], F32, "ExternalOutput")
        dbg6 = _dram(nc, "dbg6", [1, ML_S], F32, "ExternalOutput")
    qT = _dram(nc, "qT", [2, 256, ML_S], BF16, "ExternalInput")
    kT = _dram(nc, "kT", [2, 256, ML_S], BF16, "ExternalInput")
    ktm = _dram(nc, "ktm", [2, ML_S, 256], BF16, "ExternalInput")
    vtm = _dram(nc, "vtm", [2, ML_S, 512], BF16, "ExternalInput")
    sotm = _dram(nc, "sotm", [2, ML_S, 512], F32, "ExternalInput")
    gi_d = _dram(nc, "gi", [2, ML_S], F32, "ExternalInput")
    gf_d = _dram(nc, "gf", [2, ML_S], F32, "ExternalInput")
    gb_d = _dram(nc, "gb", [1, 2], F32, "ExternalInput")
    ng_d = _dram(nc, "ng", [128, 512], F32, "ExternalInput")
    hT = _dram(nc, "hT", [2, 512, ML_S], BF16, "ExternalOutput")
    P = Prog(nc)
    P.serial = {"vector", "scalar", "gpsimd"}
    qs = P.sbuf("qs", [128, 2, ML_S], BF16)
    ks = P.sbuf("ks", [128, 2, ML_S], BF16)
    ktm_s = P.sbuf("ktm_s", [128, ML_NCH, 256], BF16)
    va = P.sbuf("va", [128, ML_NCH, 514], BF16)
    hts_r = [P.sbuf(f"hts{i}", [128, 4, 1024], BF16) for i in range(2)]
    ng = P.sbuf("ng_s", [128, 512], F32)
    gb = P.sbuf("gb_s", [1, 2], F32)
    mask = P.sbuf("mask", [128, 128], F32)
    ident = P.sbuf("ident", [128, 128], F32)
    ones_row = P.sbuf("ones_row", [1, 128], F32)
    one11 = P.sbuf("one11", [1, 1], F32)
    g_in = [P.sbuf(f"grow{i}", [1, ML_S], F32) for i in range(2)]
    t2 = g_in[1]
    cF = P.sbuf("g_cF", [1, ML_S], F32)
    bv = g_in[0]
    Mr = P.sbuf("g_M", [1, ML_S], F32)
    wr = bv
    fr = cF
    ones_bc = one11[0:1, 0:1].to_broadcast([1, ML_S])
    rcr = P.sbuf("g_rc", [1, ML_NCH], F32)
    wcol = P.sbuf("wcol", [128, ML_NCH], F32)
    fcol = P.sbuf("fcol", [128, ML_NCH], F32)
    rcb = P.sbuf("rcb", [128, ML_NCH], F32)
    Cs = P.sbuf("Cs", [128, 2, 514], F32)
    Cb = P.sbuf("Cb", [128, 2, 514], BF16)
    PT = P.sbuf("PT", [128, 128], BF16)
    kw = P.sbuf("kw", [128, 256], BF16)
    junk = P.sbuf("junk", [128, 512], F32)
    hn = P.sbuf("hn", [128, 512], F32)
    hg = P.sbuf("hg", [128, 512], F32)
    sm = P.sbuf("sm", [128, 8], F32)
    so_ring = Ring(P, "so", 3, [128, 512], F32)
    psA = P.psum("psA", [128, 512], F32)
    psB = P.psum("psB", [128, 512], F32)
    psC = P.psum("psC", [128, 512], F32)
    psD = [P.psum(f"psD{i}", [128, 512], F32) for i in range(2)]
    psT = P.psum("psT", [128, 4, 128], F32)
    psG = P.psum("psG", [128, 512], F32)

    S = {n: P.sem("m_" + n) for n in ("ld", "cst", "g", "gp", "gc", "st", "pt", "rs", "cb", "nd", "kw", "dc", "cs",
                                      "ss", "hn", "hg", "tr", "cp", "out", "sq1", "sq2")}
    P.op("gpsimd", lambda e: e.memset(mask[:], 1.0))
    P.op("gpsimd", lambda e: e.affine_select(out=mask[:], in_=mask[:], pattern=[[1, 128]], compare_op=ALU.is_ge,
                                             fill=0.0, base=0, channel_multiplier=-1))
    P.op("gpsimd", lambda e: e.memset(ident[:], 1.0))
    P.op("gpsimd", lambda e: e.affine_select(out=ident[:], in_=ident[:], pattern=[[1, 128]], compare_op=ALU.is_equal,
                                             fill=0.0, base=0, channel_multiplier=-1))
    P.op("gpsimd", lambda e: e.memset(ones_row[:], 1.0))
    P.op("gpsimd", lambda e: e.memset(one11[:], 1.0))
    P.op("gpsimd", lambda e: e.memset(va[:, :, 512:514], 1.0), inc=S["cst"])
    v_cst = S["cst"].n
    P.op("sync", lambda e: e.dma_start(out=ng[:], in_=ng_d), inc=S["ld"], dma=True)
    P.op("sync", lambda e: e.dma_start(out=gb[:], in_=gb_d), inc=S["ld"], dma=True)
    P.op("vector", lambda e: e.tensor_scalar_mul(gb[:, 0:1], gb[:, 0:1], 1.0 / 15.0), waits=[(S["ld"], S["ld"].n)])
    P.op("vector", lambda e: e.tensor_scalar_mul(gb[:, 1:2], gb[:, 1:2], -1.0))
    t_gb = P.tok("vector")

    n = 0
    for b in range(2):
        wprev = [(S["cp"], S["cp"].n), (S["cs"], S["cs"].n), (S["out"], S["out"].n)] if b > 0 else []
        P.op("sync", lambda e, b=b: e.dma_start(out=qs[:], in_=qT[b].rearrange("(c p) s -> p c s", p=128)),
             waits=wprev, inc=S["ld"], dma=True)
        P.op("sync", lambda e, b=b: e.dma_start(out=ks[:], in_=kT[b].rearrange("(c p) s -> p c s", p=128)),
             inc=S["ld"], dma=True)
        P.op("sync", lambda e, b=b: e.dma_start(out=ktm_s[:], in_=ktm[b].rearrange("(c p) d -> p c d", p=128)),
             inc=S["ld"], dma=True)
        P.op("sync", lambda e, b=b: e.dma_start(out=va[:, :, 0:512], in_=vtm[b].rearrange("(c p) d -> p c d", p=128)),
             inc=S["ld"], dma=True)
        P.op("sync", lambda e, b=b: e.dma_start(out=g_in[0][:], in_=gi_d[b:b + 1, :]), inc=S["ld"], dma=True)
        P.op("sync", lambda e, b=b: e.dma_start(out=g_in[1][:], in_=gf_d[b:b + 1, :]), inc=S["ld"], dma=True)
        v_ld = S["ld"].n
        gi_t, gf_t = g_in
        P.op("scalar", lambda e: e.activation(out=t2[:], in_=gf_t[:], func=AF.Exp, bias=gb[:, 1:2], scale=-1.0),
             waits=[(S["ld"], v_ld), (S["cs"], S["cs"].n), (S["hn"], S["hn"].n), t_gb])
        P.op("scalar", lambda e: e.activation(out=t2[:], in_=t2[:], func=AF.Ln, bias=1.0, scale=1.0))
        P.op("scalar", lambda e: e.activation(out=gi_t[:], in_=gi_t[:], func=AF.Tanh, bias=gb[:, 0:1], scale=1.0 / 15.0),
             inc=S["g"])
        vg = S["g"].n
        P.op("vector", lambda e: e.tensor_tensor_scan(out=cF[:], data0=ones_bc, data1=t2[:], initial=0.0,
                                                      op0=ALU.mult, op1=ALU.add), waits=[(S["g"], vg), (S["cst"], v_cst)])
        P.op("vector", lambda e: e.scalar_tensor_tensor(out=bv[:], in0=gi_t[:], scalar=15.0, in1=cF[:],
                                                        op0=ALU.mult, op1=ALU.add))
        P.op("vector", lambda e: e.tensor_tensor_scan(out=Mr[:], data0=ones_bc, data1=bv[:], initial=0.0,
                                                      op0=ALU.mult, op1=ALU.max))
        Mv = Mr[:].rearrange("p (c j) -> p c j", j=128)
        Mend = Mv[:, :, 127:128]
        P.op("vector", lambda e: e.tensor_tensor(out=wr[:].rearrange("p (c j) -> p c j", j=128),
                                                 in0=bv[:].rearrange("p (c j) -> p c j", j=128),
                                                 in1=Mend.to_broadcast([1, ML_NCH, 128]), op=ALU.subtract))
        P.op("vector", lambda e: e.tensor_tensor(out=fr[:].rearrange("p (c j) -> p c j", j=128),
                                                 in0=cF[:].rearrange("p (c j) -> p c j", j=128),
                                                 in1=Mend.to_broadcast([1, ML_NCH, 128]), op=ALU.subtract))
        P.op("vector", lambda e: e.memset(rcr[:], 0.0))
        P.op("vector", lambda e: e.tensor_tensor(out=rcr[:, 1:ML_NCH], in0=Mr[:, 127:ML_S - 128:128],
                                                 in1=Mr[:, 255:ML_S:128], op=ALU.subtract), inc=S["gp"])
        vgp = S["gp"].n
        P.op("scalar", lambda e: e.activation(out=wr[:], in_=wr[:], func=AF.Exp), waits=[(S["gp"], vgp)])
        P.op("scalar", lambda e: e.activation(out=fr[:], in_=fr[:], func=AF.Exp))
        P.op("scalar", lambda e: e.activation(out=rcr[:], in_=rcr[:], func=AF.Exp), inc=S["g"])
        vg2 = S["g"].n
        for c in range(ML_NCH):
            P.op("tensor", lambda e, c=c: e.matmul(psG[:, c:c + 1], wr[0:1, c * 128:(c + 1) * 128], one11[0:1, 0:1],
                                                   start=True, stop=True),
                 waits=[(S["g"], vg2), (S["gc"], S["gc"].n)] if c == 0 else ())
            P.op("tensor", lambda e, c=c: e.matmul(psG[:, 32 + c:33 + c], fr[0:1, c * 128:(c + 1) * 128], one11[0:1, 0:1],
                                                   start=True, stop=True))
        P.op("tensor", lambda e: e.matmul(psG[:, 64:96], ones_row[0:1, 0:128], rcr[0:1, 0:ML_NCH], start=True, stop=True),
             inc=S["gp"])
        vgp2 = S["gp"].n
        P.op("vector", lambda e: e.tensor_copy(wcol[:], psG[:, 0:32]), waits=[(S["gp"], vgp2)])
        P.op("vector", lambda e: e.tensor_copy(fcol[:], psG[:, 32:64]))
        P.op("vector", lambda e: e.tensor_copy(rcb[:], psG[:, 64:96]))
        P.op("vector", lambda e: e.memset(Cs[:], 0.0), inc=S["gc"])
        v_gc = S["gc"].n
        for c in range(ML_NCH):
            cs_ = slice(c * 128, (c + 1) * 128)
            sl = so_ring.next()
            sot = so_ring.tiles[sl]
            P.op("sync", lambda e, b=b, cs_=cs_, sot=sot: e.dma_start(out=sot[:], in_=sotm[b, cs_, :]),
                 waits=[(S["hg"], n - 2)], inc=so_ring.filled[sl], dma=True)
            for dc in range(2):
                P.op("tensor", lambda e, dc=dc, cs_=cs_: e.matmul(psA[:, 0:128], ks[:, dc, cs_], qs[:, dc, cs_],
                                                                  start=(dc == 0), stop=(dc == 1)),
                     waits=[(S["ld"], v_ld), (S["pt"], n)] if dc == 0 else (),
                     inc=S["st"] if dc == 1 else None)
            P.op("vector", lambda e, c=c: e.tensor_scalar_mul(Cs[:], Cs[:], rcb[:, c:c + 1]),
                 waits=[(S["gc"], v_gc)], inc=S["rs"])
            P.op("vector", lambda e, c=c: e.scalar_tensor_tensor(out=PT[:], in0=psA[:, 0:128], scalar=wcol[:, c:c + 1],
                                                                 in1=mask[:], op0=ALU.mult, op1=ALU.mult),
                 waits=[(S["st"], n + 1), (S["nd"], n), (S["dc"], n)], inc=S["pt"])
            P.op("scalar", lambda e: e.copy(Cb[:], Cs[:]), waits=[(S["rs"], n + 1), (S["nd"], n)], inc=S["cb"])
            P.op("scalar", lambda e, c=c: e.activation(out=kw[:], in_=ktm_s[:, c, :], func=AF.Copy, scale=wcol[:, c:c + 1]),
                 waits=[(S["ld"], v_ld), (S["gc"], v_gc), (S["dc"], n)], inc=S["kw"])
            P.op("tensor", lambda e, c=c: e.matmul(psB[:, :], PT[:], va[:, c, 0:512], start=True, stop=False),
                 waits=[(S["pt"], n + 1), (S["cb"], n + 1), (S["hn"], n), (S["ss"], n), (S["cst"], v_cst)])
            for dc in range(2):
                P.op("tensor", lambda e, dc=dc, cs_=cs_: e.matmul(psB[:, :], qs[:, dc, cs_], Cb[:, dc, 0:512],
                                                                  start=False, stop=(dc == 1)))
            P.op("tensor", lambda e, c=c: e.matmul(psC[:, 0:1], PT[:], va[:, c, 512:513], start=True, stop=False))
            for dc in range(2):
                P.op("tensor", lambda e, dc=dc, cs_=cs_: e.matmul(psC[:, 0:1], qs[:, dc, cs_], Cb[:, dc, 512:513],
                                                                  start=False, stop=(dc == 1)),
                     inc=S["nd"] if dc == 1 else None)
            for dc in range(2):
                P.op("tensor", lambda e, dc=dc, c=c: e.matmul(psD[dc][:, :], kw[:, dc * 128:(dc + 1) * 128], va[:, c, 0:512],
                                                              start=True, stop=True),
                     waits=[(S["kw"], n + 1), (S["cs"], n)] if dc == 0 else ())
            for dc in range(2):
                P.op("tensor", lambda e, dc=dc, c=c: e.matmul(psC[:, 8 + dc:9 + dc], kw[:, dc * 128:(dc + 1) * 128],
                                                              va[:, c, 512:513], start=True, stop=True),
                     inc=S["dc"] if dc == 1 else None)
            P.op("scalar", lambda e: e.memzero(sm[:, 0:1]))
            P.op("scalar", lambda e: e.activation(out=junk[:], in_=psB[:, :], func=AF.Square, accum_out=sm[:, 0:1]),
                 waits=[(S["nd"], n + 1), (S["hn"], n)], inc=S["ss"])
            P.op("vector", lambda e: e.tensor_scalar_mul(sm[:, 6:7], psC[:, 0:1], -1.0), waits=[(S["nd"], n + 1)])
            P.op("vector", lambda e: e.tensor_tensor(out=sm[:, 1:2], in0=sm[:, 6:7], in1=psC[:, 0:1], op=ALU.max))
            P.op("vector", lambda e, c=c: e.tensor_tensor(out=sm[:, 1:2], in0=sm[:, 1:2], in1=fcol[:, c:c + 1], op=ALU.max))
            P.op("vector", lambda e: e.reciprocal(sm[:, 2:3], sm[:, 1:2]))
            P.op("vector", lambda e: e.tensor_tensor(out=sm[:, 3:4], in0=sm[:, 2:3], in1=sm[:, 2:3], op=ALU.mult),
                 waits=[(S["ss"], n + 1)])
            P.op("vector", lambda e: e.tensor_tensor(out=sm[:, 3:4], in0=sm[:, 3:4], in1=sm[:, 0:1], op=ALU.mult))
            P.op("vector", lambda e: e.tensor_scalar(out=sm[:, 3:4], in0=sm[:, 3:4], scalar1=1.0 / 512.0, scalar2=ML_EPS,
                                                     op0=ALU.mult, op1=ALU.add))
            vq = P.op("vector", lambda e: e.tensor_copy(sm[:, 7:8], sm[:, 3:4]), inc=S["sq1"])
            vq2 = P.op("scalar", lambda e: e.sqrt(sm[:, 7:8], sm[:, 7:8]), waits=[(S["sq1"], vq)], inc=S["sq2"])
            P.op("vector", lambda e: e.reciprocal(sm[:, 4:5], sm[:, 7:8]), waits=[(S["sq2"], vq2)])
            P.op("vector", lambda e: e.tensor_tensor(out=sm[:, 5:6], in0=sm[:, 4:5], in1=sm[:, 2:3], op=ALU.mult))
            P.op("vector", lambda e: e.scalar_tensor_tensor(out=hn[:], in0=psB[:, :], scalar=sm[:, 5:6], in1=ng[:],
                                                            op0=ALU.mult, op1=ALU.mult),
                 waits=[(S["hg"], n)], inc=S["hn"])
            for dc in range(2):
                P.op("vector", lambda e, dc=dc: e.tensor_tensor(out=Cs[:, dc, 0:512], in0=Cs[:, dc, 0:512], in1=psD[dc][:, :],
                                                                op=ALU.add),
                     waits=[(S["dc"], n + 1)] if dc == 0 else ())
            P.op("vector", lambda e: e.tensor_tensor(out=Cs[:, :, 512], in0=Cs[:, :, 512], in1=psC[:, 8:10], op=ALU.add),
                 inc=S["cs"])
            P.op("gpsimd", lambda e, sot=sot: e.tensor_tensor(out=hg[:], in0=hn[:], in1=sot[:], op=ALU.mult),
                 waits=[(S["hn"], n + 1), so_ring.fill_wait(sl), (S["tr"], n)], inc=S["hg"])
            for j in range(4):
                P.op("tensor", lambda e, j=j: e.transpose(psT[:, j, :], hg[:, j * 128:(j + 1) * 128], ident[:]),
                     waits=[(S["hg"], n + 1), (S["cp"], n)] if j == 0 else (), inc=S["tr"] if j == 3 else None)
            grp = (n // 8)
            hts = hts_r[grp % 2]
            lc = slice((c % 8) * 128, (c % 8 + 1) * 128)
            wl = [(S["tr"], n + 1)]
            if c % 8 == 0 and grp >= 2:
                wl.append((S["out"], 16 * (grp - 1)))
            P.op("scalar", lambda e, lc=lc, hts=hts: e.copy(hts[:, :, lc], psT[:, :, :]), waits=wl, inc=S["cp"])
            n += 1
            if c % 8 == 7:
                g0 = (c // 8) * 1024
                P.op("sync", lambda e, b=b, hts=hts, g0=g0: e.dma_start(
                    out=hT[b, :, g0:g0 + 1024].rearrange("(j p) s -> p j s", p=128), in_=hts[:]),
                     waits=[(S["cp"], n)], inc=S["out"], dma=True)
    if debug:
        dw = [(S["cp"], S["cp"].n), (S["cs"], S["cs"].n), (S["hg"], S["hg"].n)]
        P.op("sync", lambda e: e.dma_start(out=dbg1[:, 0:32], in_=wcol[:]), waits=dw, inc=S["out"], dma=True)
        P.op("sync", lambda e: e.dma_start(out=dbg1[:, 32:64], in_=fcol[:]), inc=S["out"], dma=True)
        P.op("sync", lambda e: e.dma_start(out=dbg1[:, 64:96], in_=rcb[:]), inc=S["out"], dma=True)
        P.op("sync", lambda e: e.dma_start(out=dbg2, in_=hn[:]), inc=S["out"], dma=True)
        P.op("sync", lambda e: e.dma_start(out=dbg3, in_=hg[:]), inc=S["out"], dma=True)
        P.op("sync", lambda e: e.dma_start(out=dbg4, in_=PT[:]), inc=S["out"], dma=True)
        P.op("sync", lambda e: e.dma_start(out=dbg5, in_=sm[:]), inc=S["out"], dma=True)
        P.op("sync", lambda e: e.dma_start(out=dbg6, in_=Mr[:]), inc=S["out"], dma=True)
    P.wait("sync", S["out"], S["out"].n)
    P.run()
    return nc


LN_EPS_P = LN_EPS / (ALPHA * ALPHA)


class LNCtx:
    def __init__(self, T):
        P = T.P
        self.T = T
        self.xs = Slots(P, "lx", 2, [128, TT], F32)
        self.zs = Slots(P, "lz", 2, [128, TT], F32)
        self.sq = Slots(P, "lq", 2, [128, TT], F32)
        self.ones_col = P.sbuf("ones_col", [128, 1], F32)
        self.ones_row = P.sbuf("ones_row1", [1, 128], F32)
        self.rows = P.sbuf("ln_rows", [1, 3, TT], F32)
        self.rstd_b = P.sbuf("rstd_b", [128, TT], F32)
        self.nmr_b = P.sbuf("nmr_b", [128, TT], F32)
        self.s_stat = P.sem("s_stat")
        P.op("gpsimd", lambda e: e.memset(self.ones_col[:], 1.0))
        P.op("gpsimd", lambda e: e.memset(self.ones_row[:], 1.0))
        self.t_init = P.tok("gpsimd")
        self.zwrite = {}
        self.stat_rel = []
        self.bc_rel = []
        self.rows_rel = []
        self.last_stat = None
        self.out_toks = []


def residual_ep(T, L, x_src, xwaits, zd, t, gA):
    P = T.P
    L.zwrite = {}

    def ep(bi, oc, bank, fw):
        ch = bi * 4 + oc
        ps = T.ps[bank]
        xs, xt, xrel = L.xs.get()
        v = P.op("sync", lambda e: e.dma_start(out=xt[:], in_=x_src[ch * 128:(ch + 1) * 128, t * TT:(t + 1) * TT]),
                 waits=xrel + list(xwaits(ch)), inc=L.xs.sem_in[xs], dma=True)
        tx = (L.xs.sem_in[xs], v)
        zs, zt, zrel = L.zs.get()
        v = P.op("vector", lambda e: e.scalar_tensor_tensor(out=zt[:], in0=ps[:, 0:TT], scalar=gA[:, ch:ch + 1], in1=xt[:],
                                                            op0=ALU.mult, op1=ALU.add),
                 waits=[fw, tx] + zrel, inc=T.pbank.freed[bank])
        tz = (T.pbank.freed[bank], v)
        L.xs.release(xs, [tz])
        sq, sqt, sqrel = L.sq.get()
        P.op("scalar", lambda e: e.activation(out=sqt[:], in_=zt[:], func=AF.Square), waits=[tz] + sqrel)
        tsq = P.tok("scalar")
        v = P.op("sync", lambda e: e.dma_start(out=zd[ch * 128:(ch + 1) * 128, :], in_=zt[:]), waits=[tz],
                 inc=L.zs.sem_out[zs], dma=True)
        tdma = (L.zs.sem_out[zs], v)
        L.zwrite[ch] = tdma
        first, last = ch == 0, ch == KC - 1
        w1 = [tz, L.t_init] + (L.stat_rel if first else [])
        P.op("tensor", lambda e: e.matmul(T.ps[6][0:1, 0:TT], L.ones_col[:, 0:1], zt[:], start=first, stop=last), waits=w1)
        v = P.op("tensor", lambda e: e.matmul(T.ps[7][0:1, 0:TT], L.ones_col[:, 0:1], sqt[:], start=first, stop=last),
                 waits=[tsq], inc=L.s_stat)
        tstat = (L.s_stat, v)
        L.last_stat = tstat
        L.zs.release(zs, [tdma, tstat])
        L.sq.release(sq, [tstat])

    return ep


def ln_finish(T, L, zd, t, lng, lnb, x_dst, nxt=None, final=False):
    P = T.P
    rows = L.rows
    P.op("vector", lambda e: e.tensor_scalar_mul(rows[:, 0, :], T.ps[6][0:1, 0:TT], 1.0 / D), waits=[L.last_stat] + L.rows_rel)
    P.op("vector", lambda e: e.tensor_scalar_mul(rows[:, 1, :], T.ps[7][0:1, 0:TT], 1.0 / D))
    tcopy = P.tok("vector")
    P.op("vector", lambda e: e.tensor_tensor(out=rows[:, 2, :], in0=rows[:, 0, :], in1=rows[:, 0, :], op=ALU.mult))
    P.op("vector", lambda e: e.tensor_tensor(out=rows[:, 1, :], in0=rows[:, 1, :], in1=rows[:, 2, :], op=ALU.subtract))
    P.op("vector", lambda e: e.tensor_scalar_add(rows[:, 1, :], rows[:, 1, :], LN_EPS_P))
    t1 = P.tok("vector")
    P.op("scalar", lambda e: e.sqrt(rows[:, 2, :], rows[:, 1, :]), waits=[t1])
    t2 = P.tok("scalar")
    P.op("vector", lambda e: e.reciprocal(rows[:, 1, :], rows[:, 2, :]), waits=[t2])
    P.op("vector", lambda e: e.scalar_tensor_tensor(out=rows[:, 2, :], in0=rows[:, 0, :], scalar=-1.0, in1=rows[:, 1, :],
                                                    op0=ALU.mult, op1=ALU.mult))
    t3 = P.tok("vector")
    P.op("tensor", lambda e: e.matmul(T.ps[6][:, 0:TT], L.ones_row[0:1, 0:128], rows[0:1, 1, :], start=True, stop=True),
         waits=[t3, tcopy])
    v = P.op("tensor", lambda e: e.matmul(T.ps[7][:, 0:TT], L.ones_row[0:1, 0:128], rows[0:1, 2, :], start=True, stop=True),
             inc=L.s_stat)
    tb = (L.s_stat, v)
    P.op("vector", lambda e: e.tensor_copy(L.rstd_b[:], T.ps[6][:, 0:TT]), waits=[tb] + L.bc_rel)
    P.op("vector", lambda e: e.tensor_copy(L.nmr_b[:], T.ps[7][:, 0:TT]))
    tbc = P.tok("vector")
    L.stat_rel = [tbc]
    L.rows_rel = [tb]
    xw = {}
    tn = None
    for c in range(KC):
        xs, xt, xrel = L.xs.get()
        v = P.op("sync", lambda e, xt=xt, c=c: e.dma_start(out=xt[:], in_=zd[c * 128:(c + 1) * 128, :]),
                 waits=xrel + [L.zwrite[c]], inc=L.xs.sem_in[xs], dma=True)
        tin = (L.xs.sem_in[xs], v)
        P.op("vector", lambda e, xt=xt: e.tensor_tensor(out=xt[:], in0=xt[:], in1=L.rstd_b[:], op=ALU.mult), waits=[tin, tbc])
        P.op("vector", lambda e, xt=xt: e.tensor_tensor(out=xt[:], in0=xt[:], in1=L.nmr_b[:], op=ALU.add))
        tv = P.tok("vector")
        zs, zt, zrel = L.zs.get()
        P.op("scalar", lambda e, xt=xt, zt=zt, c=c: e.activation(out=zt[:], in_=xt[:], func=AF.Identity,
                                                                 bias=lnb[:, c:c + 1], scale=lng[:, c:c + 1]),
             waits=[tv] + zrel)
        tx = P.tok("scalar")
        L.xs.release(xs, [tx])
        v = P.op("sync", lambda e, zt=zt, c=c: e.dma_start(out=x_dst[c * 128:(c + 1) * 128, t * TT:(t + 1) * TT], in_=zt[:]),
                 waits=[tx], inc=L.zs.sem_out[zs], dma=True)
        tout = (L.zs.sem_out[zs], v)
        xw[c] = tout
        if final:
            L.out_toks.append(tout)
        rel = [tout]
        if nxt is not None:
            act, onepsc, sh, relw = nxt
            P.op("vector", lambda e, zt=zt, c=c, act=act, onepsc=onepsc, sh=sh: e.tensor_scalar(
                out=act[:, c, :], in0=zt[:], scalar1=onepsc[:, c:c + 1], scalar2=sh[:, c:c + 1], op0=ALU.mult, op1=ALU.add),
                waits=[tx] + (list(relw) if c == 0 else []))
            tn = P.tok("vector")
            rel.append(tn)
        L.zs.release(zs, rel)
    L.bc_rel = [P.tok("vector")]
    return xw, tn


def load_mod(P, modv_d, ncols):
    t = P.sbuf("modv_sb", [128, ncols], F32)
    s = P.sem("s_modv")
    v = P.op("sync", lambda e: e.dma_start(out=t[:], in_=modv_d), inc=s, dma=True)
    return t, (s, v)


def ffn(T, L, w_in, w_out, x_src, xwaits, zd, t, gA, act_ready, sg):
    P = T.P
    blocks = [[(j * 256, 256), (FF + j * 256, 256)] for j in range(FF // 256)]

    sg_state = {}

    def ep_in(bi, oc, bank, fw):
        ps = T.ps[bank]
        if oc < 2:
            s, st, rel = sg.get()
            P.op("scalar", lambda e: e.activation(out=st[:], in_=ps[:, 0:TT], func=AF.Silu), waits=[fw] + rel,
                 inc=T.pbank.freed[bank])
            sg_state[oc] = (s, st, P.tok("scalar"))
        else:
            s, st, tk = sg_state[oc - 2]
            hc = bi * 2 + (oc - 2)
            w = [fw, tk] + (list(T.hid_rel) if (bi == 0 and oc == 2) else [])
            P.op("vector", lambda e: e.tensor_tensor(out=T.hid[:, hc, :], in0=st[:], in1=ps[:, 0:TT], op=ALU.mult),
                 waits=w, inc=T.pbank.freed[bank])
            sg.release(s, [P.tok("vector")])

    lf = T.gemm(w_in, D, blocks, ep_in, act_waits=[act_ready])
    T.act_rel = [lf]
    hid_ready = P.tok("vector")
    blocks2 = [[(c0, 512)] for c0 in range(0, D, 512)]
    lf2 = T.gemm(w_out, FF, blocks2, residual_ep(T, L, x_src, xwaits, zd, t, gA), act=T.hid, act_waits=[hid_ready])
    T.hid_rel = [lf2]


def build_phase_d1(mode="d1"):
    nc = bass.Bass("TRN2", target_bir_lowering=False)
    xT = _dram(nc, "xT", [D, NTOK], F32, "ExternalInput")
    if mode == "d1":
        hT = _dram(nc, "hT", [D, NTOK], BF16, "ExternalInput")
    else:
        yT = _dram(nc, "yT", [D, NTOK], F32, "ExternalInput")
        gT = _dram(nc, "gT", [D, NTOK], BF16, "ExternalInput")
    w_o = _dram(nc, "w_o", [D, D], F32, "ExternalInput")
    w_in = _dram(nc, "w_in", [D, 2 * FF], F32, "ExternalInput")
    w_out = _dram(nc, "w_out", [FF, D], F32, "ExternalInput")
    modv_d = _dram(nc, "modv", [128, 8 * KC], F32, "ExternalInput")
    outT = _dram(nc, "outT", [D, NTOK], F32, "ExternalOutput")
    x1T = nc.dram_tensor("x1T", [D, NTOK], F32, kind="Internal").ap()
    zd = nc.dram_tensor("zscr", [D, TT], F32, kind="Internal").ap()
    P = Prog(nc)
    P.serial = {"vector", "scalar", "gpsimd"}
    T = Tok(nc, P, hid_chunks=86, rings=False)
    L = LNCtx(T)
    sg = L.sq
    mv, tmod = load_mod(P, modv_d, 8 * KC)
    col = lambda i: mv[:, i * KC:(i + 1) * KC]
    onepsc = P.sbuf("onepsc", [128, KC], F32)
    P.op("vector", lambda e: e.tensor_scalar_add(onepsc[:], col(4), 1.0), waits=[tmod])
    P.op("vector", lambda e: e.tensor_scalar_mul(col(0), col(0), 1.0 / ALPHA))
    P.op("vector", lambda e: e.tensor_scalar_mul(col(5), col(5), 1.0 / ALPHA))
    tconst = P.tok("vector")
    s_act = P.sem("s_actin")
    if mode != "d1":
        yin = Slots(P, "yin", 2, [128, TT], F32)
        gin = Slots(P, "gin", 2, [128, TT], BF16)
    for t in range(NT):
        if mode == "d1":
            v = P.op("sync", lambda e, t=t: e.dma_start(out=T.act[:], in_=hT[:, t * TT:(t + 1) * TT].rearrange("(c p) s -> p c s", p=128)),
                     waits=list(T.act_rel), inc=s_act, dma=True)
            act_ready = (s_act, v)
        else:
            for c in range(KC):
                ys, yt, yrel = yin.get()
                gs, gt, grel = gin.get()
                v1 = P.op("sync", lambda e, yt=yt, c=c, t=t: e.dma_start(out=yt[:], in_=yT[c * 128:(c + 1) * 128, t * TT:(t + 1) * TT]),
                          waits=yrel, inc=yin.sem_in[ys], dma=True)
                v2 = P.op("sync", lambda e, gt=gt, c=c, t=t: e.dma_start(out=gt[:], in_=gT[c * 128:(c + 1) * 128, t * TT:(t + 1) * TT]),
                          waits=grel, inc=gin.sem_in[gs], dma=True)
                P.op("vector", lambda e, yt=yt, gt=gt, c=c: e.tensor_tensor(out=T.act[:, c, :], in0=yt[:], in1=gt[:], op=ALU.mult),
                     waits=[(yin.sem_in[ys], v1), (gin.sem_in[gs], v2)] + (list(T.act_rel) if c == 0 else []))
                tk = P.tok("vector")
                yin.release(ys, [tk])
                gin.release(gs, [tk])
            act_ready = P.tok("vector")
        blocks = [[(c0, 512)] for c0 in range(0, D, 512)]
        lf = T.gemm(w_o, D, blocks, residual_ep(T, L, xT, lambda ch: [], zd, t, col(0)), act_waits=[act_ready, tconst])
        T.act_rel = [lf]
        xw, tn = ln_finish(T, L, zd, t, col(1), col(2), x1T, nxt=(T.act, onepsc, col(3), T.act_rel))
        ffn(T, L, w_in, w_out, x1T, lambda ch, xw=xw: [xw[ch]], zd, t, col(5), tn, sg)
        ln_finish(T, L, zd, t, col(6), col(7), outT, nxt=None, final=True)
    for tk in L.out_toks:
        P.wait("sync", *tk)
    P.run()
    return nc


class Buf:
    def __init__(self, name, dma_sem=None):
        self.name = name
        self.w = None
        self.r = []
        self.dma_sem = dma_sem


class Dep:
    def __init__(self, P):
        self.P = P
        self.s_pe = P.sem("dep_pe")
        self.n = 0
        self.selfwait = True

    def buf(self, name, dma=False):
        return Buf(name, self.P.sem("d_" + name) if dma else None)

    def _waits(self, reads, writes):
        ws = []
        for b in reads:
            if b.w is not None:
                ws.append(b.w)
        for b in writes:
            if b.w is not None:
                ws.append(b.w)
            ws.extend(b.r)
        return ws

    def _commit(self, tok, reads, writes):
        for b in reads:
            if b not in writes:
                b.r.append(tok)
        for b in writes:
            b.w = tok
            b.r = []

    def op(self, eng, fn, reads=(), writes=()):
        P = self.P
        P.op(eng, fn, waits=self._waits(reads, writes), selfwait=self.selfwait)
        tok = P.tok(eng)
        self._commit(tok, reads, writes)
        return tok

    def pe(self, fns, reads=(), writes=()):
        P = self.P
        ws = self._waits(reads, writes)
        for i, fn in enumerate(fns):
            if i == len(fns) - 1:
                v = P.op("tensor", fn, waits=ws if i == 0 else (), inc=self.s_pe)
            else:
                P.op("tensor", fn, waits=ws if i == 0 else ())
        tok = (self.s_pe, v)
        self._commit(tok, reads, writes)
        return tok

    def dma(self, eng, fn, sem_buf, reads=(), writes=()):
        P = self.P
        v = P.op(eng, fn, waits=self._waits(reads, writes), inc=sem_buf.dma_sem, dma=True)
        tok = (sem_buf.dma_sem, v)
        self._commit(tok, reads, writes)
        return tok


RW_TOK = 8192
RW_S = 4096
RW_C = 64
RW_WN = 64
RW_NCW = RW_WN // RW_C
GN_EPS = 64e-5
DEC = -0.6065306597126334


def build_phase_e(ntok=RW_TOK, seq=RW_S, stage=99):
    nc = bass.Bass("TRN2", target_bir_lowering=False)
    rT = _dram(nc, "rT", [512, ntok], BF16, "ExternalInput")
    kT = _dram(nc, "kT", [512, ntok], BF16, "ExternalInput")
    vT = _dram(nc, "vT", [512, ntok], BF16, "ExternalInput")
    wT = _dram(nc, "wT", [512, ntok], F32, "ExternalInput")
    aT = _dram(nc, "aT", [512, ntok], F32, "ExternalInput")
    pv_d = _dram(nc, "pv", [64, 48], F32, "ExternalInput")
    yT = _dram(nc, "yT", [512, ntok], F32, "ExternalOutput")
    P = Prog(nc)
    P.serial = {"vector", "scalar", "gpsimd"}
    dp = Dep(P)
    dp.selfwait = False
    W, NC_, C = RW_WN, RW_NCW, RW_C
    sb = lambda n, sh, dt: P.sbuf(n, sh, dt)
    V, S_, g = "vector", "scalar", "gpsimd"
    ones64 = sb("ones64", [64, 64], F32)
    mask2 = sb("mask2", [64, 1, 128], F32)
    maskLT = sb("maskLT", [64, 1, 64], F32)
    blkm = sb("blkm", [64, 1, 64], F32)
    ET = sb("ET", [4, 64], F32)
    identf = sb("identf", [64, 64], F32)
    I64 = sb("I64", [64, 1, 64], F32)
    segm = sb("segm", [64, 8, W], F32)
    pv = sb("pv_s", [64, 48], F32)
    P.op(g, lambda e: e.memset(ones64[:], 1.0))
    P.op(g, lambda e: e.memset(mask2[:], 1.0))
    P.op(g, lambda e: e.affine_select(out=mask2[:, 0, 0:64], in_=mask2[:, 0, 0:64], pattern=[[1, 64]], compare_op=ALU.is_ge,
                                      fill=0.0, base=-1, channel_multiplier=-1))
    P.op(g, lambda e: e.affine_select(out=mask2[:, 0, 64:128], in_=mask2[:, 0, 64:128], pattern=[[1, 64]], compare_op=ALU.is_ge,
                                      fill=0.0, base=0, channel_multiplier=-1))
    P.op(g, lambda e: e.memset(maskLT[:], 1.0))
    P.op(g, lambda e: e.affine_select(out=maskLT[:, 0, :], in_=maskLT[:, 0, :], pattern=[[-1, 64]], compare_op=ALU.is_ge,
                                      fill=0.0, base=-1, channel_multiplier=1))
    P.op(g, lambda e: e.memset(identf[:], 1.0))
    P.op(g, lambda e: e.affine_select(out=identf[:], in_=identf[:], pattern=[[1, 64]], compare_op=ALU.is_equal,
                                      fill=0.0, base=0, channel_multiplier=-1))
    P.op(g, lambda e: e.memset(ET[:], 1.0))
    P.op(g, lambda e: e.affine_select(out=ET[:], in_=ET[:], pattern=[[1, 64]], compare_op=ALU.is_ge, fill=0.0, base=0, channel_multiplier=-16))
    P.op(g, lambda e: e.affine_select(out=ET[:], in_=ET[:], pattern=[[-1, 64]], compare_op=ALU.is_ge, fill=0.0, base=15, channel_multiplier=16))
    P.op(g, lambda e: e.tensor_copy(I64[:, 0, :], identf[:]))
    P.op(g, lambda e: e.memset(segm[:], 1.0))
    P.op(g, lambda e: e.memset(segm[:].rearrange("p h (c j) -> p (h c) j", j=C)[:, :, 0:1], 0.0))
    cb = dp.buf("const")
    cb.w = P.tok(g)
    Q_tmp = None
    pvb = dp.buf("pv", dma=True)
    dp.dma("sync", lambda e: e.dma_start(out=pv[:], in_=pv_d), pvb, writes=[pvb])
    dp.op(V, lambda e: e.tensor_scalar(out=pv[:, 16:24], in0=pv[:, 8:16], scalar1=-1.0, scalar2=1.0, op0=ALU.mult, op1=ALU.add),
          reads=[pvb], writes=[pvb])
    pbc = lambda q: pv[:, q * 8:(q + 1) * 8].unsqueeze(2).to_broadcast([64, 8, W])

    WT = []
    for i_ in range(2):
        sfx = str(i_)
        WT.append(dict(
            r_w=sb("r_w" + sfx, [64, 8, W], BF16), k_w=sb("k_w" + sfx, [64, 8, W], BF16), v_w=sb("v_w" + sfx, [64, 8, W], BF16),
            w_w=sb("w_w" + sfx, [64, 8, W], F32), a_w=sb("a_w" + sfx, [64, 8, W], F32), v_w32=sb("v_w32" + sfx, [64, 8, W], F32),
            AR=sb("AR" + sfx, [64, 8, NC_, 2, C], F32), BK=sb("BK" + sfx, [64, 8, NC_, 2, C], F32), BKh=sb("BKh" + sfx, [64, 8, 2, W], F32),
            gC=sb("gC" + sfx, [64, 8, NC_], F32), bonus=sb("bonus" + sfx, [64, 8, W], F32),
            Vt=sb("Vt" + sfx, [64, NC_, 512], F32), Bt=sb("Bt" + sfx, [64, NC_, 512], F32), Kt=sb("Kt" + sfx, [64, NC_, 512], F32),
            B_in=dp.buf("in" + sfx, dma=True), B_v32=dp.buf("v32" + sfx), B_AR=dp.buf("AR" + sfx), B_BK=dp.buf("BK" + sfx),
            B_BKh=dp.buf("BKh" + sfx), B_gC=dp.buf("gC" + sfx), B_bonus=dp.buf("bonus" + sfx), B_tm=dp.buf("tokmajor" + sfx),
            A1s=sb("A1s" + sfx, [64, 8, 128], F32), A2s=sb("A2s" + sfx, [64, 8, 128], F32), Gb=sb("Gb" + sfx, [64, 8, 64], F32),
            B_A1s=dp.buf("A1s" + sfx), B_A2s=dp.buf("A2s" + sfx), B_G=dp.buf("G" + sfx)))
    YW = 256
    ystage_ = [sb(f"ystage{i_}", [64, 8, YW], F32) for i_ in range(2)]
    B_ystage_ = [dp.buf(f"ystage{i_}", dma=True) for i_ in range(2)]
    UNP = ("r_w, k_w, v_w, w_w, a_w, v_w32, AR, BK, BKh, gC, bonus, Vt, Bt, Kt, B_in, B_v32, B_AR, B_BK, B_BKh, B_gC, B_bonus, B_tm, "
           "A1s, A2s, Gb, B_A1s, B_A2s, B_G")
    unp = lambda pb: tuple(WT[pb][n_.strip()] for n_ in UNP.split(","))
    tn = ("sg", "lg", "lgp", "d", "egi", "kk", "sq", "k2", "bv")
    tmp = {n: sb("t_" + n, [64, 8, W], F32) for n in tn}
    B_t = {n: dp.buf("t_" + n) for n in tn}
    Xs = [sb(f"Xs{i}", [64, 8, 64], F32) for i in range(2)]
    Ys = [sb(f"Ys{i}", [64, 8, 64], F32) for i in range(2)]
    Hb_ = sb("Hb_", [64, 8, 64], F32)
    Lo = sb("Lo", [64, 8, 64], F32); LoT = sb("LoT", [64, 8, 64], F32)
    B_Lo = dp.buf("Lo"); B_LoT = dp.buf("LoT")
    X1s = sb("X1s", [64, 512], F32); Us = sb("Us", [64, 512], F32)
    Hf = sb("Hf", [64, 8, 64], F32); Hbf = Hf
    ysq = sb("ysq", [64, 512], F32); yn = sb("yn", [64, 512], F32)
    st = sb("st", [64, 6, 8], F32)
    B_Xs = [dp.buf("Xs0"), dp.buf("Xs1")]; B_Ys = [dp.buf("Ys0"), dp.buf("Ys1")]
    B_H = dp.buf("H"); B_X1s = dp.buf("X1s"); B_Us = dp.buf("Us")
    B_Hf = dp.buf("Hf"); B_Hf = dp.buf("Hbf"); B_ysq = dp.buf("ysq"); B_yn = dp.buf("yn"); B_st = dp.buf("st")
    Q = [P.psum(f"Q{i}", [128, 512], F32) for i in range(8)]
    B_Q = [dp.buf(f"Q{i}") for i in range(8)]
    h3 = lambda q, j: q[0:64, :].rearrange("p (h j) -> p h j", j=j)
    fl = lambda t: t[:].rearrange("p h j -> p (h j)")
    B_blk = dp.buf("blk")
    dp.pe([lambda e: e.matmul(Q[0][0:64, 0:64], ET[:], ET[:], start=True, stop=True)], reads=[cb], writes=[B_Q[0]])
    dp.op(V, lambda e: e.tensor_copy(blkm[:, 0, :], Q[0][0:64, 0:64]), reads=[B_Q[0]], writes=[cb])

    def prep(t0, pb):
        (r_w, k_w, v_w, w_w, a_w, v_w32, AR, BK, BKh, gC, bonus, Vt, Bt, Kt, B_in, B_v32, B_AR, B_BK, B_BKh, B_gC, B_bonus, B_tm,
         A1s, A2s, Gb, B_A1s, B_A2s, B_G) = unp(pb)
        for (dst, src) in ((r_w, rT), (k_w, kT), (v_w, vT), (w_w, wT), (a_w, aT)):
            dp.dma("sync", lambda e, dst=dst, src=src: e.dma_start(out=dst[:], in_=src[:, t0:t0 + W].rearrange("(h n) s -> n h s", n=64)),
                   B_in, writes=[B_in])
        dp.op(S_, lambda e: e.copy(v_w32[:], v_w[:]), reads=[B_in], writes=[B_v32])
        T_ = tmp
        c4 = lambda ap: ap.rearrange("p h (c j) -> p h c j", j=C)
        Bi = B_in
        dp.op(S_, lambda e: e.activation(out=T_["sg"][:], in_=w_w[:], func=AF.Sigmoid), reads=[Bi], writes=[B_t["sg"]])
        dp.op(V, lambda e: e.tensor_scalar_mul(T_["sg"][:], T_["sg"][:], DEC), writes=[B_t["sg"]])
        dp.op(V, lambda e: e.tensor_tensor_scan(out=fl(T_["lg"]), data0=fl(segm), data1=fl(T_["sg"]), initial=0.0,
                                                op0=ALU.mult, op1=ALU.add), reads=[B_t["sg"], cb], writes=[B_t["lg"]])
        dp.op("gpsimd", lambda e: e.tensor_tensor(out=T_["lgp"][:], in0=T_["lg"][:], in1=T_["sg"][:], op=ALU.subtract),
              reads=[B_t["lg"], B_t["sg"]], writes=[B_t["lgp"]])
        for h in range(8):
            dp.op(V, lambda e, h=h: e.tensor_tensor(out=c4(T_["d"][:])[:, h], in0=c4(T_["lg"][:])[:, h, :, C - 1:C].to_broadcast([64, NC_, C]),
                                                    in1=c4(T_["lg"][:])[:, h], op=ALU.subtract), reads=[B_t["lg"]], writes=[B_t["d"]])
        yield
        dp.op(S_, lambda e: e.activation(out=gC[:], in_=c4(T_["lg"][:])[:, :, :, C - 1], func=AF.Exp), reads=[B_t["lg"]], writes=[B_gC])
        dp.op(S_, lambda e: e.activation(out=T_["egi"][:], in_=T_["lg"][:], func=AF.Exp, scale=-1.0), reads=[B_t["lg"]], writes=[B_t["egi"]])
        dp.op(S_, lambda e: e.activation(out=T_["lg"][:], in_=T_["lg"][:], func=AF.Exp), writes=[B_t["lg"]])
        dp.op(S_, lambda e: e.activation(out=T_["lgp"][:], in_=T_["lgp"][:], func=AF.Exp), writes=[B_t["lgp"]])
        dp.op(S_, lambda e: e.activation(out=T_["d"][:], in_=T_["d"][:], func=AF.Exp), writes=[B_t["d"]])
        yield
        dp.op("gpsimd", lambda e: e.tensor_tensor(out=T_["kk"][:], in0=k_w[:], in1=pbc(0), op=ALU.mult), reads=[Bi, pvb], writes=[B_t["kk"]])
        dp.op(S_, lambda e: e.activation(out=T_["sq"][:], in_=T_["kk"][:], func=AF.Square), reads=[B_t["kk"]], writes=[B_t["sq"]])
        for i in range(8 * W // 512):
            dp.pe([lambda e, i=i: e.matmul(Q[2][0:64, :], ones64[:], fl(T_["sq"])[:, i * 512:(i + 1) * 512], start=True, stop=True)],
                  reads=[B_t["sq"], cb], writes=[B_Q[2]])
            dp.op(S_, lambda e, i=i: e.sqrt(fl(T_["sq"])[:, i * 512:(i + 1) * 512], Q[2][0:64, :]), reads=[B_Q[2]], writes=[B_t["sq"]])
        dp.op(V, lambda e: e.tensor_scalar_max(T_["sq"][:], T_["sq"][:], 1e-12), writes=[B_t["sq"]])
        dp.op(V, lambda e: e.reciprocal(T_["sq"][:], T_["sq"][:]), writes=[B_t["sq"]])
        dp.op(V, lambda e: e.tensor_tensor(out=T_["kk"][:], in0=T_["kk"][:], in1=T_["sq"][:], op=ALU.mult),
              reads=[B_t["sq"]], writes=[B_t["kk"]])
        yield
        dp.op("gpsimd", lambda e: e.tensor_tensor(out=T_["k2"][:], in0=a_w[:], in1=pbc(1), op=ALU.mult), reads=[Bi, pvb], writes=[B_t["k2"]])
        dp.op("gpsimd", lambda e: e.tensor_tensor(out=T_["k2"][:], in0=T_["k2"][:], in1=pbc(2), op=ALU.add), writes=[B_t["k2"]])
        dp.op("gpsimd", lambda e: e.tensor_tensor(out=T_["k2"][:], in0=T_["k2"][:], in1=k_w[:], op=ALU.mult), reads=[Bi], writes=[B_t["k2"]])
        dp.op(V, lambda e: e.tensor_tensor(out=T_["bv"][:], in0=T_["kk"][:], in1=a_w[:], op=ALU.mult),
              reads=[Bi, B_t["kk"]], writes=[B_t["bv"]])
        yield
        dp.op("gpsimd", lambda e: e.tensor_tensor(out=AR[:, :, :, 1, :], in0=c4(r_w[:]), in1=c4(T_["lg"][:]), op=ALU.mult),
              reads=[Bi, B_t["lg"]], writes=[B_AR])
        dp.op(V, lambda e: e.scalar_tensor_tensor(out=AR[:, :, :, 0, :], in0=c4(T_["kk"][:]), scalar=-1.0, in1=c4(T_["lgp"][:]),
                                                  op0=ALU.mult, op1=ALU.mult), reads=[B_t["kk"], B_t["lgp"]], writes=[B_AR])
        dp.op(V, lambda e: e.tensor_tensor(out=BK[:, :, :, 0, :], in0=c4(T_["bv"][:]), in1=c4(T_["egi"][:]), op=ALU.mult),
              reads=[B_t["bv"], B_t["egi"]], writes=[B_BK])
        dp.op("gpsimd", lambda e: e.tensor_tensor(out=BK[:, :, :, 1, :], in0=c4(T_["k2"][:]), in1=c4(T_["egi"][:]), op=ALU.mult),
              reads=[B_t["k2"], B_t["egi"]], writes=[B_BK])
        dp.op(V, lambda e: e.tensor_tensor(out=BKh[:, :, 0, :], in0=T_["bv"][:], in1=T_["d"][:], op=ALU.mult),
              reads=[B_t["bv"], B_t["d"]], writes=[B_BKh])
        dp.op("gpsimd", lambda e: e.tensor_tensor(out=BKh[:, :, 1, :], in0=T_["k2"][:], in1=T_["d"][:], op=ALU.mult),
              reads=[B_t["k2"], B_t["d"]], writes=[B_BKh])
        yield
        dp.op("gpsimd", lambda e: e.tensor_tensor(out=T_["sq"][:], in0=r_w[:], in1=pbc(3), op=ALU.mult), reads=[Bi, pvb], writes=[B_t["sq"]])
        dp.op("gpsimd", lambda e: e.tensor_tensor(out=T_["sq"][:], in0=T_["sq"][:], in1=T_["k2"][:], op=ALU.mult), reads=[B_t["k2"]], writes=[B_t["sq"]])
        for i in range(8 * W // 512):
            dp.pe([lambda e, i=i: e.matmul(Q[2][0:64, :], ones64[:], fl(T_["sq"])[:, i * 512:(i + 1) * 512], start=True, stop=True)],
                  reads=[B_t["sq"], cb], writes=[B_Q[2]])
            dp.op(V, lambda e, i=i: e.tensor_tensor(out=fl(bonus)[:, i * 512:(i + 1) * 512], in0=Q[2][0:64, :],
                                                    in1=fl(v_w)[:, i * 512:(i + 1) * 512], op=ALU.mult), reads=[B_Q[2], Bi], writes=[B_bonus])


    def tm_chunk(c, pb):
        (r_w, k_w, v_w, w_w, a_w, v_w32, AR, BK, BKh, gC, bonus, Vt, Bt, Kt, B_in, B_v32, B_AR, B_BK, B_BKh, B_gC, B_bonus, B_tm,
         A1s, A2s, Gb, B_A1s, B_A2s, B_G) = unp(pb)
        cs_ = slice(c * C, (c + 1) * C)
        for ti, (dst, srcf, rb) in enumerate(((Vt, lambda h: v_w32[:, h, cs_], B_v32), (Bt, lambda h: BKh[:, h, 0, cs_], B_BKh),
                                               (Kt, lambda h: BKh[:, h, 1, cs_], B_BKh))):
            dp.pe([lambda e, h=h, srcf=srcf: e.matmul(Q[3][0:64, h * 64:(h + 1) * 64], srcf(h), identf[:], start=True, stop=True)
                   for h in range(8)], reads=[rb, cb], writes=[B_Q[3]])
            if ti == 1:
                dp.op(S_, lambda e, dst=dst: e.copy(dst[:, c, :], Q[3][0:64, :]), reads=[B_Q[3]], writes=[B_tm])
            else:
                dp.op(V, lambda e, dst=dst: e.tensor_copy(dst[:, c, :], Q[3][0:64, :]), reads=[B_Q[3]], writes=[B_tm])


    def pre(c, pb):
        (r_w, k_w, v_w, w_w, a_w, v_w32, AR, BK, BKh, gC, bonus, Vt, Bt, Kt, B_in, B_v32, B_AR, B_BK, B_BKh, B_gC, B_bonus, B_tm,
         A1s, A2s, Gb, B_A1s, B_A2s, B_G) = unp(pb)
        hs = lambda h: slice(h * 64, (h + 1) * 64)
        dp.pe([lambda e, h=h: e.matmul(Q[h // 4][0:64, (h % 4) * 128:(h % 4 + 1) * 128], BK[:, h, c, 0, :], AR[:, h, c, :, :],
                                       start=True, stop=True) for h in range(8)], reads=[B_AR, B_BK], writes=[B_Q[0], B_Q[1]])
        dp.pe([lambda e, h=h: e.matmul(Q[2 + h // 4][0:64, (h % 4) * 128:(h % 4 + 1) * 128], BK[:, h, c, 1, :], AR[:, h, c, :, :],
                                       start=True, stop=True) for h in range(8)], reads=[B_AR, B_BK], writes=[B_Q[2], B_Q[3]])
        dp.pe([lambda e, h=h: e.matmul(Q[4][0:64, hs(h)], AR[:, h, c, 0, :], BK[:, h, c, 0, :], start=True, stop=True)
               for h in range(8)], reads=[B_AR, B_BK], writes=[B_Q[4]])
        yield
        m2b = mask2[:].to_broadcast([64, 4, 128])
        dp.op(V, lambda e: e.tensor_tensor(out=A1s[:, 0:4, :], in0=h3(Q[0], 128), in1=m2b, op=ALU.mult), reads=[B_Q[0], cb], writes=[B_A1s])
        dp.op(V, lambda e: e.tensor_tensor(out=A1s[:, 4:8, :], in0=h3(Q[1], 128), in1=m2b, op=ALU.mult), reads=[B_Q[1], cb], writes=[B_A1s])
        dp.op(V, lambda e: e.tensor_tensor(out=A2s[:, 0:4, :], in0=h3(Q[2], 128), in1=m2b, op=ALU.mult), reads=[B_Q[2], cb], writes=[B_A2s])
        dp.op(V, lambda e: e.tensor_tensor(out=A2s[:, 4:8, :], in0=h3(Q[3], 128), in1=m2b, op=ALU.mult), reads=[B_Q[3], cb], writes=[B_A2s])
        bm8 = blkm[:].to_broadcast([64, 8, 64])
        I8 = I64[:].to_broadcast([64, 8, 64])
        dp.op(V, lambda e: e.tensor_tensor(out=LoT[:], in0=h3(Q[4], 64), in1=maskLT[:].to_broadcast([64, 8, 64]), op=ALU.mult),
              reads=[B_Q[4], cb], writes=[B_LoT])
        dp.op(V, lambda e: e.tensor_tensor(out=Ys[0][:], in0=LoT[:], in1=bm8, op=ALU.mult), reads=[B_LoT, cb], writes=[B_Ys[0]])
        dp.op(V, lambda e: e.tensor_tensor(out=Xs[0][:], in0=A1s[:, :, 0:64], in1=bm8, op=ALU.mult), reads=[B_A1s, cb], writes=[B_Xs[0]])
        dp.op(V, lambda e: e.tensor_tensor(out=Lo[:], in0=A1s[:, :, 0:64], in1=Xs[0][:], op=ALU.subtract),
              reads=[B_A1s, B_Xs[0]], writes=[B_Lo])
        yield
        dp.op(V, lambda e: e.tensor_tensor(out=Gb[:], in0=Xs[0][:], in1=I8, op=ALU.add), reads=[B_Xs[0], cb], writes=[B_G])
        cur = 0
        for lvl in range(1, 4):
            nxt = 1 - cur
            if lvl < 3:
                dp.pe([lambda e, h=h, cur=cur: e.matmul(Q[0][0:64, hs(h)], Ys[cur][:, h, :], Xs[cur][:, h, :], start=True, stop=True)
                       for h in range(8)], reads=[B_Xs[cur], B_Ys[cur]], writes=[B_Q[0]])
            dp.pe([lambda e, h=h, cur=cur: e.matmul(Q[1][0:64, hs(h)], Xs[cur][:, h, :], Ys[cur][:, h, :], start=True, stop=True)
                   for h in range(8)], reads=[B_Xs[cur], B_Ys[cur]], writes=[B_Q[1]])
            if lvl < 3:
                dp.op(S_, lambda e, nxt=nxt: e.copy(fl(Xs[nxt]), Q[0][0:64, :]), reads=[B_Q[0]], writes=[B_Xs[nxt]])
            dp.op(V, lambda e, nxt=nxt: e.tensor_copy(fl(Ys[nxt]), Q[1][0:64, :]), reads=[B_Q[1]], writes=[B_Ys[nxt]])
            dp.pe([lambda e, h=h, nxt=nxt: e.matmul(Q[2][0:64, hs(h)], Ys[nxt][:, h, :], Gb[:, h, :], start=True, stop=True)
                   for h in range(8)], reads=[B_G, B_Ys[nxt]], writes=[B_Q[2]])
            dp.op(V, lambda e: e.tensor_tensor(out=fl(Gb), in0=fl(Gb), in1=Q[2][0:64, :], op=ALU.add), reads=[B_Q[2]], writes=[B_G])
            cur = nxt
            yield
        dp.pe([lambda e, h=h: e.transpose(Q[3][0:64, hs(h)], Gb[:, h, :], identf[:]) for h in range(8)], reads=[B_G, cb], writes=[B_Q[3]])
        dp.op(S_, lambda e: e.copy(fl(Hb_), Q[3][0:64, :]), reads=[B_Q[3]], writes=[B_H])
        ZTs, BZT = Ys[0], B_Ys[0]
        R1, BR1, R2, BR2 = Xs[0], B_Xs[0], Xs[1], B_Xs[1]
        dp.pe([lambda e, h=h: e.matmul(Q[1][0:64, hs(h)], Lo[:, h, :], Hb_[:, h, :], start=True, stop=True) for h in range(8)],
              reads=[B_H, B_Lo], writes=[B_Q[1]])
        dp.op(S_, lambda e: e.copy(fl(ZTs), Q[1][0:64, :]), reads=[B_Q[1]], writes=[BZT])
        yield
        dp.pe([lambda e, h=h: e.matmul(Q[0][0:64, hs(h)], ZTs[:, h, :], Gb[:, h, :], start=True, stop=True) for h in range(8)],
              reads=[BZT, B_G], writes=[B_Q[0]])
        dp.op(V, lambda e: e.tensor_tensor(out=fl(R1), in0=fl(Gb), in1=Q[0][0:64, :], op=ALU.add), reads=[B_Q[0], B_G], writes=[BR1])
        yield
        dp.pe([lambda e, h=h: e.matmul(Q[2][0:64, hs(h)], ZTs[:, h, :], R1[:, h, :], start=True, stop=True) for h in range(8)],
              reads=[BZT, BR1], writes=[B_Q[2]])
        dp.op(V, lambda e: e.tensor_tensor(out=fl(R2), in0=fl(Gb), in1=Q[2][0:64, :], op=ALU.add), reads=[B_Q[2], B_G], writes=[BR2])
        yield
        dp.pe([lambda e, h=h: e.matmul(Q[0][0:64, hs(h)], ZTs[:, h, :], R2[:, h, :], start=True, stop=True) for h in range(8)],
              reads=[BZT, BR2], writes=[B_Q[0]])
        dp.op(V, lambda e: e.tensor_tensor(out=fl(Gb), in0=fl(Gb), in1=Q[0][0:64, :], op=ALU.add), reads=[B_Q[0]], writes=[B_G])

    def seqg(c, pb, k):
        hs = lambda h: slice(h * 64, (h + 1) * 64)
        (r_w, k_w, v_w, w_w, a_w, v_w32, AR, BK, BKh, gC, bonus, Vt, Bt, Kt, B_in, B_v32, B_AR, B_BK, B_BKh, B_gC, B_bonus, B_tm,
         A1s, A2s, Gb, B_A1s, B_A2s, B_G) = unp(pb)
        ystage = ystage_[(k // 4) % 2]; B_ystage = B_ystage_[(k // 4) % 2]; yoff = (k % 4) * C
        fX = []
        for h in range(8):
            fX.append(lambda e, h=h: e.matmul(Q[7][0:64, hs(h)], AR[:, h, c, 0, :], Hbf[:, h, :], start=True, stop=False))
            fX.append(lambda e, h=h: e.matmul(Q[7][0:64, hs(h)], A2s[:, h, 0:64], Vt[:, c, hs(h)], start=False, stop=True))
        dp.pe(fX, reads=[B_AR, B_Hf, B_A2s, B_tm], writes=[B_Q[7]])
        dp.op(S_, lambda e: e.copy(X1s[:], Q[7][0:64, :]), reads=[B_Q[7]], writes=[B_X1s])
        yield
        dp.pe([lambda e, h=h: e.matmul(Q[7][0:64, hs(h)], Gb[:, h, :], X1s[:, hs(h)], start=True, stop=True) for h in range(8)],
              reads=[B_G, B_X1s], writes=[B_Q[7]])
        dp.op(S_, lambda e: e.copy(Us[:], Q[7][0:64, :]), reads=[B_Q[7]], writes=[B_Us])
        yield
        fY = []
        for h in range(8):
            fY.append(lambda e, h=h: e.matmul(Q[5][0:64, hs(h)], AR[:, h, c, 1, :], Hbf[:, h, :], start=True, stop=False))
            fY.append(lambda e, h=h: e.matmul(Q[5][0:64, hs(h)], A1s[:, h, 64:128], Us[:, hs(h)], start=False, stop=False))
            fY.append(lambda e, h=h: e.matmul(Q[5][0:64, hs(h)], A2s[:, h, 64:128], Vt[:, c, hs(h)], start=False, stop=True))
        dp.pe(fY, reads=[B_AR, B_Hf, B_A1s, B_A2s, B_Us, B_tm], writes=[B_Q[5]])
        yield
        fH = []
        for h in range(8):
            fH.append(lambda e, h=h: e.matmul(Q[6][0:64, hs(h)], Bt[:, c, hs(h)], Us[:, hs(h)], start=True, stop=False))
            fH.append(lambda e, h=h: e.matmul(Q[6][0:64, hs(h)], Kt[:, c, hs(h)], Vt[:, c, hs(h)], start=False, stop=True))
        dp.pe(fH, reads=[B_Us, B_tm], writes=[B_Q[6]])
        dp.op(V, lambda e: e.tensor_tensor(out=Hf[:], in0=Hf[:], in1=gC[:, :, c:c + 1].to_broadcast([64, 8, 64]), op=ALU.mult),
              reads=[B_gC], writes=[B_Hf])
        dp.op(V, lambda e: e.tensor_tensor(out=fl(Hf), in0=fl(Hf), in1=Q[6][0:64, :], op=ALU.add), reads=[B_Q[6]], writes=[B_Hf])
        yield
        y3 = h3(Q[5], 64)
        yn3 = yn[:].rearrange("p (h j) -> p h j", j=64)
        dp.op(V, lambda e: e.tensor_reduce(out=st[:, 0, :], in_=y3, axis=AX.X, op=ALU.add), reads=[B_Q[5]], writes=[B_st])
        dp.op(S_, lambda e: e.activation(out=ysq[:], in_=Q[5][0:64, :], func=AF.Square), reads=[B_Q[5]], writes=[B_ysq])
        dp.op(V, lambda e: e.tensor_reduce(out=st[:, 1, :], in_=ysq[:].rearrange("p (h j) -> p h j", j=64), axis=AX.X, op=ALU.add),
              reads=[B_ysq], writes=[B_st])
        yield
        dp.op(V, lambda e: e.tensor_scalar_mul(st[:, 0, :], st[:, 0, :], 1.0 / 64), writes=[B_st])
        dp.op(V, lambda e: e.tensor_tensor(out=st[:, 2, :], in0=st[:, 0, :], in1=st[:, 0, :], op=ALU.mult), writes=[B_st])
        dp.op(V, lambda e: e.scalar_tensor_tensor(out=st[:, 1, :], in0=st[:, 1, :], scalar=1.0 / 64, in1=st[:, 2, :],
                                                  op0=ALU.mult, op1=ALU.subtract), writes=[B_st])
        dp.op(V, lambda e: e.tensor_scalar_add(st[:, 1, :], st[:, 1, :], GN_EPS), writes=[B_st])
        dp.op(S_, lambda e: e.sqrt(st[:, 3, :], st[:, 1, :]), reads=[B_st], writes=[B_st])
        dp.op(V, lambda e: e.reciprocal(st[:, 4, :], st[:, 3, :]), reads=[B_st], writes=[B_st])
        dp.op(V, lambda e: e.tensor_tensor(out=yn3, in0=y3, in1=st[:, 0, :].unsqueeze(2).to_broadcast([64, 8, 64]), op=ALU.subtract),
              reads=[B_Q[5], B_st], writes=[B_yn])
        dp.op(V, lambda e: e.tensor_tensor(out=yn3, in0=yn3, in1=st[:, 4, :].unsqueeze(2).to_broadcast([64, 8, 64]), op=ALU.mult),
              reads=[B_st], writes=[B_yn])
        yield
        dp.pe([lambda e, h=h: e.transpose(Q[5][0:64, hs(h)], yn[:, hs(h)], identf[:]) for h in range(8)],
              reads=[B_yn, cb], writes=[B_Q[5]])
        yo = ystage[:, :, yoff:yoff + C]
        gcol = pv[:, 32:40].unsqueeze(2).to_broadcast([64, 8, 64])
        bcol = pv[:, 40:48].unsqueeze(2).to_broadcast([64, 8, 64])
        dp.op(V, lambda e: e.tensor_tensor(out=yo, in0=h3(Q[5], 64), in1=gcol, op=ALU.mult), reads=[B_Q[5], pvb], writes=[B_ystage])
        dp.op(V, lambda e: e.tensor_tensor(out=yo, in0=yo, in1=bcol, op=ALU.add), reads=[pvb], writes=[B_ystage])
        dp.op(V, lambda e: e.tensor_tensor(out=yo, in0=yo, in1=bonus[:, :, c * C:(c + 1) * C], op=ALU.add),
              reads=[B_bonus], writes=[B_ystage])

        yield

    def stageA(k):
        pb = k % 2
        yield from prep(k * C, pb)
        tm_chunk(0, pb)
        yield
        yield from pre(0, pb)

    def stageB(k):
        if k % (seq // C) == 0:
            dp.op(V, lambda e: e.memset(Hf[:], 0.0), writes=[B_Hf])
        yield from seqg(0, k % 2, k)
        if k % 4 == 3:
            t0 = (k - 3) * C
            yb = (k // 4) % 2
            dp.dma("sync", lambda e, t0=t0, yb=yb: e.dma_start(out=yT[:, t0:t0 + YW].rearrange("(h n) s -> n h s", n=64), in_=ystage_[yb][:]),
                   B_ystage_[yb], reads=[B_ystage_[yb]])

    def drain(*gens):
        gens = list(gens)
        while gens:
            for g_ in list(gens):
                try:
                    next(g_)
                except StopIteration:
                    gens.remove(g_)

    nchunks = ntok // C
    for k in range(nchunks):
        if k == 0:
            drain(stageA(0))
        else:
            drain(stageA(k), stageB(k - 1))
    drain(stageB(nchunks - 1))
    for yb in range(2):
        P.wait("sync", B_ystage_[yb].dma_sem, B_ystage_[yb].dma_sem.n)
    P.run()
    return nc


def build_phase_d2():
    nc = bass.Bass("TRN2", target_bir_lowering=False)
    xT = _dram(nc, "xT", [D, NTOK], F32, "ExternalInput")
    xprev_d = _dram(nc, "xprev", [128, KC], F32, "ExternalInput")
    modv_d = _dram(nc, "modv", [128, 16 * KC + 1], F32, "ExternalInput")
    Wd = {n: _dram(nc, n, sh, F32, "ExternalInput") for n, sh in (
        ("w_r", [D, D]), ("w_k", [D, D]), ("w_v", [D, D]), ("w1", [D, 128]), ("w2", [128, D]),
        ("a1", [D, 128]), ("a2", [128, D]), ("g1", [D, 512]), ("g2", [512, D]))}
    outs = {n: _dram(nc, n, [D, NTOK], dt, "ExternalOutput") for n, dt in (
        ("rT", BF16), ("kT", BF16), ("vT", BF16), ("gT", BF16), ("wT", F32), ("aT", F32))}
    P = Prog(nc)
    P.serial = {"vector", "scalar", "gpsimd"}
    T = Tok(nc, P, hid_chunks=0, rings=False)
    act2 = P.sbuf("act2", [128, KC, TT], BF16)
    acts = [T.act, act2]
    U = P.sbuf("U", [128, KC, TT + 1], F32)
    h1 = P.sbuf("h1", [128, 4, TT], BF16)
    lastcol = P.sbuf("lastcol", [128, KC], F32)
    xs = Slots(P, "dx", 2, [128, TT], F32)
    tmps = Slots(P, "dtmp", 2, [128, TT], F32)
    ostf = Slots(P, "dof", 2, [128, TT], F32)
    ostb = Slots(P, "dob", 2, [128, TT], BF16)
    mv, tmod = load_mod(P, modv_d, 16 * KC + 1)
    col = lambda i: mv[:, i * KC:(i + 1) * KC]
    flag = mv[:, 16 * KC:16 * KC + 1]
    onepsc = P.sbuf("onepsc", [128, KC], F32)
    xp = P.sbuf("xp", [128, KC], F32)
    s_xp = P.sem("s_xp")
    v = P.op("sync", lambda e: e.dma_start(out=xp[:], in_=xprev_d), inc=s_xp, dma=True)
    P.op("vector", lambda e: e.tensor_scalar_add(onepsc[:], col(1), 1.0), waits=[tmod, (s_xp, v)])
    P.op("vector", lambda e: e.tensor_scalar(out=mv[:, 8 * KC:14 * KC], in0=mv[:, 2 * KC:8 * KC], scalar1=-1.0, scalar2=1.0,
                                             op0=ALU.mult, op1=ALU.add))
    P.op("vector", lambda e: e.tensor_tensor(out=xp[:], in0=xp[:], in1=onepsc[:], op=ALU.mult))
    P.op("vector", lambda e: e.tensor_tensor(out=xp[:], in0=xp[:], in1=col(0), op=ALU.add))
    P.op("vector", lambda e: e.tensor_scalar_mul(lastcol[:], xp[:], flag))
    tconst = P.tok("vector")
    out_toks = []
    act_rel = [[], []]
    h1_rel = []
    U_rel = []
    ai = 0
    for t in range(NT):
        P.op("vector", lambda e: e.tensor_copy(U[:, :, 0], lastcol[:]), waits=[tconst] + U_rel)
        for c in range(KC):
            s, xt, rel = xs.get()
            v = P.op("sync", lambda e, xt=xt, c=c, t=t: e.dma_start(out=xt[:], in_=xT[c * 128:(c + 1) * 128, t * TT:(t + 1) * TT]),
                     waits=rel, inc=xs.sem_in[s], dma=True)
            P.op("scalar", lambda e, xt=xt, c=c: e.activation(out=U[:, c, 1:TT + 1], in_=xt[:], func=AF.Identity,
                                                              bias=col(0)[:, c:c + 1], scale=onepsc[:, c:c + 1]),
                 waits=[(xs.sem_in[s], v), tconst] + (U_rel if c == 0 else []))
            xs.release(s, [P.tok("scalar")])
        tU = P.tok("scalar")
        P.op("vector", lambda e: e.tensor_copy(lastcol[:], U[:, :, TT]), waits=[tU])
        tUv = P.tok("vector")
        U_readers = []

        def build_act(j):
            nonlocal ai
            a = acts[ai % 2]
            rel = act_rel[ai % 2]
            for c in range(KC):
                s, tt, trel = tmps.get()
                P.op("scalar", lambda e, tt=tt, c=c, j=j: e.activation(out=tt[:], in_=U[:, c, 1:TT + 1], func=AF.Copy,
                                                                       scale=col(8 + j)[:, c:c + 1]), waits=[tU, tUv] + trel)
                tk = P.tok("scalar")
                P.op("vector", lambda e, tt=tt, c=c, j=j, a=a: e.scalar_tensor_tensor(out=a[:, c, :], in0=U[:, c, 0:TT], scalar=col(2 + j)[:, c:c + 1],
                                                                                      in1=tt[:], op0=ALU.mult, op1=ALU.add),
                     waits=[tk, tU, tUv] + (list(rel) if c == 0 else []))
                tmps.release(s, [P.tok("vector")])
            U_readers.append(P.tok("vector"))
            U_readers.append(P.tok("scalar"))
            idx = ai % 2
            ai += 1
            return a, idx, P.tok("vector")

        def ep_store(dst, bf, func=None, bias=None, t=t):
            def ep(bi, oc, bank, fw):
                ch = bi * 4 + oc
                ps = T.ps[bank]
                pool = ostb if bf else ostf
                s, st, rel = pool.get()
                if func is None and bias is None:
                    eng = T.ev_eng()
                    if eng == "vector":
                        P.op(eng, lambda e: e.tensor_copy(st[:], ps[:, 0:TT]), waits=[fw] + rel, inc=T.pbank.freed[bank])
                    else:
                        P.op(eng, lambda e: e.copy(st[:], ps[:, 0:TT]), waits=[fw] + rel, inc=T.pbank.freed[bank])
                else:
                    P.op("scalar", lambda e: e.activation(out=st[:], in_=ps[:, 0:TT], func=func or AF.Identity,
                                                          bias=(bias[:, ch:ch + 1] if bias is not None else 0.0)),
                         waits=[fw] + rel, inc=T.pbank.freed[bank])
                tk = (T.pbank.freed[bank], T.pbank.freed[bank].n)
                v = P.op("sync", lambda e: e.dma_start(out=dst[ch * 128:(ch + 1) * 128, t * TT:(t + 1) * TT], in_=st[:]),
                         waits=[tk], inc=pool.sem_out[s], dma=True)
                tko = (pool.sem_out[s], v)
                pool.release(s, [tko])
                out_toks.append(tko)
            return ep

        def ep_h1(func, nch):
            def ep(bi, oc, bank, fw):
                ps = T.ps[bank]
                P.op("scalar", lambda e: e.activation(out=h1[:, oc, :], in_=ps[:, 0:TT], func=func),
                     waits=[fw] + (list(h1_rel) if oc == 0 else []), inc=T.pbank.freed[bank])
            return ep

        full = [[(c0, 512)] for c0 in range(0, D, 512)]
        plan = [(0, "w_r", "rT", None), (1, "w1", None, "w"), (2, "w_k", "kT", None), (3, "w_v", "vT", None),
                (4, "a1", None, "a"), (5, "g1", None, "g")]
        for (j, wname, oname, lora) in plan:
            a, idx, ta = build_act(j)
            if lora is None:
                lf = T.gemm(Wd[wname], D, full, ep_store(outs[oname], True), act=a, act_waits=[ta])
                act_rel[idx] = [lf]
            elif lora == "w":
                lf = T.gemm(Wd["w1"], D, [[(0, 128)]], ep_h1(AF.Tanh, 1), act=a, act_waits=[ta])
                act_rel[idx] = [lf]
                th = P.tok("scalar")
                lf2 = T.gemm(Wd["w2"], 128, full, ep_store(outs["wT"], False, AF.Identity, col(14)), act=h1, act_waits=[th])
                h1_rel[:] = [lf2]
            elif lora == "a":
                lf = T.gemm(Wd["a1"], D, [[(0, 128)]], ep_h1(AF.Copy, 1), act=a, act_waits=[ta])
                act_rel[idx] = [lf]
                th = P.tok("scalar")
                lf2 = T.gemm(Wd["a2"], 128, full, ep_store(outs["aT"], False, AF.Sigmoid, col(15)), act=h1, act_waits=[th])
                h1_rel[:] = [lf2]
            else:
                lf = T.gemm(Wd["g1"], D, [[(0, 512)]], ep_h1(AF.Sigmoid, 4), act=a, act_waits=[ta])
                act_rel[idx] = [lf]
                th = P.tok("scalar")
                lf2 = T.gemm(Wd["g2"], 512, full, ep_store(outs["gT"], True), act=h1, act_waits=[th])
                h1_rel[:] = [lf2]
        U_rel = list(U_readers[-4:])
    for tk in out_toks[-8:]:
        P.wait("sync", *tk)
    for pool in (ostf, ostb):
        for s in range(pool.n):
            P.wait("sync", pool.sem_out[s], pool.sem_out[s].n)
    P.run()
    return nc


A_NCH = 24


def build_phase_a():
    nc = bass.Bass("TRN2", target_bir_lowering=False)
    cT_d = _dram(nc, "cT", [128, KC, 2], F32, "ExternalInput")
    W_d = _dram(nc, "ada_w", [2, D, A_NCH * 128], F32, "ExternalInput")
    b_d = _dram(nc, "ada_b", [128, 2 * A_NCH], F32, "ExternalInput")
    o_d = _dram(nc, "modT", [128, 2 * A_NCH, 2], F32, "ExternalOutput")
    P = Prog(nc)
    P.serial = {"vector", "scalar", "gpsimd"}
    dp = Dep(P)
    cT = P.sbuf("cT_s", [128, KC, 2], F32)
    sg = P.sbuf("sg_s", [128, KC, 2], F32)
    bs = P.sbuf("b_s", [128, 2 * A_NCH], F32)
    osb = P.sbuf("o_s", [128, 2 * A_NCH, 2], F32)
    wb = [P.sbuf(f"aw{i}", [128, KC, 256], F32) for i in range(2)]
    Bc = dp.buf("c", dma=True); Bb = dp.buf("b", dma=True); Bo = dp.buf("o", dma=True)
    Bw = [dp.buf("w0", dma=True), dp.buf("w1", dma=True)]
    ps = [P.psum(f"aps{i}", [128, 512], F32) for i in range(2)]
    Bp = [dp.buf("p0"), dp.buf("p1")]
    dp.dma("sync", lambda e: e.dma_start(out=cT[:], in_=cT_d), Bc, writes=[Bc])
    dp.dma("sync", lambda e: e.dma_start(out=bs[:], in_=b_d), Bb, writes=[Bb])
    dp.op("scalar", lambda e: e.activation(out=sg[:], in_=cT[:], func=AF.Sigmoid), reads=[Bc], writes=[Bc])
    dp.op("vector", lambda e: e.tensor_tensor(out=cT[:], in0=cT[:], in1=sg[:], op=ALU.mult), writes=[Bc])
    nblk = 2 * A_NCH // 2
    for blk in range(nblk):
        l = blk // (A_NCH // 2)
        c0 = (blk % (A_NCH // 2)) * 256
        i = blk % 2
        dp.dma("sync" if i == 0 else "gpsimd", lambda e, l=l, c0=c0, i=i: e.dma_start(
            out=wb[i][:], in_=W_d[l, :, c0:c0 + 256].rearrange("(k p) n -> p k n", p=128)), Bw[i], writes=[Bw[i]])
        for oc in range(2):
            ch = blk * 2 + oc
            dp.pe([lambda e, kk=kk, oc=oc, i=i: e.matmul(ps[oc][:, 0:2], wb[i][:, kk, oc * 128:(oc + 1) * 128], cT[:, kk, :],
                                                         start=(kk == 0), stop=(kk == KC - 1)) for kk in range(KC)],
                  reads=[Bw[i], Bc], writes=[Bp[oc]])
            dp.op("vector", lambda e, ch=ch, oc=oc: e.tensor_tensor(out=osb[:, ch, :], in0=ps[oc][:, 0:2],
                                                                    in1=bs[:, ch:ch + 1].to_broadcast([128, 2]), op=ALU.add),
                  reads=[Bp[oc], Bb], writes=[Bo])
    dp.dma("sync", lambda e: e.dma_start(out=o_d, in_=osb[:]), Bo, reads=[Bo])
    P.wait("sync", Bo.dma_sem, Bo.dma_sem.n)
    P.run()
    return nc


_NC = {}
_DBG = None


def _get(name, fn):
    if name not in _NC:
        _NC[name] = fn()
    return _NC[name]


def _fm(v):
    return np.ascontiguousarray(np.asarray(v, np.float32).reshape(-1, 128).T)


def _run(nc, in_maps):
    res = run_bass_kernel_spmd(nc, in_maps, core_ids=list(range(len(in_maps))))
    return res.results


def kernel(x, c, ada_w, ada_b, mix_ln_g, mix_ln_b, ffn_ln_g, ffn_ln_b, ffn_w_in, ffn_w_out,
           ml_w_in, ml_b_i, ml_b_f, ml_norm_g, ml_w_out,
           rw_mu, rw_w_r, rw_w_k, rw_w_v, rw_w0, rw_w1, rw_w2, rw_a0, rw_a1, rw_a2, rw_g1, rw_g2,
           rw_k_k, rw_k_a, rw_r_k, rw_lnx_g, rw_lnx_b, rw_w_o):
    f32 = np.float32
    x = np.asarray(x, f32)
    xf = x.reshape(8192, D)
    ca = np.ascontiguousarray
    cT = ca(np.asarray(c, f32).T.reshape(KC, 128, 2).transpose(1, 0, 2))
    ims = []
    for j in range(NCORE):
        sl = slice(j * 3072, (j + 1) * 3072)
        bj = np.concatenate([np.asarray(ada_b[l, sl], f32).reshape(A_NCH, 128).T for l in range(2)], axis=1)
        ims.append({"cT": cT, "ada_w": ca(np.asarray(ada_w[:, :, sl], f32)), "ada_b": ca(bj)})
    ra = _run(_get("a", build_phase_a), ims)
    mod = np.zeros((2, 2, 6 * D), f32)
    for j in range(NCORE):
        o = ra[j]["modT"]
        for l in range(2):
            mod[l, :, j * 3072:(j + 1) * 3072] = o[:, l * A_NCH:(l + 1) * A_NCH, :].transpose(2, 1, 0).reshape(2, 3072)
    del ims, ra
    if _DBG is not None:
        _DBG['mod'] = mod.copy()
    mparts = lambda l, b: [mod[l, b, i * D:(i + 1) * D] for i in range(6)]
    xT = [ca(xf[i * NTOK:(i + 1) * NTOK].T) for i in range(NCORE)]
    w_in0 = ca(np.asarray(ml_w_in[0], f32))
    ims = []
    for i in range(NCORE):
        sh_m, sc_m = mparts(0, i // 4)[0:2]
        ims.append({"xT": xT[i], "w_in": w_in0, "modv": ca(np.concatenate([_fm(sh_m), _fm(sc_m)], axis=1))})
    rb = _run(_get("b", build_phase_b), ims)
    qkvT = np.concatenate([rb[i]["qkvT"] for i in range(NCORE)], axis=1)
    soT = np.concatenate([rb[i]["soT"] for i in range(NCORE)], axis=1)
    gT = np.concatenate([rb[i]["gT"] for i in range(NCORE)], axis=1)
    del ims, rb, w_in0
    ims = []
    for j in range(NCORE):
        q = qkvT[j * 256:(j + 1) * 256].reshape(256, 2, ML_S).transpose(1, 0, 2)
        k = qkvT[2048 + j * 256:2048 + (j + 1) * 256].reshape(256, 2, ML_S).transpose(1, 0, 2)
        v = qkvT[4096 + j * 512:4096 + (j + 1) * 512].reshape(512, 2, ML_S).transpose(1, 2, 0)
        so = soT[j * 512:(j + 1) * 512].reshape(512, 2, ML_S).transpose(1, 2, 0)
        ims.append({"qT": ca(q), "kT": ca(k), "ktm": ca(k.transpose(0, 2, 1)), "vtm": ca(v), "sotm": ca(so),
                    "gi": ca(gT[j].reshape(2, ML_S)), "gf": ca(gT[8 + j].reshape(2, ML_S)),
                    "gb": np.array([[ml_b_i[0, j], ml_b_f[0, j]]], f32),
                    "ng": ca(np.tile(np.asarray(ml_norm_g[0, j * 512:(j + 1) * 512], f32)[None, :], (128, 1)))})
    rc = _run(_get("c", build_phase_c), ims)
    hT = np.concatenate([rc[j]["hT"].transpose(1, 0, 2).reshape(512, 8192) for j in range(NCORE)], axis=0)
    if _DBG is not None:
        _DBG['qkvT'] = qkvT; _DBG['soT'] = soT; _DBG['gT'] = gT; _DBG['hT'] = hT
    del ims, rc, qkvT, soT, gT
    w_o0 = ca(np.asarray(ml_w_out[0], f32)); fwi = ca(np.asarray(ffn_w_in[0], f32)); fwo = ca(np.asarray(ffn_w_out[0], f32))
    ims = []
    for i in range(NCORE):
        sh_m, sc_m, gt_m, sh_f, sc_f, gt_f = mparts(0, i // 4)
        cols = [gt_m, mix_ln_g[0], mix_ln_b[0], sh_f, sc_f, gt_f, ffn_ln_g[0], ffn_ln_b[0]]
        ims.append({"xT": xT[i], "hT": ca(hT[:, i * NTOK:(i + 1) * NTOK]), "w_o": w_o0, "w_in": fwi, "w_out": fwo,
                    "modv": ca(np.concatenate([_fm(v) for v in cols], axis=1))})
    rd = _run(_get("d1", lambda: build_phase_d1("d1")), ims)
    x2T = [rd[i]["outT"] for i in range(NCORE)]
    if _DBG is not None:
        _DBG['x2T'] = [a.copy() for a in x2T]
    del ims, rd, hT, w_o0, fwi, fwo, xT
    g1p = np.zeros((D, 512), f32); g1p[:, :480] = rw_g1[0]
    g2p = np.zeros((512, D), f32); g2p[:480] = rw_g2[0]
    Wd2 = {"w_r": ca(np.asarray(rw_w_r[0], f32)), "w_k": ca(np.asarray(rw_w_k[0], f32)), "w_v": ca(np.asarray(rw_w_v[0], f32)),
           "w1": ca(np.asarray(rw_w1[0], f32)), "w2": ca(np.asarray(rw_w2[0], f32)), "a1": ca(np.asarray(rw_a1[0], f32)),
           "a2": ca(np.asarray(rw_a2[0], f32)), "g1": g1p, "g2": g2p}
    ims = []
    for i in range(NCORE):
        sh_m, sc_m = mparts(1, i // 4)[0:2]
        first = (i % 4 == 0)
        xprev = np.zeros((128, KC), f32) if first else ca(x2T[i - 1][:, NTOK - 1].reshape(KC, 128).T)
        cols = [_fm(sh_m), _fm(sc_m)] + [_fm(rw_mu[0, jj]) for jj in range(6)] + [np.zeros((128, 6 * KC), f32),
                                                                                  _fm(rw_w0[0]), _fm(rw_a0[0]),
                                                                                  np.full((128, 1), 0.0 if first else 1.0, f32)]
        im = {"xT": x2T[i], "xprev": xprev, "modv": ca(np.concatenate(cols, axis=1))}
        im.update(Wd2)
        ims.append(im)
    r2 = _run(_get("d2", build_phase_d2), ims)
    cat = lambda n: np.concatenate([r2[i][n] for i in range(NCORE)], axis=1)
    rT, kT, vT, wT, aT = cat("rT"), cat("kT"), cat("vT"), cat("wT"), cat("aT")
    gTs = [r2[i]["gT"] for i in range(NCORE)]
    if _DBG is not None:
        _DBG.update(rT=rT, kT=kT, vT=vT, wT=wT, aT=aT, gTs=gTs)
    del ims, r2, Wd2
    ims = []
    for j in range(NCORE):
        sl = slice(j * 512, (j + 1) * 512)
        hv = lambda v: np.asarray(v, f32).reshape(-1)[sl].reshape(8, 64).T
        pv = np.concatenate([hv(rw_k_k[0]), hv(rw_k_a[0]), np.zeros((64, 8), f32), hv(rw_r_k[0]), hv(rw_lnx_g[0]), hv(rw_lnx_b[0])], axis=1)
        ims.append({"rT": ca(rT[sl]), "kT": ca(kT[sl]), "vT": ca(vT[sl]), "wT": ca(wT[sl]), "aT": ca(aT[sl]), "pv": ca(pv)})
    re_ = _run(_get("e", build_phase_e), ims)
    yT = np.concatenate([re_[j]["yT"] for j in range(NCORE)], axis=0)
    if _DBG is not None:
        _DBG['yT'] = yT
    del ims, re_, rT, kT, vT, wT, aT
    w_o1 = ca(np.asarray(rw_w_o[0], f32)); fwi = ca(np.asarray(ffn_w_in[1], f32)); fwo = ca(np.asarray(ffn_w_out[1], f32))
    ims = []
    for i in range(NCORE):
        sh_m, sc_m, gt_m, sh_f, sc_f, gt_f = mparts(1, i // 4)
        cols = [gt_m, mix_ln_g[1], mix_ln_b[1], sh_f, sc_f, gt_f, ffn_ln_g[1], ffn_ln_b[1]]
        ims.append({"xT": x2T[i], "yT": ca(yT[:, i * NTOK:(i + 1) * NTOK]), "gT": gTs[i], "w_o": w_o1, "w_in": fwi, "w_out": fwo,
                    "modv": ca(np.concatenate([_fm(v) for v in cols], axis=1))})
    rf = _run(_get("f", lambda: build_phase_d1("f")), ims)
    out = np.concatenate([rf[i]["outT"].T for i in range(NCORE)], axis=0).reshape(2, ML_S, D)
    return np.ascontiguousarray(out.astype(f32))
```

```python
import numpy as np
import ml_dtypes
from contextlib import ExitStack
import concourse.bass as bass
import concourse.mybir as mybir
from concourse.bass_utils import run_bass_kernel_spmd

F32 = mybir.dt.float32
BF16 = mybir.dt.bfloat16
AF = mybir.ActivationFunctionType
ALU = mybir.AluOpType
AX = mybir.AxisListType
NPBF = ml_dtypes.bfloat16

D = 4096
KC = D // 128
NCORE = 8
NTOK = 1024
TT = 512
NT = NTOK // TT
FF = 11008
ML_IN = 12304
ALPHA = 4 ** 0.25
LN_EPS = 1e-5
ENGS = ("tensor", "vector", "scalar", "gpsimd", "sync")


class Sem:
    def __init__(self, h, name):
        self.h = h
        self.name = name
        self.n = 0


class Prog:
    def __init__(self, nc):
        self.nc = nc
        self.q = {e: [] for e in ENGS}
        self.waited = {e: {} for e in ENGS}
        self.stack = ExitStack()
        self.serial = set()
        self.last = {}
        self.chain_sem = {}

    def sem(self, name):
        return Sem(self.stack.enter_context(self.nc.semaphore(name)), name)

    def sbuf(self, name, shape, dt):
        return self.stack.enter_context(self.nc.sbuf_tensor(name, shape, dt))

    def psum(self, name, shape, dt):
        return self.stack.enter_context(self.nc.psum_tensor(name, shape, dt))

    def wait(self, eng, sem, val):
        if val <= 0:
            return
        w = self.waited[eng]
        if w.get(sem.name, 0) >= val:
            return
        w[sem.name] = val
        self.q[eng].append(lambda e, s=sem.h, v=val: e.wait_ge(s, v))

    def op(self, eng, fn, waits=(), inc=None, dma=False, selfwait=True):
        for s, v in waits:
            self.wait(eng, s, v)
        if eng in self.serial and not dma:
            if eng in self.last and selfwait:
                self.wait(eng, *self.last[eng])
            if inc is None:
                if eng not in self.chain_sem:
                    self.chain_sem[eng] = self.sem("chain_" + eng)
                inc = self.chain_sem[eng]
            self.last[eng] = (inc, inc.n + 1)
        if inc is None:
            self.q[eng].append(lambda e, f=fn: f(e))
            return None
        k = 16 if dma else 1
        inc.n += k
        self.q[eng].append(lambda e, f=fn, sh=inc.h, kk=k: f(e).then_inc(sh, kk))
        return inc.n

    def tok(self, eng):
        return self.last[eng]

    def run(self):
        with self.nc.Block() as block:
            for ename in ENGS:
                lst = self.q[ename]

                def body(e, lst=lst):
                    for f in lst:
                        f(e)

                getattr(block, ename)(body)
        self.stack.close()


class Ring:
    def __init__(self, P, name, n, shape=None, dt=None, tiles=None):
        self.n = n
        self.tiles = tiles if tiles is not None else [P.sbuf(f"{name}{i}", shape, dt) for i in range(n)]
        self.filled = [P.sem(f"{name}_f{i}") for i in range(n)]
        self.freed = [P.sem(f"{name}_e{i}") for i in range(n)]
        self.i = 0

    def next(self):
        s = self.i % self.n
        self.i += 1
        return s

    def free_wait(self, s):
        return (self.freed[s], self.freed[s].n)

    def fill_wait(self, s):
        return (self.filled[s], self.filled[s].n)


class Slots:
    def __init__(self, P, name, n, shape, dt):
        self.n = n
        self.tiles = [P.sbuf(f"{name}{i}", shape, dt) for i in range(n)]
        self.rel = [[] for _ in range(n)]
        self.sem_in = [P.sem(f"{name}_i{i}") for i in range(n)]
        self.sem_out = [P.sem(f"{name}_o{i}") for i in range(n)]
        self.i = 0

    def get(self):
        s = self.i % self.n
        self.i += 1
        return s, self.tiles[s], list(self.rel[s])

    def release(self, s, toks):
        self.rel[s] = [t for t in toks if t is not None]


class Tok:
    WB_ELEMS = 8192
    NBUF = 3
    KG = 16

    def __init__(self, nc, P, hid_chunks=86, rings=True, kg=None):
        self.nc, self.P = nc, P
        if kg is not None:
            self.KG = kg
            self.WB_ELEMS = kg * 512
        self.wb = [P.sbuf(f"wb{i}", [128, self.WB_ELEMS], BF16) for i in range(self.NBUF)]
        self.act = P.sbuf("act", [128, KC, TT], BF16)
        self.hid = P.sbuf("hid", [128, hid_chunks, TT], BF16) if hid_chunks else None
        self.ps = [P.psum(f"ps{i}", [128, 512], F32) for i in range(8)]
        self.pbank = Ring(P, "pb", 8, tiles=self.ps)
        self.gi = 0
        self.mi = 0
        self.s_wl = [P.sem(f"s_wl{i}") for i in range(self.NBUF)]
        self.s_wf = P.sem("s_wf")
        self.wcount = 0
        self.wrel = {}
        if rings:
            self.ostf = Ring(P, "ostf", 2, [128, TT], F32)
            self.ostb = Ring(P, "ostb", 2, [128, TT], BF16)
            self.xs = Ring(P, "xs", 3, [128, TT], F32)
        self.act_rel = []
        self.hid_rel = []
        self.evi = 0

    def gbank(self):
        b = self.gi % 6
        self.gi += 1
        return b

    def mbank(self):
        b = 6 + self.mi % 2
        self.mi += 1
        return b

    def ev_eng(self):
        self.evi += 1
        return "vector" if self.evi % 2 else "scalar"

    def load_w(self, Wv, k0, kn, segs):
        P = self.P
        idx = self.wcount
        self.wcount += 1
        b = idx % self.NBUF
        ncols = sum(n for _, n in segs)
        assert kn * ncols <= self.WB_ELEMS
        view = self.wb[b][:, 0:kn * ncols].rearrange("p (k c) -> p k c", c=ncols)
        waits = []
        if idx - self.NBUF in self.wrel:
            waits.append(self.wrel[idx - self.NBUF])
        off = 0
        val = None
        for (c0, n) in segs:
            src = Wv[k0 * 128:(k0 + kn) * 128, c0:c0 + n].rearrange("(k p) n -> p k n", p=128)
            dst = view[:, :, off:off + n]
            val = P.op("gpsimd", lambda e, dst=dst, src=src: e.dma_start(out=dst, in_=src),
                       waits=waits, inc=self.s_wl[b], dma=True)
            waits = []
            off += n
        return idx, view, (self.s_wl[b], val)

    def gemm(self, Wv, K, blocks, ep, act=None, m_last=128, act_waits=()):
        P = self.P
        act = self.act if act is None else act
        kct = K // 128
        kgs = []
        k0 = 0
        while k0 < kct:
            kn = min(self.KG, kct - k0)
            kgs.append((k0, kn))
            k0 += kn
        loads = [(bi, gi) for bi in range(len(blocks)) for gi in range(len(kgs))]
        pending = {}

        def issue(li):
            bi, gi = loads[li]
            k0, kn = kgs[gi]
            pending[li] = self.load_w(Wv, k0, kn, blocks[bi])

        issue(0)
        if len(loads) > 1:
            issue(1)
        li = 0
        last_full = None
        for bi, segs in enumerate(blocks):
            ncols = sum(n for _, n in segs)
            nch = (ncols + 127) // 128
            banks = [self.gbank() for _ in range(nch)]
            for gi, (k0, kn) in enumerate(kgs):
                if li + 2 < len(loads):
                    issue(li + 2)
                idx, view, wwait = pending.pop(li)
                li += 1
                for oc in range(nch):
                    bank = banks[oc]
                    M = min(128, ncols - oc * 128)
                    for kk in range(kn):
                        first = gi == 0 and kk == 0
                        last = gi == len(kgs) - 1 and kk == kn - 1
                        waits = []
                        if kk == 0:
                            waits.append(wwait)
                            waits.extend(act_waits)
                            if first:
                                waits.append(self.pbank.free_wait(bank))
                        fn = lambda e, bank=bank, M=M, view=view, kk=kk, oc=oc, k0=k0, first=first, last=last: e.matmul(
                            self.ps[bank][0:M, 0:TT], view[:, kk, oc * 128:oc * 128 + M], act[:, k0 + kk, :],
                            start=first, stop=last)
                        if last:
                            P.op("tensor", fn, waits=waits, inc=self.pbank.filled[bank])
                            if oc == nch - 1:
                                self.wrel[idx] = self.pbank.fill_wait(bank)
                        elif kk == kn - 1 and oc == nch - 1:
                            v = P.op("tensor", fn, waits=waits, inc=self.s_wf)
                            self.wrel[idx] = (self.s_wf, v)
                        else:
                            P.op("tensor", fn, waits=waits)
            last_full = self.pbank.fill_wait(banks[-1])
            for oc in range(nch):
                bank = banks[oc]
                ep(bi, oc, bank, self.pbank.fill_wait(bank))
        return last_full

    def gemm_multi(self, Wv, K, blocks, ep, acts, act_waits=()):
        P = self.P
        kct = K // 128
        assert kct <= self.KG
        pending = {}

        def issue(li):
            pending[li] = self.load_w(Wv, 0, kct, blocks[li])

        issue(0)
        if len(blocks) > 1:
            issue(1)
        last_full = None
        for bi, segs in enumerate(blocks):
            if bi + 2 < len(blocks):
                issue(bi + 2)
            idx, view, wwait = pending.pop(bi)
            ncols = sum(n for _, n in segs)
            nch = (ncols + 127) // 128
            for ti, act in enumerate(acts):
                banks = [self.gbank() for _ in range(nch)]
                for oc in range(nch):
                    bank = banks[oc]
                    M = min(128, ncols - oc * 128)
                    for kk in range(kct):
                        waits = []
                        if kk == 0:
                            waits = [wwait] + list(act_waits) + [self.pbank.free_wait(bank)]
                        fn = lambda e, bank=bank, M=M, view=view, kk=kk, oc=oc, act=act: e.matmul(
                            self.ps[bank][0:M, 0:TT], view[:, kk, oc * 128:oc * 128 + M], act[:, kk, :],
                            start=(kk == 0), stop=(kk == kct - 1))
                        if kk == kct - 1:
                            P.op("tensor", fn, waits=waits, inc=self.pbank.filled[bank])
                        else:
                            P.op("tensor", fn, waits=waits)
                last_full = self.pbank.fill_wait(banks[-1])
                if ti == len(acts) - 1:
                    self.wrel[idx] = last_full
                for oc in range(nch):
                    ep(bi, oc, banks[oc], self.pbank.fill_wait(banks[oc]), ti)
        return last_full


def _dram(nc, name, shape, dt, kind):
    return nc.dram_tensor(name, list(shape), dt, kind=kind).ap()


def _finish(P, out_waits):
    for s, v in out_waits:
        P.wait("sync", s, v)


class OutTracker:
    def __init__(self):
        self.sems = {}

    def add(self, sem):
        self.sems[sem.name] = sem

    def waits(self):
        return [(s, s.n) for s in self.sems.values()]


def modulate(T, xT, t, onepsc, sh, extra_waits=(), act=None):
    P = T.P
    act = T.act if act is None else act
    for c in range(KC):
        s = T.xs.next()
        xt = T.xs.tiles[s]
        P.op("sync", lambda e, xt=xt, c=c: e.dma_start(out=xt[:], in_=xT[c * 128:(c + 1) * 128, t * TT:(t + 1) * TT]),
             waits=[T.xs.free_wait(s)], inc=T.xs.filled[s], dma=True)
        waits = [T.xs.fill_wait(s)]
        if c == 0:
            waits += list(T.act_rel) + list(extra_waits)
        P.op("scalar", lambda e, xt=xt, c=c, act=act: e.activation(out=act[:, c, :], in_=xt[:], func=AF.Identity,
                                                          bias=sh[:, c:c + 1], scale=onepsc[:, c:c + 1]),
             waits=waits, inc=T.xs.freed[s])
    T.act_ready = (T.xs.freed[s], T.xs.freed[s].n)


def build_phase_b():
    nc = bass.Bass("TRN2", target_bir_lowering=False)
    xT = _dram(nc, "xT", [D, NTOK], F32, "ExternalInput")
    W = _dram(nc, "w_in", [D, ML_IN], F32, "ExternalInput")
    modv = _dram(nc, "modv", [128, 2 * KC], F32, "ExternalInput")
    qkvT = _dram(nc, "qkvT", [8192, NTOK], BF16, "ExternalOutput")
    soT = _dram(nc, "soT", [D, NTOK], F32, "ExternalOutput")
    gT = _dram(nc, "gT", [16, NTOK], F32, "ExternalOutput")
    P = Prog(nc)
    T = Tok(nc, P, hid_chunks=0, kg=32)
    act1 = P.sbuf("act1", [128, KC, TT], BF16)
    acts = [T.act, act1]
    mod_sb = P.sbuf("mod_sb", [128, 2 * KC], F32)
    onepsc = P.sbuf("onepsc", [128, KC], F32)
    s_c = P.sem("s_const")
    v = P.op("sync", lambda e: e.dma_start(out=mod_sb[:], in_=modv), inc=s_c, dma=True)
    s_c2 = P.sem("s_const2")
    v2 = P.op("vector", lambda e: e.tensor_scalar_add(onepsc[:], mod_sb[:, KC:2 * KC], 1.0), waits=[(s_c, v)], inc=s_c2)
    sh = mod_sb
    outs = OutTracker()
    for t in range(NT):
        modulate(T, xT, t, onepsc, sh, extra_waits=[(s_c2, v2)], act=acts[t])
    blocks = [[(c0, 512)] for c0 in range(0, 12288, 512)] + [[(12288, 16)]]

    def ep(bi, oc, bank, fw, t):
        col = bi * 512 + oc * 128
        ps = T.ps[bank]
        eng = T.ev_eng()
        if bi < 16:
            ring = T.ostb
            s = ring.next()
            st = ring.tiles[s]
            if bi < 4 or bi >= 8:
                if eng == "vector":
                    fn = lambda e: e.tensor_copy(st[:], ps[:, 0:TT])
                else:
                    fn = lambda e: e.copy(st[:], ps[:, 0:TT])
            else:
                if eng == "vector":
                    fn = lambda e: e.tensor_scalar_mul(st[:], ps[:, 0:TT], 0.0625)
                else:
                    fn = lambda e: e.mul(st[:], ps[:, 0:TT], 0.0625)
            P.op(eng, fn, waits=[fw, ring.free_wait(s)], inc=T.pbank.freed[bank])
            P.op("sync", lambda e: e.dma_start(out=qkvT[col:col + 128, t * TT:(t + 1) * TT], in_=st[:]),
                 waits=[(T.pbank.freed[bank], T.pbank.freed[bank].n)], inc=ring.freed[s], dma=True)
            outs.add(ring.freed[s])
        elif bi < 24:
            ring = T.ostf
            s = ring.next()
            st = ring.tiles[s]
            P.op("scalar", lambda e: e.activation(out=st[:], in_=ps[:, 0:TT], func=AF.Sigmoid),
                 waits=[fw, ring.free_wait(s)], inc=T.pbank.freed[bank])
            c2 = col - 8192
            P.op("sync", lambda e: e.dma_start(out=soT[c2:c2 + 128, t * TT:(t + 1) * TT], in_=st[:]),
                 waits=[(T.pbank.freed[bank], T.pbank.freed[bank].n)], inc=ring.freed[s], dma=True)
            outs.add(ring.freed[s])
        else:
            ring = T.ostf
            s = ring.next()
            st = ring.tiles[s]
            P.op("vector", lambda e: e.tensor_copy(st[0:16, :], ps[0:16, 0:TT]),
                 waits=[fw, ring.free_wait(s)], inc=T.pbank.freed[bank])
            P.op("sync", lambda e: e.dma_start(out=gT[:, t * TT:(t + 1) * TT], in_=st[0:16, :]),
                 waits=[(T.pbank.freed[bank], T.pbank.freed[bank].n)], inc=ring.freed[s], dma=True)
            outs.add(ring.freed[s])

    T.gemm_multi(W, D, blocks, ep, acts, act_waits=[T.act_ready])
    _finish(P, outs.waits())
    P.run()
    return nc


ML_S = 4096
ML_NCH = ML_S // 128
ML_EPS = 1e-6


def build_phase_c(debug=False):
    nc = bass.Bass("TRN2", target_bir_lowering=False)
    if debug:
        dbg1 = _dram(nc, "dbg1", [128, 96], F32, "ExternalOutput")
        dbg2 = _dram(nc, "dbg2", [128, 512], F32, "ExternalOutput")
        dbg3 = _dram(nc, "dbg3", [128, 512], F32, "ExternalOutput")
        dbg4 = _dram(nc, "dbg4", [128, 128], BF16, "ExternalOutput")
        dbg5 = _dram(nc, "dbg5", [128, 8], F32, "ExternalOutput")
        dbg6 = _dram(nc, "dbg6", [1, ML_S], F32, "ExternalOutput")
    qT = _dram(nc, "qT", [2, 256, ML_S], BF16, "ExternalInput")
    kT = _dram(nc, "kT", [2, 256, ML_S], BF16, "ExternalInput")
    ktm = _dram(nc, "ktm", [2, ML_S, 256], BF16, "ExternalInput")
    vtm = _dram(nc, "vtm", [2, ML_S, 512], BF16, "ExternalInput")
    sotm = _dram(nc, "sotm", [2, ML_S, 512], F32, "ExternalInput")
    gi_d = _dram(nc, "gi", [2, ML_S], F32, "ExternalInput")
    gf_d = _dram(nc, "gf", [2, ML_S], F32, "ExternalInput")
    gb_d = _dram(nc, "gb", [1, 2], F32, "ExternalInput")
    ng_d = _dram(nc, "ng", [128, 512], F32, "ExternalInput")
    hT = _dram(nc, "hT", [2, 512, ML_S], BF16, "ExternalOutput")
    P = Prog(nc)
    P.serial = {"vector", "scalar", "gpsimd"}
    qs = P.sbuf("qs", [128, 2, ML_S], BF16)
    ks = P.sbuf("ks", [128, 2, ML_S], BF16)
    ktm_s = P.sbuf("ktm_s", [128, ML_NCH, 256], BF16)
    va = P.sbuf("va", [128, ML_NCH, 514], BF16)
    hts_r = [P.sbuf(f"hts{i}", [128, 4, 1024], BF16) for i in range(2)]
    ng = P.sbuf("ng_s", [128, 512], F32)
    gb = P.sbuf("gb_s", [1, 2], F32)
    mask = P.sbuf("mask", [128, 128], F32)
    ident = P.sbuf("ident", [128, 128], F32)
    ones_row = P.sbuf("ones_row", [1, 128], F32)
    one11 = P.sbuf("one11", [1, 1], F32)
    g_in = [P.sbuf(f"grow{i}", [1, ML_S], F32) for i in range(2)]
    t2 = g_in[1]
    cF = P.sbuf("g_cF", [1, ML_S], F32)
    bv = g_in[0]
    Mr = P.sbuf("g_M", [1, ML_S], F32)
    wr = bv
    fr = cF
    ones_bc = one11[0:1, 0:1].to_broadcast([1, ML_S])
    rcr = P.sbuf("g_rc", [1, ML_NCH], F32)
    wcol = P.sbuf("wcol", [128, ML_NCH], F32)
    fcol = P.sbuf("fcol", [128, ML_NCH], F32)
    rcb = P.sbuf("rcb", [128, ML_NCH], F32)
    Cs = P.sbuf("Cs", [128, 2, 514], F32)
    Cb = P.sbuf("Cb", [128, 2, 514], BF16)
    PT = P.sbuf("PT", [128, 128], BF16)
    kw = P.sbuf("kw", [128, 256], BF16)
    junk = P.sbuf("junk", [128, 512], F32)
    hn = P.sbuf("hn", [128, 512], F32)
    hg = P.sbuf("hg", [128, 512], F32)
    sm = P.sbuf("sm", [128, 8], F32)
    so_ring = Ring(P, "so", 3, [128, 512], F32)
    psA = P.psum("psA", [128, 512], F32)
    psB = P.psum("psB", [128, 512], F32)
    psC = P.psum("psC", [128, 512], F32)
    psD = [P.psum(f"psD{i}", [128, 512], F32) for i in range(2)]
    psT = P.psum("psT", [128, 4, 128], F32)
    psG = P.psum("psG", [128, 512], F32)

    S = {n: P.sem("m_" + n) for n in ("ld", "cst", "g", "gp", "gc", "st", "pt", "rs", "cb", "nd", "kw", "dc", "cs",
                                      "ss", "hn", "hg", "tr", "cp", "out", "sq1", "sq2")}
    P.op("gpsimd", lambda e: e.memset(mask[:], 1.0))
    P.op("gpsimd", lambda e: e.affine_select(out=mask[:], in_=mask[:], pattern=[[1, 128]], compare_op=ALU.is_ge,
                                             fill=0.0, base=0, channel_multiplier=-1))
    P.op("gpsimd", lambda e: e.memset(ident[:], 1.0))
    P.op("gpsimd", lambda e: e.affine_select(out=ident[:], in_=ident[:], pattern=[[1, 128]], compare_op=ALU.is_equal,
                                             fill=0.0, base=0, channel_multiplier=-1))
    P.op("gpsimd", lambda e: e.memset(ones_row[:], 1.0))
    P.op("gpsimd", lambda e: e.memset(one11[:], 1.0))
    P.op("gpsimd", lambda e: e.memset(va[:, :, 512:514], 1.0), inc=S["cst"])
    v_cst = S["cst"].n
    P.op("sync", lambda e: e.dma_start(out=ng[:], in_=ng_d), inc=S["ld"], dma=True)
    P.op("sync", lambda e: e.dma_start(out=gb[:], in_=gb_d), inc=S["ld"], dma=True)
    P.op("vector", lambda e: e.tensor_scalar_mul(gb[:, 0:1], gb[:, 0:1], 1.0 / 15.0), waits=[(S["ld"], S["ld"].n)])
    P.op("vector", lambda e: e.tensor_scalar_mul(gb[:, 1:2], gb[:, 1:2], -1.0))
    t_gb = P.tok("vector")

    n = 0
    for b in range(2):
        wprev = [(S["cp"], S["cp"].n), (S["cs"], S["cs"].n), (S["out"], S["out"].n)] if b > 0 else []
        P.op("sync", lambda e, b=b: e.dma_start(out=qs[:], in_=qT[b].rearrange("(c p) s -> p c s", p=128)),
             waits=wprev, inc=S["ld"], dma=True)
        P.op("sync", lambda e, b=b: e.dma_start(out=ks[:], in_=kT[b].rearrange("(c p) s -> p c s", p=128)),
             inc=S["ld"], dma=True)
        P.op("sync", lambda e, b=b: e.dma_start(out=ktm_s[:], in_=ktm[b].rearrange("(c p) d -> p c d", p=128)),
             inc=S["ld"], dma=True)
        P.op("sync", lambda e, b=b: e.dma_start(out=va[:, :, 0:512], in_=vtm[b].rearrange("(c p) d -> p c d", p=128)),
             inc=S["ld"], dma=True)
        P.op("sync", lambda e, b=b: e.dma_start(out=g_in[0][:], in_=gi_d[b:b + 1, :]), inc=S["ld"], dma=True)
        P.op("sync", lambda e, b=b: e.dma_start(out=g_in[1][:], in_=gf_d[b:b + 1, :]), inc=S["ld"], dma=True)
        v_ld = S["ld"].n
        gi_t, gf_t = g_in
        P.op("scalar", lambda e: e.activation(out=t2[:], in_=gf_t[:], func=AF.Exp, bias=gb[:, 1:2], scale=-1.0),
             waits=[(S["ld"], v_ld), (S["cs"], S["cs"].n), (S["hn"], S["hn"].n), t_gb])
        P.op("scalar", lambda e: e.activation(out=t2[:], in_=t2[:], func=AF.Ln, bias=1.0, scale=1.0))
        P.op("scalar", lambda e: e.activation(out=gi_t[:], in_=gi_t[:], func=AF.Tanh, bias=gb[:, 0:1], scale=1.0 / 15.0),
             inc=S["g"])
        vg = S["g"].n
        P.op("vector", lambda e: e.tensor_tensor_scan(out=cF[:], data0=ones_bc, data1=t2[:], initial=0.0,
                                                      op0=ALU.mult, op1=ALU.add), waits=[(S["g"], vg), (S["cst"], v_cst)])
        P.op("vector", lambda e: e.scalar_tensor_tensor(out=bv[:], in0=gi_t[:], scalar=15.0, in1=cF[:],
                                                        op0=ALU.mult, op1=ALU.add))
        P.op("vector", lambda e: e.tensor_tensor_scan(out=Mr[:], data0=ones_bc, data1=bv[:], initial=0.0,
                                                      op0=ALU.mult, op1=ALU.max))
        Mv = Mr[:].rearrange("p (c j) -> p c j", j=128)
        Mend = Mv[:, :, 127:128]
        P.op("vector", lambda e: e.tensor_tensor(out=wr[:].rearrange("p (c j) -> p c j", j=128),
                                                 in0=bv[:].rearrange("p (c j) -> p c j", j=128),
                                                 in1=Mend.to_broadcast([1, ML_NCH, 128]), op=ALU.subtract))
        P.op("vector", lambda e: e.tensor_tensor(out=fr[:].rearrange("p (c j) -> p c j", j=128),
                                                 in0=cF[:].rearrange("p (c j) -> p c j", j=128),
                                                 in1=Mend.to_broadcast([1, ML_NCH, 128]), op=ALU.subtract))
        P.op("vector", lambda e: e.memset(rcr[:], 0.0))
        P.op("vector", lambda e: e.tensor_tensor(out=rcr[:, 1:ML_NCH], in0=Mr[:, 127:ML_S - 128:128],
                                                 in1=Mr[:, 255:ML_S:128], op=ALU.subtract), inc=S["gp"])
        vgp = S["gp"].n
        P.op("scalar", lambda e: e.activation(out=wr[:], in_=wr[:], func=AF.Exp), waits=[(S["gp"], vgp)])
        P.op("scalar", lambda e: e.activation(out=fr[:], in_=fr[:], func=AF.Exp))
        P.op("scalar", lambda e: e.activation(out=rcr[:], in_=rcr[:], func=AF.Exp), inc=S["g"])
        vg2 = S["g"].n
        for c in range(ML_NCH):
            P.op("tensor", lambda e, c=c: e.matmul(psG[:, c:c + 1], wr[0:1, c * 128:(c + 1) * 128], one11[0:1, 0:1],
                                                   start=True, stop=True),
                 waits=[(S["g"], vg2), (S["gc"], S["gc"].n)] if c == 0 else ())
            P.op("tensor", lambda e, c=c: e.matmul(psG[:, 32 + c:33 + c], fr[0:1, c * 128:(c + 1) * 128], one11[0:1, 0:1],
                                                   start=True, stop=True))
        P.op("tensor", lambda e: e.matmul(psG[:, 64:96], ones_row[0:1, 0:128], rcr[0:1, 0:ML_NCH], start=True, stop=True),
             inc=S["gp"])
        vgp2 = S["gp"].n
        P.op("vector", lambda e: e.tensor_copy(wcol[:], psG[:, 0:32]), waits=[(S["gp"], vgp2)])
        P.op("vector", lambda e: e.tensor_copy(fcol[:], psG[:, 32:64]))
        P.op("vector", lambda e: e.tensor_copy(rcb[:], psG[:, 64:96]))
        P.op("vector", lambda e: e.memset(Cs[:], 0.0), inc=S["gc"])
        v_gc = S["gc"].n
        for c in range(ML_NCH):
            cs_ = slice(c * 128, (c + 1) * 128)
            sl = so_ring.next()
            sot = so_ring.tiles[sl]
            P.op("sync", lambda e, b=b, cs_=cs_, sot=sot: e.dma_start(out=sot[:], in_=sotm[b, cs_, :]),
                 waits=[(S["hg"], n - 2)], inc=so_ring.filled[sl], dma=True)
            for dc in range(2):
                P.op("tensor", lambda e, dc=dc, cs_=cs_: e.matmul(psA[:, 0:128], ks[:, dc, cs_], qs[:, dc, cs_],
                                                                  start=(dc == 0), stop=(dc == 1)),
                     waits=[(S["ld"], v_ld), (S["pt"], n)] if dc == 0 else (),
                     inc=S["st"] if dc == 1 else None)
            P.op("vector", lambda e, c=c: e.tensor_scalar_mul(Cs[:], Cs[:], rcb[:, c:c + 1]),
                 waits=[(S["gc"], v_gc)], inc=S["rs"])
            P.op("vector", lambda e, c=c: e.scalar_tensor_tensor(out=PT[:], in0=psA[:, 0:128], scalar=wcol[:, c:c + 1],
                                                                 in1=mask[:], op0=ALU.mult, op1=ALU.mult),
                 waits=[(S["st"], n + 1), (S["nd"], n), (S["dc"], n)], inc=S["pt"])
            P.op("scalar", lambda e: e.copy(Cb[:], Cs[:]), waits=[(S["rs"], n + 1), (S["nd"], n)], inc=S["cb"])
            P.op("scalar", lambda e, c=c: e.activation(out=kw[:], in_=ktm_s[:, c, :], func=AF.Copy, scale=wcol[:, c:c + 1]),
                 waits=[(S["ld"], v_ld), (S["gc"], v_gc), (S["dc"], n)], inc=S["kw"])
            P.op("tensor", lambda e, c=c: e.matmul(psB[:, :], PT[:], va[:, c, 0:512], start=True, stop=False),
                 waits=[(S["pt"], n + 1), (S["cb"], n + 1), (S["hn"], n), (S["ss"], n), (S["cst"], v_cst)])
            for dc in range(2):
                P.op("tensor", lambda e, dc=dc, cs_=cs_: e.matmul(psB[:, :], qs[:, dc, cs_], Cb[:, dc, 0:512],
                                                                  start=False, stop=(dc == 1)))
            P.op("tensor", lambda e, c=c: e.matmul(psC[:, 0:1], PT[:], va[:, c, 512:513], start=True, stop=False))
            for dc in range(2):
                P.op("tensor", lambda e, dc=dc, cs_=cs_: e.matmul(psC[:, 0:1], qs[:, dc, cs_], Cb[:, dc, 512:513],
                                                                  start=False, stop=(dc == 1)),
                     inc=S["nd"] if dc == 1 else None)
            for dc in range(2):
                P.op("tensor", lambda e, dc=dc, c=c: e.matmul(psD[dc][:, :], kw[:, dc * 128:(dc + 1) * 128], va[:, c, 0:512],
                                                              start=True, stop=True),
                     waits=[(S["kw"], n + 1), (S["cs"], n)] if dc == 0 else ())
            for dc in range(2):
                P.op("tensor", lambda e, dc=dc, c=c: e.matmul(psC[:, 8 + dc:9 + dc], kw[:, dc * 128:(dc + 1) * 128],
                                                              va[:, c, 512:513], start=True, stop=True),
                     inc=S["dc"] if dc == 1 else None)
            P.op("scalar", lambda e: e.memzero(sm[:, 0:1]))
            P.op("scalar", lambda e: e.activation(out=junk[:], in_=psB[:, :], func=AF.Square, accum_out=sm[:, 0:1]),
                 waits=[(S["nd"], n + 1), (S["hn"], n)], inc=S["ss"])
            P.op("vector", lambda e: e.tensor_scalar_mul(sm[:, 6:7], psC[:, 0:1], -1.0), waits=[(S["nd"], n + 1)])
            P.op("vector", lambda e: e.tensor_tensor(out=sm[:, 1:2], in0=sm[:, 6:7], in1=psC[:, 0:1], op=ALU.max))
            P.op("vector", lambda e, c=c: e.tensor_tensor(out=sm[:, 1:2], in0=sm[:, 1:2], in1=fcol[:, c:c + 1], op=ALU.max))
            P.op("vector", lambda e: e.reciprocal(sm[:, 2:3], sm[:, 1:2]))
            P.op("vector", lambda e: e.tensor_tensor(out=sm[:, 3:4], in0=sm[:, 2:3], in1=sm[:, 2:3], op=ALU.mult),
                 waits=[(S["ss"], n + 1)])
            P.op("vector", lambda e: e.tensor_tensor(out=sm[:, 3:4], in0=sm[:, 3:4], in1=sm[:, 0:1], op=ALU.mult))
            P.op("vector", lambda e: e.tensor_scalar(out=sm[:, 3:4], in0=sm[:, 3:4], scalar1=1.0 / 512.0, scalar2=ML_EPS,
                                                     op0=ALU.mult, op1=ALU.add))
            vq = P.op("vector", lambda e: e.tensor_copy(sm[:, 7:8], sm[:, 3:4]), inc=S["sq1"])
            vq2 = P.op("scalar", lambda e: e.sqrt(sm[:, 7:8], sm[:, 7:8]), waits=[(S["sq1"], vq)], inc=S["sq2"])
            P.op("vector", lambda e: e.reciprocal(sm[:, 4:5], sm[:, 7:8]), waits=[(S["sq2"], vq2)])
            P.op("vector", lambda e: e.tensor_tensor(out=sm[:, 5:6], in0=sm[:, 4:5], in1=sm[:, 2:3], op=ALU.mult))
            P.op("vector", lambda e: e.scalar_tensor_tensor(out=hn[:], in0=psB[:, :], scalar=sm[:, 5:6], in1=ng[:],
                                                            op0=ALU.mult, op1=ALU.mult),
                 waits=[(S["hg"], n)], inc=S["hn"])
            for dc in range(2):
                P.op("vector", lambda e, dc=dc: e.tensor_tensor(out=Cs[:, dc, 0:512], in0=Cs[:, dc, 0:512], in1=psD[dc][:, :],
                                                                op=ALU.add),
                     waits=[(S["dc"], n + 1)] if dc == 0 else ())
            P.op("vector", lambda e: e.tensor_tensor(out=Cs[:, :, 512], in0=Cs[:, :, 512], in1=psC[:, 8:10], op=ALU.add),
                 inc=S["cs"])
            P.op("gpsimd", lambda e, sot=sot: e.tensor_tensor(out=hg[:], in0=hn[:], in1=sot[:], op=ALU.mult),
                 waits=[(S["hn"], n + 1), so_ring.fill_wait(sl), (S["tr"], n)], inc=S["hg"])
            for j in range(4):
                P.op("tensor", lambda e, j=j: e.transpose(psT[:, j, :], hg[:, j * 128:(j + 1) * 128], ident[:]),
                     waits=[(S["hg"], n + 1), (S["cp"], n)] if j == 0 else (), inc=S["tr"] if j == 3 else None)
            grp = (n // 8)
            hts = hts_r[grp % 2]
            lc = slice((c % 8) * 128, (c % 8 + 1) * 128)
            wl = [(S["tr"], n + 1)]
            if c % 8 == 0 and grp >= 2:
                wl.append((S["out"], 16 * (grp - 1)))
            P.op("scalar", lambda e, lc=lc, hts=hts: e.copy(hts[:, :, lc], psT[:, :, :]), waits=wl, inc=S["cp"])
            n += 1
            if c % 8 == 7:
                g0 = (c // 8) * 1024
                P.op("sync", lambda e, b=b, hts=hts, g0=g0: e.dma_start(
                    out=hT[b, :, g0:g0 + 1024].rearrange("(j p) s -> p j s", p=128), in_=hts[:]),
                     waits=[(S["cp"], n)], inc=S["out"], dma=True)
    if debug:
        dw = [(S["cp"], S["cp"].n), (S["cs"], S["cs"].n), (S["hg"], S["hg"].n)]
        P.op("sync", lambda e: e.dma_start(out=dbg1[:, 0:32], in_=wcol[:]), waits=dw, inc=S["out"], dma=True)
        P.op("sync", lambda e: e.dma_start(out=dbg1[:, 32:64], in_=fcol[:]), inc=S["out"], dma=True)
        P.op("sync", lambda e: e.dma_start(out=dbg1[:, 64:96], in_=rcb[:]), inc=S["out"], dma=True)
        P.op("sync", lambda e: e.dma_start(out=dbg2, in_=hn[:]), inc=S["out"], dma=True)
        P.op("sync", lambda e: e.dma_start(out=dbg3, in_=hg[:]), inc=S["out"], dma=True)
        P.op("sync", lambda e: e.dma_start(out=dbg4, in_=PT[:]), inc=S["out"], dma=True)
        P.op("sync", lambda e: e.dma_start(out=dbg5, in_=sm[:]), inc=S["out"], dma=True)
        P.op("sync", lambda e: e.dma_start(out=dbg6, in_=Mr[:]), inc=S["out"], dma=True)
    P.wait("sync", S["out"], S["out"].n)
    P.run()
    return nc


LN_EPS_P = LN_EPS / (ALPHA * ALPHA)


class LNCtx:
    def __init__(self, T):
        P = T.P
        self.T = T
        self.xs = Slots(P, "lx", 2, [128, TT], F32)
        self.zs = Slots(P, "lz", 2, [128, TT], F32)
        self.sq = Slots(P, "lq", 2, [128, TT], F32)
        self.ones_col = P.sbuf("ones_col", [128, 1], F32)
        self.ones_row = P.sbuf("ones_row1", [1, 128], F32)
        self.rows = P.sbuf("ln_rows", [1, 3, TT], F32)
        self.rstd_b = P.sbuf("rstd_b", [128, TT], F32)
        self.nmr_b = P.sbuf("nmr_b", [128, TT], F32)
        self.s_stat = P.sem("s_stat")
        P.op("gpsimd", lambda e: e.memset(self.ones_col[:], 1.0))
        P.op("gpsimd", lambda e: e.memset(self.ones_row[:], 1.0))
        self.t_init = P.tok("gpsimd")
        self.zwrite = {}
        self.stat_rel = []
        self.bc_rel = []
        self.rows_rel = []
        self.last_stat = None
        self.out_toks = []


def residual_ep(T, L, x_src, xwaits, zd, t, gA):
    P = T.P
    L.zwrite = {}

    def ep(bi, oc, bank, fw):
        ch = bi * 4 + oc
        ps = T.ps[bank]
        xs, xt, xrel = L.xs.get()
        v = P.op("sync", lambda e: e.dma_start(out=xt[:], in_=x_src[ch * 128:(ch + 1) * 128, t * TT:(t + 1) * TT]),
                 waits=xrel + list(xwaits(ch)), inc=L.xs.sem_in[xs], dma=True)
        tx = (L.xs.sem_in[xs], v)
        zs, zt, zrel = L.zs.get()
        v = P.op("vector", lambda e: e.scalar_tensor_tensor(out=zt[:], in0=ps[:, 0:TT], scalar=gA[:, ch:ch + 1], in1=xt[:],
                                                            op0=ALU.mult, op1=ALU.add),
                 waits=[fw, tx] + zrel, inc=T.pbank.freed[bank])
        tz = (T.pbank.freed[bank], v)
        L.xs.release(xs, [tz])
        sq, sqt, sqrel = L.sq.get()
        P.op("scalar", lambda e: e.activation(out=sqt[:], in_=zt[:], func=AF.Square), waits=[tz] + sqrel)
        tsq = P.tok("scalar")
        v = P.op("sync", lambda e: e.dma_start(out=zd[ch * 128:(ch + 1) * 128, :], in_=zt[:]), waits=[tz],
                 inc=L.zs.sem_out[zs], dma=True)
        tdma = (L.zs.sem_out[zs], v)
        L.zwrite[ch] = tdma
        first, last = ch == 0, ch == KC - 1
        w1 = [tz, L.t_init] + (L.stat_rel if first else [])
        P.op("tensor", lambda e: e.matmul(T.ps[6][0:1, 0:TT], L.ones_col[:, 0:1], zt[:], start=first, stop=last), waits=w1)
        v = P.op("tensor", lambda e: e.matmul(T.ps[7][0:1, 0:TT], L.ones_col[:, 0:1], sqt[:], start=first, stop=last),
                 waits=[tsq], inc=L.s_stat)
        tstat = (L.s_stat, v)
        L.last_stat = tstat
        L.zs.release(zs, [tdma, tstat])
        L.sq.release(sq, [tstat])

    return ep


def ln_finish(T, L, zd, t, lng, lnb, x_dst, nxt=None, final=False):
    P = T.P
    rows = L.rows
    P.op("vector", lambda e: e.tensor_scalar_mul(rows[:, 0, :], T.ps[6][0:1, 0:TT], 1.0 / D), waits=[L.last_stat] + L.rows_rel)
    P.op("vector", lambda e: e.tensor_scalar_mul(rows[:, 1, :], T.ps[7][0:1, 0:TT], 1.0 / D))
    tcopy = P.tok("vector")
    P.op("vector", lambda e: e.tensor_tensor(out=rows[:, 2, :], in0=rows[:, 0, :], in1=rows[:, 0, :], op=ALU.mult))
    P.op("vector", lambda e: e.tensor_tensor(out=rows[:, 1, :], in0=rows[:, 1, :], in1=rows[:, 2, :], op=ALU.subtract))
    P.op("vector", lambda e: e.tensor_scalar_add(rows[:, 1, :], rows[:, 1, :], LN_EPS_P))
    t1 = P.tok("vector")
    P.op("scalar", lambda e: e.sqrt(rows[:, 2, :], rows[:, 1, :]), waits=[t1])
    t2 = P.tok("scalar")
    P.op("vector", lambda e: e.reciprocal(rows[:, 1, :], rows[:, 2, :]), waits=[t2])
    P.op("vector", lambda e: e.scalar_tensor_tensor(out=rows[:, 2, :], in0=rows[:, 0, :], scalar=-1.0, in1=rows[:, 1, :],
                                                    op0=ALU.mult, op1=ALU.mult))
    t3 = P.tok("vector")
    P.op("tensor", lambda e: e.matmul(T.ps[6][:, 0:TT], L.ones_row[0:1, 0:128], rows[0:1, 1, :], start=True, stop=True),
         waits=[t3, tcopy])
    v = P.op("tensor", lambda e: e.matmul(T.ps[7][:, 0:TT], L.ones_row[0:1, 0:128], rows[0:1, 2, :], start=True, stop=True),
             inc=L.s_stat)
    tb = (L.s_stat, v)
    P.op("vector", lambda e: e.tensor_copy(L.rstd_b[:], T.ps[6][:, 0:TT]), waits=[tb] + L.bc_rel)
    P.op("vector", lambda e: e.tensor_copy(L.nmr_b[:], T.ps[7][:, 0:TT]))
    tbc = P.tok("vector")
    L.stat_rel = [tbc]
    L.rows_rel = [tb]
    xw = {}
    tn = None
    for c in range(KC):
        xs, xt, xrel = L.xs.get()
        v = P.op("sync", lambda e, xt=xt, c=c: e.dma_start(out=xt[:], in_=zd[c * 128:(c + 1) * 128, :]),
                 waits=xrel + [L.zwrite[c]], inc=L.xs.sem_in[xs], dma=True)
        tin = (L.xs.sem_in[xs], v)
        P.op("vector", lambda e, xt=xt: e.tensor_tensor(out=xt[:], in0=xt[:], in1=L.rstd_b[:], op=ALU.mult), waits=[tin, tbc])
        P.op("vector", lambda e, xt=xt: e.tensor_tensor(out=xt[:], in0=xt[:], in1=L.nmr_b[:], op=ALU.add))
        tv = P.tok("vector")
        zs, zt, zrel = L.zs.get()
        P.op("scalar", lambda e, xt=xt, zt=zt, c=c: e.activation(out=zt[:], in_=xt[:], func=AF.Identity,
                                                                 bias=lnb[:, c:c + 1], scale=lng[:, c:c + 1]),
             waits=[tv] + zrel)
        tx = P.tok("scalar")
        L.xs.release(xs, [tx])
        v = P.op("sync", lambda e, zt=zt, c=c: e.dma_start(out=x_dst[c * 128:(c + 1) * 128, t * TT:(t + 1) * TT], in_=zt[:]),
                 waits=[tx], inc=L.zs.sem_out[zs], dma=True)
        tout = (L.zs.sem_out[zs], v)
        xw[c] = tout
        if final:
            L.out_toks.append(tout)
        rel = [tout]
        if nxt is not None:
            act, onepsc, sh, relw = nxt
            P.op("vector", lambda e, zt=zt, c=c, act=act, onepsc=onepsc, sh=sh: e.tensor_scalar(
                out=act[:, c, :], in0=zt[:], scalar1=onepsc[:, c:c + 1], scalar2=sh[:, c:c + 1], op0=ALU.mult, op1=ALU.add),
                waits=[tx] + (list(relw) if c == 0 else []))
            tn = P.tok("vector")
            rel.append(tn)
        L.zs.release(zs, rel)
    L.bc_rel = [P.tok("vector")]
    return xw, tn


def load_mod(P, modv_d, ncols):
    t = P.sbuf("modv_sb", [128, ncols], F32)
    s = P.sem("s_modv")
    v = P.op("sync", lambda e: e.dma_start(out=t[:], in_=modv_d), inc=s, dma=True)
    return t, (s, v)


def ffn(T, L, w_in, w_out, x_src, xwaits, zd, t, gA, act_ready, sg):
    P = T.P
    blocks = [[(j * 256, 256), (FF + j * 256, 256)] for j in range(FF // 256)]

    sg_state = {}

    def ep_in(bi, oc, bank, fw):
        ps = T.ps[bank]
        if oc < 2:
            s, st, rel = sg.get()
            P.op("scalar", lambda e: e.activation(out=st[:], in_=ps[:, 0:TT], func=AF.Silu), waits=[fw] + rel,
                 inc=T.pbank.freed[bank])
            sg_state[oc] = (s, st, P.tok("scalar"))
        else:
            s, st, tk = sg_state[oc - 2]
            hc = bi * 2 + (oc - 2)
            w = [fw, tk] + (list(T.hid_rel) if (bi == 0 and oc == 2) else [])
            P.op("vector", lambda e: e.tensor_tensor(out=T.hid[:, hc, :], in0=st[:], in1=ps[:, 0:TT], op=ALU.mult),
                 waits=w, inc=T.pbank.freed[bank])
            sg.release(s, [P.tok("vector")])

    lf = T.gemm(w_in, D, blocks, ep_in, act_waits=[act_ready])
    T.act_rel = [lf]
    hid_ready = P.tok("vector")
    blocks2 = [[(c0, 512)] for c0 in range(0, D, 512)]
    lf2 = T.gemm(w_out, FF, blocks2, residual_ep(T, L, x_src, xwaits, zd, t, gA), act=T.hid, act_waits=[hid_ready])
    T.hid_rel = [lf2]


def build_phase_d1(mode="d1"):
    nc = bass.Bass("TRN2", target_bir_lowering=False)
    xT = _dram(nc, "xT", [D, NTOK], F32, "ExternalInput")
    if mode == "d1":
        hT = _dram(nc, "hT", [D, NTOK], BF16, "ExternalInput")
    else:
        yT = _dram(nc, "yT", [D, NTOK], F32, "ExternalInput")
        gT = _dram(nc, "gT", [D, NTOK], BF16, "ExternalInput")
    w_o = _dram(nc, "w_o", [D, D], F32, "ExternalInput")
    w_in = _dram(nc, "w_in", [D, 2 * FF], F32, "ExternalInput")
    w_out = _dram(nc, "w_out", [FF, D], F32, "ExternalInput")
    modv_d = _dram(nc, "modv", [128, 8 * KC], F32, "ExternalInput")
    outT = _dram(nc, "outT", [D, NTOK], F32, "ExternalOutput")
    x1T = nc.dram_tensor("x1T", [D, NTOK], F32, kind="Internal").ap()
    zd = nc.dram_tensor("zscr", [D, TT], F32, kind="Internal").ap()
    P = Prog(nc)
    P.serial = {"vector", "scalar", "gpsimd"}
    T = Tok(nc, P, hid_chunks=86, rings=False)
    L = LNCtx(T)
    sg = L.sq
    mv, tmod = load_mod(P, modv_d, 8 * KC)
    col = lambda i: mv[:, i * KC:(i + 1) * KC]
    onepsc = P.sbuf("onepsc", [128, KC], F32)
    P.op("vector", lambda e: e.tensor_scalar_add(onepsc[:], col(4), 1.0), waits=[tmod])
    P.op("vector", lambda e: e.tensor_scalar_mul(col(0), col(0), 1.0 / ALPHA))
    P.op("vector", lambda e: e.tensor_scalar_mul(col(5), col(5), 1.0 / ALPHA))
    tconst = P.tok("vector")
    s_act = P.sem("s_actin")
    if mode != "d1":
        yin = Slots(P, "yin", 2, [128, TT], F32)
        gin = Slots(P, "gin", 2, [128, TT], BF16)
    for t in range(NT):
        if mode == "d1":
            v = P.op("sync", lambda e, t=t: e.dma_start(out=T.act[:], in_=hT[:, t * TT:(t + 1) * TT].rearrange("(c p) s -> p c s", p=128)),
                     waits=list(T.act_rel), inc=s_act, dma=True)
            act_ready = (s_act, v)
        else:
            for c in range(KC):
                ys, yt, yrel = yin.get()
                gs, gt, grel = gin.get()
                v1 = P.op("sync", lambda e, yt=yt, c=c, t=t: e.dma_start(out=yt[:], in_=yT[c * 128:(c + 1) * 128, t * TT:(t + 1) * TT]),
                          waits=yrel, inc=yin.sem_in[ys], dma=True)
                v2 = P.op("sync", lambda e, gt=gt, c=c, t=t: e.dma_start(out=gt[:], in_=gT[c * 128:(c + 1) * 128, t * TT:(t + 1) * TT]),
                          waits=grel, inc=gin.sem_in[gs], dma=True)
                P.op("vector", lambda e, yt=yt, gt=gt, c=c: e.tensor_tensor(out=T.act[:, c, :], in0=yt[:], in1=gt[:], op=ALU.mult),
                     waits=[(yin.sem_in[ys], v1), (gin.sem_in[gs], v2)] + (list(T.act_rel) if c == 0 else []))
                tk = P.tok("vector")
                yin.release(ys, [tk])
                gin.release(gs, [tk])
            act_ready = P.tok("vector")
        blocks = [[(c0, 512)] for c0 in range(0, D, 512)]
        lf = T.gemm(w_o, D, blocks, residual_ep(T, L, xT, lambda ch: [], zd, t, col(0)), act_waits=[act_ready, tconst])
        T.act_rel = [lf]
        xw, tn = ln_finish(T, L, zd, t, col(1), col(2), x1T, nxt=(T.act, onepsc, col(3), T.act_rel))
        ffn(T, L, w_in, w_out, x1T, lambda ch, xw=xw: [xw[ch]], zd, t, col(5), tn, sg)
        ln_finish(T, L, zd, t, col(6), col(7), outT, nxt=None, final=True)
    for tk in L.out_toks:
        P.wait("sync", *tk)
    P.run()
    return nc


class Buf:
    def __init__(self, name, dma_sem=None):
        self.name = name
        self.w = None
        self.r = []
        self.dma_sem = dma_sem


class Dep:
    def __init__(self, P):
        self.P = P
        self.s_pe = P.sem("dep_pe")
        self.n = 0
        self.selfwait = True

    def buf(self, name, dma=False):
        return Buf(name, self.P.sem("d_" + name) if dma else None)

    def _waits(self, reads, writes):
        ws = []
        for b in reads:
            if b.w is not None:
                ws.append(b.w)
        for b in writes:
            if b.w is not None:
                ws.append(b.w)
            ws.extend(b.r)
        return ws

    def _commit(self, tok, reads, writes):
        for b in reads:
            if b not in writes:
                b.r.append(tok)
        for b in writes:
            b.w = tok
            b.r = []

    def op(self, eng, fn, reads=(), writes=()):
        P = self.P
        P.op(eng, fn, waits=self._waits(reads, writes), selfwait=self.selfwait)
        tok = P.tok(eng)
        self._commit(tok, reads, writes)
        return tok

    def pe(self, fns, reads=(), writes=()):
        P = self.P
        ws = self._waits(reads, writes)
        for i, fn in enumerate(fns):
            if i == len(fns) - 1:
                v = P.op("tensor", fn, waits=ws if i == 0 else (), inc=self.s_pe)
            else:
                P.op("tensor", fn, waits=ws if i == 0 else ())
        tok = (self.s_pe, v)
        self._commit(tok, reads, writes)
        return tok

    def dma(self, eng, fn, sem_buf, reads=(), writes=()):
        P = self.P
        v = P.op(eng, fn, waits=self._waits(reads, writes), inc=sem_buf.dma_sem, dma=True)
        tok = (sem_buf.dma_sem, v)
        self._commit(tok, reads, writes)
        return tok


RW_TOK = 8192
RW_S = 4096
RW_C = 64
RW_WN = 64
RW_NCW = RW_WN // RW_C
GN_EPS = 64e-5
DEC = -0.6065306597126334


def build_phase_e(ntok=RW_TOK, seq=RW_S, stage=99):
    nc = bass.Bass("TRN2", target_bir_lowering=False)
    rT = _dram(nc, "rT", [512, ntok], BF16, "ExternalInput")
    kT = _dram(nc, "kT", [512, ntok], BF16, "ExternalInput")
    vT = _dram(nc, "vT", [512, ntok], BF16, "ExternalInput")
    wT = _dram(nc, "wT", [512, ntok], F32, "ExternalInput")
    aT = _dram(nc, "aT", [512, ntok], F32, "ExternalInput")
    pv_d = _dram(nc, "pv", [64, 48], F32, "ExternalInput")
    yT = _dram(nc, "yT", [512, ntok], F32, "ExternalOutput")
    P = Prog(nc)
    P.serial = {"vector", "scalar", "gpsimd"}
    dp = Dep(P)
    dp.selfwait = False
    W, NC_, C = RW_WN, RW_NCW, RW_C
    sb = lambda n, sh, dt: P.sbuf(n, sh, dt)
    V, S_, g = "vector", "scalar", "gpsimd"
    ones64 = sb("ones64", [64, 64], F32)
    mask2 = sb("mask2", [64, 1, 128], F32)
    maskLT = sb("maskLT", [64, 1, 64], F32)
    blkm = sb("blkm", [64, 1, 64], F32)
    ET = sb("ET", [4, 64], F32)
    identf = sb("identf", [64, 64], F32)
    I64 = sb("I64", [64, 1, 64], F32)
    segm = sb("segm", [64, 8, W], F32)
    pv = sb("pv_s", [64, 48], F32)
    P.op(g, lambda e: e.memset(ones64[:], 1.0))
    P.op(g, lambda e: e.memset(mask2[:], 1.0))
    P.op(g, lambda e: e.affine_select(out=mask2[:, 0, 0:64], in_=mask2[:, 0, 0:64], pattern=[[1, 64]], compare_op=ALU.is_ge,
                                      fill=0.0, base=-1, channel_multiplier=-1))
    P.op(g, lambda e: e.affine_select(out=mask2[:, 0, 64:128], in_=mask2[:, 0, 64:128], pattern=[[1, 64]], compare_op=ALU.is_ge,
                                      fill=0.0, base=0, channel_multiplier=-1))
    P.op(g, lambda e: e.memset(maskLT[:], 1.0))
    P.op(g, lambda e: e.affine_select(out=maskLT[:, 0, :], in_=maskLT[:, 0, :], pattern=[[-1, 64]], compare_op=ALU.is_ge,
                                      fill=0.0, base=-1, channel_multiplier=1))
    P.op(g, lambda e: e.memset(identf[:], 1.0))
    P.op(g, lambda e: e.affine_select(out=identf[:], in_=identf[:], pattern=[[1, 64]], compare_op=ALU.is_equal,
                                      fill=0.0, base=0, channel_multiplier=-1))
    P.op(g, lambda e: e.memset(ET[:], 1.0))
    P.op(g, lambda e: e.affine_select(out=ET[:], in_=ET[:], pattern=[[1, 64]], compare_op=ALU.is_ge, fill=0.0, base=0, channel_multiplier=-16))
    P.op(g, lambda e: e.affine_select(out=ET[:], in_=ET[:], pattern=[[-1, 64]], compare_op=ALU.is_ge, fill=0.0, base=15, channel_multiplier=16))
    P.op(g, lambda e: e.tensor_copy(I64[:, 0, :], identf[:]))
    P.op(g, lambda e: e.memset(segm[:], 1.0))
    P.op(g, lambda e: e.memset(segm[:].rearrange("p h (c j) -> p (h c) j", j=C)[:, :, 0:1], 0.0))
    cb = dp.buf("const")
    cb.w = P.tok(g)
    Q_tmp = None
    pvb = dp.buf("pv", dma=True)
    dp.dma("sync", lambda e: e.dma_start(out=pv[:], in_=pv_d), pvb, writes=[pvb])
    dp.op(V, lambda e: e.tensor_scalar(out=pv[:, 16:24], in0=pv[:, 8:16], scalar1=-1.0, scalar2=1.0, op0=ALU.mult, op1=ALU.add),
          reads=[pvb], writes=[pvb])
    pbc = lambda q: pv[:, q * 8:(q + 1) * 8].unsqueeze(2).to_broadcast([64, 8, W])

    WT = []
    for i_ in range(3):
        sfx = str(i_)
        WT.append(dict(
            r_w=sb("r_w" + sfx, [64, 8, W], BF16), k_w=sb("k_w" + sfx, [64, 8, W], BF16), v_w=sb("v_w" + sfx, [64, 8, W], BF16),
            w_w=sb("w_w" + sfx, [64, 8, W], F32), a_w=sb("a_w" + sfx, [64, 8, W], F32), v_w32=sb("v_w32" + sfx, [64, 8, W], F32),
            AR=sb("AR" + sfx, [64, 8, NC_, 2, C], F32), BK=sb("BK" + sfx, [64, 8, NC_, 2, C], F32), BKh=sb("BKh" + sfx, [64, 8, 2, W], F32),
            gC=sb("gC" + sfx, [64, 8, NC_], F32), bonus=sb("bonus" + sfx, [64, 8, W], F32),
            Vt=sb("Vt" + sfx, [64, NC_, 512], F32), Bt=sb("Bt" + sfx, [64, NC_, 512], F32), Kt=sb("Kt" + sfx, [64, NC_, 512], F32),
            B_in=dp.buf("in" + sfx, dma=True), B_v32=dp.buf("v32" + sfx), B_AR=dp.buf("AR" + sfx), B_BK=dp.buf("BK" + sfx),
            B_BKh=dp.buf("BKh" + sfx), B_gC=dp.buf("gC" + sfx), B_bonus=dp.buf("bonus" + sfx), B_tm=dp.buf("tokmajor" + sfx),
            A1s=sb("A1s" + sfx, [64, 8, 128], F32), A2s=sb("A2s" + sfx, [64, 8, 128], F32), Gb=sb("Gb" + sfx, [64, 8, 64], F32),
            B_A1s=dp.buf("A1s" + sfx), B_A2s=dp.buf("A2s" + sfx), B_G=dp.buf("G" + sfx)))
    YW = 256
    ystage_ = [sb(f"ystage{i_}", [64, 8, YW], F32) for i_ in range(2)]
    B_ystage_ = [dp.buf(f"ystage{i_}", dma=True) for i_ in range(2)]
    UNP = ("r_w, k_w, v_w, w_w, a_w, v_w32, AR, BK, BKh, gC, bonus, Vt, Bt, Kt, B_in, B_v32, B_AR, B_BK, B_BKh, B_gC, B_bonus, B_tm, "
           "A1s, A2s, Gb, B_A1s, B_A2s, B_G")
    unp = lambda pb: tuple(WT[pb][n_.strip()] for n_ in UNP.split(","))
    tn = ("sg", "lg", "lgp", "d", "egi", "kk", "sq", "k2", "bv")
    tmp = {n: sb("t_" + n, [64, 8, W], F32) for n in tn}
    B_t = {n: dp.buf("t_" + n) for n in tn}
    Xs = [sb(f"Xs{i}", [64, 8, 64], F32) for i in range(2)]
    Ys = [sb(f"Ys{i}", [64, 8, 64], F32) for i in range(2)]
    Hb_ = sb("Hb_", [64, 8, 64], F32)
    Lo = sb("Lo", [64, 8, 64], F32); LoT = sb("LoT", [64, 8, 64], F32)
    B_Lo = dp.buf("Lo"); B_LoT = dp.buf("LoT")
    X1s = sb("X1s", [64, 512], F32); Us = sb("Us", [64, 512], F32)
    Hf = sb("Hf", [64, 8, 64], F32); Hbf = Hf
    ysq = sb("ysq", [64, 512], F32); yn = sb("yn", [64, 512], F32)
    st = sb("st", [64, 6, 8], F32)
    B_Xs = [dp.buf("Xs0"), dp.buf("Xs1")]; B_Ys = [dp.buf("Ys0"), dp.buf("Ys1")]
    B_H = dp.buf("H"); B_X1s = dp.buf("X1s"); B_Us = dp.buf("Us")
    B_Hf = dp.buf("Hf"); B_Hf = dp.buf("Hbf"); B_ysq = dp.buf("ysq"); B_yn = dp.buf("yn"); B_st = dp.buf("st")
    Q = [P.psum(f"Q{i}", [128, 512], F32) for i in range(8)]
    B_Q = [dp.buf(f"Q{i}") for i in range(8)]
    h3 = lambda q, j: q[0:64, :].rearrange("p (h j) -> p h j", j=j)
    fl = lambda t: t[:].rearrange("p h j -> p (h j)")
    B_blk = dp.buf("blk")
    dp.pe([lambda e: e.matmul(Q[0][0:64, 0:64], ET[:], ET[:], start=True, stop=True)], reads=[cb], writes=[B_Q[0]])
    dp.op(V, lambda e: e.tensor_copy(blkm[:, 0, :], Q[0][0:64, 0:64]), reads=[B_Q[0]], writes=[cb])

    def prep(t0, pb):
        (r_w, k_w, v_w, w_w, a_w, v_w32, AR, BK, BKh, gC, bonus, Vt, Bt, Kt, B_in, B_v32, B_AR, B_BK, B_BKh, B_gC, B_bonus, B_tm,
         A1s, A2s, Gb, B_A1s, B_A2s, B_G) = unp(pb)
        for (dst, src) in ((r_w, rT), (k_w, kT), (v_w, vT), (w_w, wT), (a_w, aT)):
            dp.dma("sync", lambda e, dst=dst, src=src: e.dma_start(out=dst[:], in_=src[:, t0:t0 + W].rearrange("(h n) s -> n h s", n=64)),
                   B_in, writes=[B_in])
        dp.op(S_, lambda e: e.copy(v_w32[:], v_w[:]), reads=[B_in], writes=[B_v32])
        T_ = tmp
        c4 = lambda ap: ap.rearrange("p h (c j) -> p h c j", j=C)
        Bi = B_in
        dp.op(S_, lambda e: e.activation(out=T_["sg"][:], in_=w_w[:], func=AF.Sigmoid), reads=[Bi], writes=[B_t["sg"]])
        dp.op(V, lambda e: e.tensor_scalar_mul(T_["sg"][:], T_["sg"][:], DEC), writes=[B_t["sg"]])
        dp.op(V, lambda e: e.tensor_tensor_scan(out=fl(T_["lg"]), data0=fl(segm), data1=fl(T_["sg"]), initial=0.0,
                                                op0=ALU.mult, op1=ALU.add), reads=[B_t["sg"], cb], writes=[B_t["lg"]])
        dp.op("gpsimd", lambda e: e.tensor_tensor(out=T_["lgp"][:], in0=T_["lg"][:], in1=T_["sg"][:], op=ALU.subtract),
              reads=[B_t["lg"], B_t["sg"]], writes=[B_t["lgp"]])
        for h in range(8):
            dp.op(V, lambda e, h=h: e.tensor_tensor(out=c4(T_["d"][:])[:, h], in0=c4(T_["lg"][:])[:, h, :, C - 1:C].to_broadcast([64, NC_, C]),
                                                    in1=c4(T_["lg"][:])[:, h], op=ALU.subtract), reads=[B_t["lg"]], writes=[B_t["d"]])
        yield
        dp.op(S_, lambda e: e.activation(out=gC[:], in_=c4(T_["lg"][:])[:, :, :, C - 1], func=AF.Exp), reads=[B_t["lg"]], writes=[B_gC])
        dp.op(S_, lambda e: e.activation(out=T_["egi"][:], in_=T_["lg"][:], func=AF.Exp, scale=-1.0), reads=[B_t["lg"]], writes=[B_t["egi"]])
        dp.op(S_, lambda e: e.activation(out=T_["lg"][:], in_=T_["lg"][:], func=AF.Exp), writes=[B_t["lg"]])
        dp.op(S_, lambda e: e.activation(out=T_["lgp"][:], in_=T_["lgp"][:], func=AF.Exp), writes=[B_t["lgp"]])
        dp.op(S_, lambda e: e.activation(out=T_["d"][:], in_=T_["d"][:], func=AF.Exp), writes=[B_t["d"]])
        yield
        dp.op("gpsimd", lambda e: e.tensor_tensor(out=T_["kk"][:], in0=k_w[:], in1=pbc(0), op=ALU.mult), reads=[Bi, pvb], writes=[B_t["kk"]])
        dp.op(S_, lambda e: e.activation(out=T_["sq"][:], in_=T_["kk"][:], func=AF.Square), reads=[B_t["kk"]], writes=[B_t["sq"]])
        for i in range(8 * W // 512):
            dp.pe([lambda e, i=i: e.matmul(Q[6][0:64, :], ones64[:], fl(T_["sq"])[:, i * 512:(i + 1) * 512], start=True, stop=True)],
                  reads=[B_t["sq"], cb], writes=[B_Q[6]])
            dp.op(S_, lambda e, i=i: e.sqrt(fl(T_["sq"])[:, i * 512:(i + 1) * 512], Q[6][0:64, :]), reads=[B_Q[6]], writes=[B_t["sq"]])
        dp.op(V, lambda e: e.tensor_scalar_max(T_["sq"][:], T_["sq"][:], 1e-12), writes=[B_t["sq"]])
        dp.op(V, lambda e: e.reciprocal(T_["sq"][:], T_["sq"][:]), writes=[B_t["sq"]])
        dp.op(V, lambda e: e.tensor_tensor(out=T_["kk"][:], in0=T_["kk"][:], in1=T_["sq"][:], op=ALU.mult),
              reads=[B_t["sq"]], writes=[B_t["kk"]])
        yield
        dp.op("gpsimd", lambda e: e.tensor_tensor(out=T_["k2"][:], in0=a_w[:], in1=pbc(1), op=ALU.mult), reads=[Bi, pvb], writes=[B_t["k2"]])
        dp.op("gpsimd", lambda e: e.tensor_tensor(out=T_["k2"][:], in0=T_["k2"][:], in1=pbc(2), op=ALU.add), writes=[B_t["k2"]])
        dp.op("gpsimd", lambda e: e.tensor_tensor(out=T_["k2"][:], in0=T_["k2"][:], in1=k_w[:], op=ALU.mult), reads=[Bi], writes=[B_t["k2"]])
        dp.op(V, lambda e: e.tensor_tensor(out=T_["bv"][:], in0=T_["kk"][:], in1=a_w[:], op=ALU.mult),
              reads=[Bi, B_t["kk"]], writes=[B_t["bv"]])
        yield
        dp.op("gpsimd", lambda e: e.tensor_tensor(out=AR[:, :, :, 1, :], in0=c4(r_w[:]), in1=c4(T_["lg"][:]), op=ALU.mult),
              reads=[Bi, B_t["lg"]], writes=[B_AR])
        dp.op(V, lambda e: e.scalar_tensor_tensor(out=AR[:, :, :, 0, :], in0=c4(T_["kk"][:]), scalar=-1.0, in1=c4(T_["lgp"][:]),
                                                  op0=ALU.mult, op1=ALU.mult), reads=[B_t["kk"], B_t["lgp"]], writes=[B_AR])
        dp.op(V, lambda e: e.tensor_tensor(out=BK[:, :, :, 0, :], in0=c4(T_["bv"][:]), in1=c4(T_["egi"][:]), op=ALU.mult),
              reads=[B_t["bv"], B_t["egi"]], writes=[B_BK])
        dp.op("gpsimd", lambda e: e.tensor_tensor(out=BK[:, :, :, 1, :], in0=c4(T_["k2"][:]), in1=c4(T_["egi"][:]), op=ALU.mult),
              reads=[B_t["k2"], B_t["egi"]], writes=[B_BK])
        dp.op(V, lambda e: e.tensor_tensor(out=BKh[:, :, 0, :], in0=T_["bv"][:], in1=T_["d"][:], op=ALU.mult),
              reads=[B_t["bv"], B_t["d"]], writes=[B_BKh])
        dp.op("gpsimd", lambda e: e.tensor_tensor(out=BKh[:, :, 1, :], in0=T_["k2"][:], in1=T_["d"][:], op=ALU.mult),
              reads=[B_t["k2"], B_t["d"]], writes=[B_BKh])
        yield
        dp.op("gpsimd", lambda e: e.tensor_tensor(out=T_["sq"][:], in0=r_w[:], in1=pbc(3), op=ALU.mult), reads=[Bi, pvb], writes=[B_t["sq"]])
        dp.op("gpsimd", lambda e: e.tensor_tensor(out=T_["sq"][:], in0=T_["sq"][:], in1=T_["k2"][:], op=ALU.mult), reads=[B_t["k2"]], writes=[B_t["sq"]])
        for i in range(8 * W // 512):
            dp.pe([lambda e, i=i: e.matmul(Q[6][0:64, :], ones64[:], fl(T_["sq"])[:, i * 512:(i + 1) * 512], start=True, stop=True)],
                  reads=[B_t["sq"], cb], writes=[B_Q[6]])
            dp.op(V, lambda e, i=i: e.tensor_tensor(out=fl(bonus)[:, i * 512:(i + 1) * 512], in0=Q[6][0:64, :],
                                                    in1=fl(v_w)[:, i * 512:(i + 1) * 512], op=ALU.mult), reads=[B_Q[6], Bi], writes=[B_bonus])


    def tm_chunk(c, pb):
        (r_w, k_w, v_w, w_w, a_w, v_w32, AR, BK, BKh, gC, bonus, Vt, Bt, Kt, B_in, B_v32, B_AR, B_BK, B_BKh, B_gC, B_bonus, B_tm,
         A1s, A2s, Gb, B_A1s, B_A2s, B_G) = unp(pb)
        cs_ = slice(c * C, (c + 1) * C)
        for ti, (dst, srcf, rb) in enumerate(((Vt, lambda h: v_w32[:, h, cs_], B_v32), (Bt, lambda h: BKh[:, h, 0, cs_], B_BKh),
                                               (Kt, lambda h: BKh[:, h, 1, cs_], B_BKh))):
            dp.pe([lambda e, h=h, srcf=srcf: e.matmul(Q[6][0:64, h * 64:(h + 1) * 64], srcf(h), identf[:], start=True, stop=True)
                   for h in range(8)], reads=[rb, cb], writes=[B_Q[6]])
            if ti == 1:
                dp.op(S_, lambda e, dst=dst: e.copy(dst[:, c, :], Q[6][0:64, :]), reads=[B_Q[6]], writes=[B_tm])
            else:
                dp.op(V, lambda e, dst=dst: e.tensor_copy(dst[:, c, :], Q[6][0:64, :]), reads=[B_Q[6]], writes=[B_tm])


    def pre(c, pb):
        (r_w, k_w, v_w, w_w, a_w, v_w32, AR, BK, BKh, gC, bonus, Vt, Bt, Kt, B_in, B_v32, B_AR, B_BK, B_BKh, B_gC, B_bonus, B_tm,
         A1s, A2s, Gb, B_A1s, B_A2s, B_G) = unp(pb)
        hs = lambda h: slice(h * 64, (h + 1) * 64)
        dp.pe([lambda e, h=h: e.matmul(Q[h // 4][0:64, (h % 4) * 128:(h % 4 + 1) * 128], BK[:, h, c, 0, :], AR[:, h, c, :, :],
                                       start=True, stop=True) for h in range(8)], reads=[B_AR, B_BK], writes=[B_Q[0], B_Q[1]])
        dp.pe([lambda e, h=h: e.matmul(Q[2 + h // 4][0:64, (h % 4) * 128:(h % 4 + 1) * 128], BK[:, h, c, 1, :], AR[:, h, c, :, :],
                                       start=True, stop=True) for h in range(8)], reads=[B_AR, B_BK], writes=[B_Q[2], B_Q[3]])
        dp.pe([lambda e, h=h: e.matmul(Q[4][0:64, hs(h)], AR[:, h, c, 0, :], BK[:, h, c, 0, :], start=True, stop=True)
               for h in range(8)], reads=[B_AR, B_BK], writes=[B_Q[4]])
        yield
        m2b = mask2[:].to_broadcast([64, 4, 128])
        dp.op(V, lambda e: e.tensor_tensor(out=A1s[:, 0:4, :], in0=h3(Q[0], 128), in1=m2b, op=ALU.mult), reads=[B_Q[0], cb], writes=[B_A1s])
        dp.op(V, lambda e: e.tensor_tensor(out=A1s[:, 4:8, :], in0=h3(Q[1], 128), in1=m2b, op=ALU.mult), reads=[B_Q[1], cb], writes=[B_A1s])
        dp.op(V, lambda e: e.tensor_tensor(out=A2s[:, 0:4, :], in0=h3(Q[2], 128), in1=m2b, op=ALU.mult), reads=[B_Q[2], cb], writes=[B_A2s])
        dp.op(V, lambda e: e.tensor_tensor(out=A2s[:, 4:8, :], in0=h3(Q[3], 128), in1=m2b, op=ALU.mult), reads=[B_Q[3], cb], writes=[B_A2s])
        bm8 = blkm[:].to_broadcast([64, 8, 64])
        I8 = I64[:].to_broadcast([64, 8, 64])
        dp.op(V, lambda e: e.tensor_tensor(out=LoT[:], in0=h3(Q[4], 64), in1=maskLT[:].to_broadcast([64, 8, 64]), op=ALU.mult),
              reads=[B_Q[4], cb], writes=[B_LoT])
        dp.op(V, lambda e: e.tensor_tensor(out=Ys[0][:], in0=LoT[:], in1=bm8, op=ALU.mult), reads=[B_LoT, cb], writes=[B_Ys[0]])
        dp.op(V, lambda e: e.tensor_tensor(out=Xs[0][:], in0=A1s[:, :, 0:64], in1=bm8, op=ALU.mult), reads=[B_A1s, cb], writes=[B_Xs[0]])
        dp.op(V, lambda e: e.tensor_tensor(out=Lo[:], in0=A1s[:, :, 0:64], in1=Xs[0][:], op=ALU.subtract),
              reads=[B_A1s, B_Xs[0]], writes=[B_Lo])
        yield
        dp.op(V, lambda e: e.tensor_tensor(out=Gb[:], in0=Xs[0][:], in1=I8, op=ALU.add), reads=[B_Xs[0], cb], writes=[B_G])
        cur = 0
        for lvl in range(1, 4):
            nxt = 1 - cur
            if lvl < 3:
                dp.pe([lambda e, h=h, cur=cur: e.matmul(Q[0][0:64, hs(h)], Ys[cur][:, h, :], Xs[cur][:, h, :], start=True, stop=True)
                       for h in range(8)], reads=[B_Xs[cur], B_Ys[cur]], writes=[B_Q[0]])
            dp.pe([lambda e, h=h, cur=cur: e.matmul(Q[1][0:64, hs(h)], Xs[cur][:, h, :], Ys[cur][:, h, :], start=True, stop=True)
                   for h in range(8)], reads=[B_Xs[cur], B_Ys[cur]], writes=[B_Q[1]])
            if lvl < 3:
                dp.op(S_, lambda e, nxt=nxt: e.copy(fl(Xs[nxt]), Q[0][0:64, :]), reads=[B_Q[0]], writes=[B_Xs[nxt]])
            dp.op(V, lambda e, nxt=nxt: e.tensor_copy(fl(Ys[nxt]), Q[1][0:64, :]), reads=[B_Q[1]], writes=[B_Ys[nxt]])
            dp.pe([lambda e, h=h, nxt=nxt: e.matmul(Q[2][0:64, hs(h)], Ys[nxt][:, h, :], Gb[:, h, :], start=True, stop=True)
                   for h in range(8)], reads=[B_G, B_Ys[nxt]], writes=[B_Q[2]])
            dp.op(V, lambda e: e.tensor_tensor(out=fl(Gb), in0=fl(Gb), in1=Q[2][0:64, :], op=ALU.add), reads=[B_Q[2]], writes=[B_G])
            cur = nxt
            yield
        dp.pe([lambda e, h=h: e.transpose(Q[3][0:64, hs(h)], Gb[:, h, :], identf[:]) for h in range(8)], reads=[B_G, cb], writes=[B_Q[3]])
        dp.op(S_, lambda e: e.copy(fl(Hb_), Q[3][0:64, :]), reads=[B_Q[3]], writes=[B_H])
        ZTs, BZT = Ys[0], B_Ys[0]
        R1, BR1, R2, BR2 = Xs[0], B_Xs[0], Xs[1], B_Xs[1]
        dp.pe([lambda e, h=h: e.matmul(Q[1][0:64, hs(h)], Lo[:, h, :], Hb_[:, h, :], start=True, stop=True) for h in range(8)],
              reads=[B_H, B_Lo], writes=[B_Q[1]])
        dp.op(S_, lambda e: e.copy(fl(ZTs), Q[1][0:64, :]), reads=[B_Q[1]], writes=[BZT])
        yield
        dp.pe([lambda e, h=h: e.matmul(Q[0][0:64, hs(h)], ZTs[:, h, :], Gb[:, h, :], start=True, stop=True) for h in range(8)],
              reads=[BZT, B_G], writes=[B_Q[0]])
        dp.op(V, lambda e: e.tensor_tensor(out=fl(R1), in0=fl(Gb), in1=Q[0][0:64, :], op=ALU.add), reads=[B_Q[0], B_G], writes=[BR1])
        yield
        dp.pe([lambda e, h=h: e.matmul(Q[2][0:64, hs(h)], ZTs[:, h, :], R1[:, h, :], start=True, stop=True) for h in range(8)],
              reads=[BZT, BR1], writes=[B_Q[2]])
        dp.op(V, lambda e: e.tensor_tensor(out=fl(R2), in0=fl(Gb), in1=Q[2][0:64, :], op=ALU.add), reads=[B_Q[2], B_G], writes=[BR2])
        yield
        dp.pe([lambda e, h=h: e.matmul(Q[0][0:64, hs(h)], ZTs[:, h, :], R2[:, h, :], start=True, stop=True) for h in range(8)],
              reads=[BZT, BR2], writes=[B_Q[0]])
        dp.op(V, lambda e: e.tensor_tensor(out=fl(Gb), in0=fl(Gb), in1=Q[0][0:64, :], op=ALU.add), reads=[B_Q[0]], writes=[B_G])

    def seqg(c, pb, k):
        hs = lambda h: slice(h * 64, (h + 1) * 64)
        (r_w, k_w, v_w, w_w, a_w, v_w32, AR, BK, BKh, gC, bonus, Vt, Bt, Kt, B_in, B_v32, B_AR, B_BK, B_BKh, B_gC, B_bonus, B_tm,
         A1s, A2s, Gb, B_A1s, B_A2s, B_G) = unp(pb)
        ystage = ystage_[(k // 4) % 2]; B_ystage = B_ystage_[(k // 4) % 2]; yoff = (k % 4) * C
        fX = []
        for h in range(8):
            fX.append(lambda e, h=h: e.matmul(Q[7][0:64, hs(h)], AR[:, h, c, 0, :], Hbf[:, h, :], start=True, stop=False))
            fX.append(lambda e, h=h: e.matmul(Q[7][0:64, hs(h)], A2s[:, h, 0:64], Vt[:, c, hs(h)], start=False, stop=True))
        dp.pe(fX, reads=[B_AR, B_Hf, B_A2s, B_tm], writes=[B_Q[7]])
        dp.op(S_, lambda e: e.copy(X1s[:], Q[7][0:64, :]), reads=[B_Q[7]], writes=[B_X1s])
        yield
        dp.pe([lambda e, h=h: e.matmul(Q[7][0:64, hs(h)], Gb[:, h, :], X1s[:, hs(h)], start=True, stop=True) for h in range(8)],
              reads=[B_G, B_X1s], writes=[B_Q[7]])
        dp.op(S_, lambda e: e.copy(Us[:], Q[7][0:64, :]), reads=[B_Q[7]], writes=[B_Us])
        yield
        fY = []
        for h in range(8):
            fY.append(lambda e, h=h: e.matmul(Q[5][0:64, hs(h)], AR[:, h, c, 1, :], Hbf[:, h, :], start=True, stop=False))
            fY.append(lambda e, h=h: e.matmul(Q[5][0:64, hs(h)], A1s[:, h, 64:128], Us[:, hs(h)], start=False, stop=False))
            fY.append(lambda e, h=h: e.matmul(Q[5][0:64, hs(h)], A2s[:, h, 64:128], Vt[:, c, hs(h)], start=False, stop=True))
        dp.pe(fY, reads=[B_AR, B_Hf, B_A1s, B_A2s, B_Us, B_tm], writes=[B_Q[5]])
        yield
        fH = []
        for h in range(8):
            fH.append(lambda e, h=h: e.matmul(Q[6][0:64, hs(h)], Bt[:, c, hs(h)], Us[:, hs(h)], start=True, stop=False))
            fH.append(lambda e, h=h: e.matmul(Q[6][0:64, hs(h)], Kt[:, c, hs(h)], Vt[:, c, hs(h)], start=False, stop=True))
        dp.pe(fH, reads=[B_Us, B_tm], writes=[B_Q[6]])
        dp.op(V, lambda e: e.tensor_tensor(out=Hf[:], in0=Hf[:], in1=gC[:, :, c:c + 1].to_broadcast([64, 8, 64]), op=ALU.mult),
              reads=[B_gC], writes=[B_Hf])
        dp.op(V, lambda e: e.tensor_tensor(out=fl(Hf), in0=fl(Hf), in1=Q[6][0:64, :], op=ALU.add), reads=[B_Q[6]], writes=[B_Hf])
        yield
        y3 = h3(Q[5], 64)
        yn3 = yn[:].rearrange("p (h j) -> p h j", j=64)
        dp.op(V, lambda e: e.tensor_reduce(out=st[:, 0, :], in_=y3, axis=AX.X, op=ALU.add), reads=[B_Q[5]], writes=[B_st])
        dp.op(S_, lambda e: e.activation(out=ysq[:], in_=Q[5][0:64, :], func=AF.Square), reads=[B_Q[5]], writes=[B_ysq])
        dp.op(V, lambda e: e.tensor_reduce(out=st[:, 1, :], in_=ysq[:].rearrange("p (h j) -> p h j", j=64), axis=AX.X, op=ALU.add),
              reads=[B_ysq], writes=[B_st])
        yield
        dp.op(V, lambda e: e.tensor_scalar_mul(st[:, 0, :], st[:, 0, :], 1.0 / 64), writes=[B_st])
        dp.op(V, lambda e: e.tensor_tensor(out=st[:, 2, :], in0=st[:, 0, :], in1=st[:, 0, :], op=ALU.mult), writes=[B_st])
        dp.op(V, lambda e: e.scalar_tensor_tensor(out=st[:, 1, :], in0=st[:, 1, :], scalar=1.0 / 64, in1=st[:, 2, :],
                                                  op0=ALU.mult, op1=ALU.subtract), writes=[B_st])
        dp.op(V, lambda e: e.tensor_scalar_add(st[:, 1, :], st[:, 1, :], GN_EPS), writes=[B_st])
        dp.op(S_, lambda e: e.sqrt(st[:, 3, :], st[:, 1, :]), reads=[B_st], writes=[B_st])
        dp.op(V, lambda e: e.reciprocal(st[:, 4, :], st[:, 3, :]), reads=[B_st], writes=[B_st])
        dp.op(V, lambda e: e.tensor_tensor(out=yn3, in0=y3, in1=st[:, 0, :].unsqueeze(2).to_broadcast([64, 8, 64]), op=ALU.subtract),
              reads=[B_Q[5], B_st], writes=[B_yn])
        dp.op(V, lambda e: e.tensor_tensor(out=yn3, in0=yn3, in1=st[:, 4, :].unsqueeze(2).to_broadcast([64, 8, 64]), op=ALU.mult),
              reads=[B_st], writes=[B_yn])
        yield
        dp.pe([lambda e, h=h: e.transpose(Q[5][0:64, hs(h)], yn[:, hs(h)], identf[:]) for h in range(8)],
              reads=[B_yn, cb], writes=[B_Q[5]])
        yo = ystage[:, :, yoff:yoff + C]
        gcol = pv[:, 32:40].unsqueeze(2).to_broadcast([64, 8, 64])
        bcol = pv[:, 40:48].unsqueeze(2).to_broadcast([64, 8, 64])
        dp.op(V, lambda e: e.tensor_tensor(out=yo, in0=h3(Q[5], 64), in1=gcol, op=ALU.mult), reads=[B_Q[5], pvb], writes=[B_ystage])
        dp.op(V, lambda e: e.tensor_tensor(out=yo, in0=yo, in1=bcol, op=ALU.add), reads=[pvb], writes=[B_ystage])
        dp.op(V, lambda e: e.tensor_tensor(out=yo, in0=yo, in1=bonus[:, :, c * C:(c + 1) * C], op=ALU.add),
              reads=[B_bonus], writes=[B_ystage])

        yield

    def stageA1(k):
        pb = k % 3
        yield from prep(k * C, pb)
        tm_chunk(0, pb)
        yield

    def stageA2(k):
        yield from pre(0, k % 3)

    def stageB(k):
        if k % (seq // C) == 0:
            dp.op(V, lambda e: e.memset(Hf[:], 0.0), writes=[B_Hf])
        yield from seqg(0, k % 3, k)
        if k % 4 == 3:
            t0 = (k - 3) * C
            yb = (k // 4) % 2
            dp.dma("sync", lambda e, t0=t0, yb=yb: e.dma_start(out=yT[:, t0:t0 + YW].rearrange("(h n) s -> n h s", n=64), in_=ystage_[yb][:]),
                   B_ystage_[yb], reads=[B_ystage_[yb]])

    def drain(*gens):
        gens = list(gens)
        while gens:
            for g_ in list(gens):
                try:
                    next(g_)
                except StopIteration:
                    gens.remove(g_)

    nchunks = ntok // C
    drain(stageA1(0))
    if nchunks > 1:
        drain(stageA2(0), stageA1(1))
    else:
        drain(stageA2(0))
    for k in range(nchunks):
        gens = [stageB(k)]
        if k + 1 < nchunks:
            gens.append(stageA2(k + 1))
        if k + 2 < nchunks:
            gens.append(stageA1(k + 2))
        drain(*gens)
    for yb in range(2):
        P.wait("sync", B_ystage_[yb].dma_sem, B_ystage_[yb].dma_sem.n)
    P.run()
    return nc


def build_phase_d2():
    nc = bass.Bass("TRN2", target_bir_lowering=False)
    xT = _dram(nc, "xT", [D, NTOK], F32, "ExternalInput")
    xprev_d = _dram(nc, "xprev", [128, KC], F32, "ExternalInput")
    modv_d = _dram(nc, "modv", [128, 16 * KC + 1], F32, "ExternalInput")
    Wd = {n: _dram(nc, n, sh, F32, "ExternalInput") for n, sh in (
        ("w_r", [D, D]), ("w_k", [D, D]), ("w_v", [D, D]), ("w1", [D, 128]), ("w2", [128, D]),
        ("a1", [D, 128]), ("a2", [128, D]), ("g1", [D, 512]), ("g2", [512, D]))}
    outs = {n: _dram(nc, n, [D, NTOK], dt, "ExternalOutput") for n, dt in (
        ("rT", BF16), ("kT", BF16), ("vT", BF16), ("gT", BF16), ("wT", F32), ("aT", F32))}
    P = Prog(nc)
    P.serial = {"vector", "scalar", "gpsimd"}
    T = Tok(nc, P, hid_chunks=0, rings=False)
    act2 = P.sbuf("act2", [128, KC, TT], BF16)
    acts = [T.act, act2]
    U = P.sbuf("U", [128, KC, TT + 1], F32)
    h1 = P.sbuf("h1", [128, 4, TT], BF16)
    lastcol = P.sbuf("lastcol", [128, KC], F32)
    xs = Slots(P, "dx", 2, [128, TT], F32)
    tmps = Slots(P, "dtmp", 2, [128, TT], F32)
    ostf = Slots(P, "dof", 2, [128, TT], F32)
    ostb = Slots(P, "dob", 2, [128, TT], BF16)
    mv, tmod = load_mod(P, modv_d, 16 * KC + 1)
    col = lambda i: mv[:, i * KC:(i + 1) * KC]
    flag = mv[:, 16 * KC:16 * KC + 1]
    onepsc = P.sbuf("onepsc", [128, KC], F32)
    xp = P.sbuf("xp", [128, KC], F32)
    s_xp = P.sem("s_xp")
    v = P.op("sync", lambda e: e.dma_start(out=xp[:], in_=xprev_d), inc=s_xp, dma=True)
    P.op("vector", lambda e: e.tensor_scalar_add(onepsc[:], col(1), 1.0), waits=[tmod, (s_xp, v)])
    P.op("vector", lambda e: e.tensor_scalar(out=mv[:, 8 * KC:14 * KC], in0=mv[:, 2 * KC:8 * KC], scalar1=-1.0, scalar2=1.0,
                                             op0=ALU.mult, op1=ALU.add))
    P.op("vector", lambda e: e.tensor_tensor(out=xp[:], in0=xp[:], in1=onepsc[:], op=ALU.mult))
    P.op("vector", lambda e: e.tensor_tensor(out=xp[:], in0=xp[:], in1=col(0), op=ALU.add))
    P.op("vector", lambda e: e.tensor_scalar_mul(lastcol[:], xp[:], flag))
    tconst = P.tok("vector")
    out_toks = []
    act_rel = [[], []]
    h1_rel = []
    U_rel = []
    ai = 0
    for t in range(NT):
        P.op("vector", lambda e: e.tensor_copy(U[:, :, 0], lastcol[:]), waits=[tconst] + U_rel)
        for c in range(KC):
            s, xt, rel = xs.get()
            v = P.op("sync", lambda e, xt=xt, c=c, t=t: e.dma_start(out=xt[:], in_=xT[c * 128:(c + 1) * 128, t * TT:(t + 1) * TT]),
                     waits=rel, inc=xs.sem_in[s], dma=True)
            P.op("scalar", lambda e, xt=xt, c=c: e.activation(out=U[:, c, 1:TT + 1], in_=xt[:], func=AF.Identity,
                                                              bias=col(0)[:, c:c + 1], scale=onepsc[:, c:c + 1]),
                 waits=[(xs.sem_in[s], v), tconst] + (U_rel if c == 0 else []))
            xs.release(s, [P.tok("scalar")])
        tU = P.tok("scalar")
        P.op("vector", lambda e: e.tensor_copy(lastcol[:], U[:, :, TT]), waits=[tU])
        tUv = P.tok("vector")
        U_readers = []

        def build_act(j):
            nonlocal ai
            a = acts[ai % 2]
            rel = act_rel[ai % 2]
            for c in range(KC):
                s, tt, trel = tmps.get()
                P.op("scalar", lambda e, tt=tt, c=c, j=j: e.activation(out=tt[:], in_=U[:, c, 1:TT + 1], func=AF.Copy,
                                                                       scale=col(8 + j)[:, c:c + 1]), waits=[tU, tUv] + trel)
                tk = P.tok("scalar")
                P.op("vector", lambda e, tt=tt, c=c, j=j, a=a: e.scalar_tensor_tensor(out=a[:, c, :], in0=U[:, c, 0:TT], scalar=col(2 + j)[:, c:c + 1],
                                                                                      in1=tt[:], op0=ALU.mult, op1=ALU.add),
                     waits=[tk, tU, tUv] + (list(rel) if c == 0 else []))
                tmps.release(s, [P.tok("vector")])
            U_readers.append(P.tok("vector"))
            U_readers.append(P.tok("scalar"))
            idx = ai % 2
            ai += 1
            return a, idx, P.tok("vector")

        def ep_store(dst, bf, func=None, bias=None, t=t):
            def ep(bi, oc, bank, fw):
                ch = bi * 4 + oc
                ps = T.ps[bank]
                pool = ostb if bf else ostf
                s, st, rel = pool.get()
                if func is None and bias is None:
                    eng = T.ev_eng()
                    if eng == "vector":
                        P.op(eng, lambda e: e.tensor_copy(st[:], ps[:, 0:TT]), waits=[fw] + rel, inc=T.pbank.freed[bank])
                    else:
                        P.op(eng, lambda e: e.copy(st[:], ps[:, 0:TT]), waits=[fw] + rel, inc=T.pbank.freed[bank])
                else:
                    P.op("scalar", lambda e: e.activation(out=st[:], in_=ps[:, 0:TT], func=func or AF.Identity,
                                                          bias=(bias[:, ch:ch + 1] if bias is not None else 0.0)),
                         waits=[fw] + rel, inc=T.pbank.freed[bank])
                tk = (T.pbank.freed[bank], T.pbank.freed[bank].n)
                v = P.op("sync", lambda e: e.dma_start(out=dst[ch * 128:(ch + 1) * 128, t * TT:(t + 1) * TT], in_=st[:]),
                         waits=[tk], inc=pool.sem_out[s], dma=True)
                tko = (pool.sem_out[s], v)
                pool.release(s, [tko])
                out_toks.append(tko)
            return ep

        def ep_h1(func, nch):
            def ep(bi, oc, bank, fw):
                ps = T.ps[bank]
                P.op("scalar", lambda e: e.activation(out=h1[:, oc, :], in_=ps[:, 0:TT], func=func),
                     waits=[fw] + (list(h1_rel) if oc == 0 else []), inc=T.pbank.freed[bank])
            return ep

        full = [[(c0, 512)] for c0 in range(0, D, 512)]
        plan = [(0, "w_r", "rT", None), (1, "w1", None, "w"), (2, "w_k", "kT", None), (3, "w_v", "vT", None),
                (4, "a1", None, "a"), (5, "g1", None, "g")]
        for (j, wname, oname, lora) in plan:
            a, idx, ta = build_act(j)
            if lora is None:
                lf = T.gemm(Wd[wname], D, full, ep_store(outs[oname], True), act=a, act_waits=[ta])
                act_rel[idx] = [lf]
            elif lora == "w":
                lf = T.gemm(Wd["w1"], D, [[(0, 128)]], ep_h1(AF.Tanh, 1), act=a, act_waits=[ta])
                act_rel[idx] = [lf]
                th = P.tok("scalar")
                lf2 = T.gemm(Wd["w2"], 128, full, ep_store(outs["wT"], False, AF.Identity, col(14)), act=h1, act_waits=[th])
                h1_rel[:] = [lf2]
            elif lora == "a":
                lf = T.gemm(Wd["a1"], D, [[(0, 128)]], ep_h1(AF.Copy, 1), act=a, act_waits=[ta])
                act_rel[idx] = [lf]
                th = P.tok("scalar")
                lf2 = T.gemm(Wd["a2"], 128, full, ep_store(outs["aT"], False, AF.Sigmoid, col(15)), act=h1, act_waits=[th])
                h1_rel[:] = [lf2]
            else:
                lf = T.gemm(Wd["g1"], D, [[(0, 512)]], ep_h1(AF.Sigmoid, 4), act=a, act_waits=[ta])
                act_rel[idx] = [lf]
                th = P.tok("scalar")
                lf2 = T.gemm(Wd["g2"], 512, full, ep_store(outs["gT"], True), act=h1, act_waits=[th])
                h1_rel[:] = [lf2]
        U_rel = list(U_readers[-4:])
    for tk in out_toks[-8:]:
        P.wait("sync", *tk)
    for pool in (ostf, ostb):
        for s in range(pool.n):
            P.wait("sync", pool.sem_out[s], pool.sem_out[s].n)
    P.run()
    return nc


A_NCH = 24


def build_phase_a():
    nc = bass.Bass("TRN2", target_bir_lowering=False)
    cT_d = _dram(nc, "cT", [128, KC, 2], F32, "ExternalInput")
    W_d = _dram(nc, "ada_w", [2, D, A_NCH * 128], F32, "ExternalInput")
    b_d = _dram(nc, "ada_b", [128, 2 * A_NCH], F32, "ExternalInput")
    o_d = _dram(nc, "modT", [128, 2 * A_NCH, 2], F32, "ExternalOutput")
    P = Prog(nc)
    P.serial = {"vector", "scalar", "gpsimd"}
    dp = Dep(P)
    cT = P.sbuf("cT_s", [128, KC, 2], F32)
    sg = P.sbuf("sg_s", [128, KC, 2], F32)
    bs = P.sbuf("b_s", [128, 2 * A_NCH], F32)
    osb = P.sbuf("o_s", [128, 2 * A_NCH, 2], F32)
    wb = [P.sbuf(f"aw{i}", [128, KC, 256], F32) for i in range(2)]
    Bc = dp.buf("c", dma=True); Bb = dp.buf("b", dma=True); Bo = dp.buf("o", dma=True)
    Bw = [dp.buf("w0", dma=True), dp.buf("w1", dma=True)]
    ps = [P.psum(f"aps{i}", [128, 512], F32) for i in range(2)]
    Bp = [dp.buf("p0"), dp.buf("p1")]
    dp.dma("sync", lambda e: e.dma_start(out=cT[:], in_=cT_d), Bc, writes=[Bc])
    dp.dma("sync", lambda e: e.dma_start(out=bs[:], in_=b_d), Bb, writes=[Bb])
    dp.op("scalar", lambda e: e.activation(out=sg[:], in_=cT[:], func=AF.Sigmoid), reads=[Bc], writes=[Bc])
    dp.op("vector", lambda e: e.tensor_tensor(out=cT[:], in0=cT[:], in1=sg[:], op=ALU.mult), writes=[Bc])
    nblk = 2 * A_NCH // 2
    for blk in range(nblk):
        l = blk // (A_NCH // 2)
        c0 = (blk % (A_NCH // 2)) * 256
        i = blk % 2
        dp.dma("sync" if i == 0 else "gpsimd", lambda e, l=l, c0=c0, i=i: e.dma_start(
            out=wb[i][:], in_=W_d[l, :, c0:c0 + 256].rearrange("(k p) n -> p k n", p=128)), Bw[i], writes=[Bw[i]])
        for oc in range(2):
            ch = blk * 2 + oc
            dp.pe([lambda e, kk=kk, oc=oc, i=i: e.matmul(ps[oc][:, 0:2], wb[i][:, kk, oc * 128:(oc + 1) * 128], cT[:, kk, :],
                                                         start=(kk == 0), stop=(kk == KC - 1)) for kk in range(KC)],
                  reads=[Bw[i], Bc], writes=[Bp[oc]])
            dp.op("vector", lambda e, ch=ch, oc=oc: e.tensor_tensor(out=osb[:, ch, :], in0=ps[oc][:, 0:2],
                                                                    in1=bs[:, ch:ch + 1].to_broadcast([128, 2]), op=ALU.add),
                  reads=[Bp[oc], Bb], writes=[Bo])
    dp.dma("sync", lambda e: e.dma_start(out=o_d, in_=osb[:]), Bo, reads=[Bo])
    P.wait("sync", Bo.dma_sem, Bo.dma_sem.n)
    P.run()
    return nc


_NC = {}
_DBG = None


def _get(name, fn):
    if name not in _NC:
        _NC[name] = fn()
    return _NC[name]


def _fm(v):
    return np.ascontiguousarray(np.asarray(v, np.float32).reshape(-1, 128).T)


def _run(nc, in_maps):
    res = run_bass_kernel_spmd(nc, in_maps, core_ids=list(range(len(in_maps))))
    return res.results


def kernel(x, c, ada_w, ada_b, mix_ln_g, mix_ln_b, ffn_ln_g, ffn_ln_b, ffn_w_in, ffn_w_out,
           ml_w_in, ml_b_i, ml_b_f, ml_norm_g, ml_w_out,
           rw_mu, rw_w_r, rw_w_k, rw_w_v, rw_w0, rw_w1, rw_w2, rw_a0, rw_a1, rw_a2, rw_g1, rw_g2,
           rw_k_k, rw_k_a, rw_r_k, rw_lnx_g, rw_lnx_b, rw_w_o):
    f32 = np.float32
    x = np.asarray(x, f32)
    xf = x.reshape(8192, D)
    ca = np.ascontiguousarray
    cT = ca(np.asarray(c, f32).T.reshape(KC, 128, 2).transpose(1, 0, 2))
    ims = []
    for j in range(NCORE):
        sl = slice(j * 3072, (j + 1) * 3072)
        bj = np.concatenate([np.asarray(ada_b[l, sl], f32).reshape(A_NCH, 128).T for l in range(2)], axis=1)
        ims.append({"cT": cT, "ada_w": ca(np.asarray(ada_w[:, :, sl], f32)), "ada_b": ca(bj)})
    ra = _run(_get("a", build_phase_a), ims)
    mod = np.zeros((2, 2, 6 * D), f32)
    for j in range(NCORE):
        o = ra[j]["modT"]
        for l in range(2):
            mod[l, :, j * 3072:(j + 1) * 3072] = o[:, l * A_NCH:(l + 1) * A_NCH, :].transpose(2, 1, 0).reshape(2, 3072)
    del ims, ra
    if _DBG is not None:
        _DBG['mod'] = mod.copy()
    mparts = lambda l, b: [mod[l, b, i * D:(i + 1) * D] for i in range(6)]
    xT = [ca(xf[i * NTOK:(i + 1) * NTOK].T) for i in range(NCORE)]
    w_in0 = ca(np.asarray(ml_w_in[0], f32))
    ims = []
    for i in range(NCORE):
        sh_m, sc_m = mparts(0, i // 4)[0:2]
        ims.append({"xT": xT[i], "w_in": w_in0, "modv": ca(np.concatenate([_fm(sh_m), _fm(sc_m)], axis=1))})
    rb = _run(_get("b", build_phase_b), ims)
    qkvT = np.concatenate([rb[i]["qkvT"] for i in range(NCORE)], axis=1)
    soT = np.concatenate([rb[i]["soT"] for i in range(NCORE)], axis=1)
    gT = np.concatenate([rb[i]["gT"] for i in range(NCORE)], axis=1)
    del ims, rb, w_in0
    ims = []
    for j in range(NCORE):
        q = qkvT[j * 256:(j + 1) * 256].reshape(256, 2, ML_S).transpose(1, 0, 2)
        k = qkvT[2048 + j * 256:2048 + (j + 1) * 256].reshape(256, 2, ML_S).transpose(1, 0, 2)
        v = qkvT[4096 + j * 512:4096 + (j + 1) * 512].reshape(512, 2, ML_S).transpose(1, 2, 0)
        so = soT[j * 512:(j + 1) * 512].reshape(512, 2, ML_S).transpose(1, 2, 0)
        ims.append({"qT": ca(q), "kT": ca(k), "ktm": ca(k.transpose(0, 2, 1)), "vtm": ca(v), "sotm": ca(so),
                    "gi": ca(gT[j].reshape(2, ML_S)), "gf": ca(gT[8 + j].reshape(2, ML_S)),
                    "gb": np.array([[ml_b_i[0, j], ml_b_f[0, j]]], f32),
                    "ng": ca(np.tile(np.asarray(ml_norm_g[0, j * 512:(j + 1) * 512], f32)[None, :], (128, 1)))})
    rc = _run(_get("c", build_phase_c), ims)
    hT = np.concatenate([rc[j]["hT"].transpose(1, 0, 2).reshape(512, 8192) for j in range(NCORE)], axis=0)
    if _DBG is not None:
        _DBG['qkvT'] = qkvT; _DBG['soT'] = soT; _DBG['gT'] = gT; _DBG['hT'] = hT
    del ims, rc, qkvT, soT, gT
    w_o0 = ca(np.asarray(ml_w_out[0], f32)); fwi = ca(np.asarray(ffn_w_in[0], f32)); fwo = ca(np.asarray(ffn_w_out[0], f32))
    ims = []
    for i in range(NCORE):
        sh_m, sc_m, gt_m, sh_f, sc_f, gt_f = mparts(0, i // 4)
        cols = [gt_m, mix_ln_g[0], mix_ln_b[0], sh_f, sc_f, gt_f, ffn_ln_g[0], ffn_ln_b[0]]
        ims.append({"xT": xT[i], "hT": ca(hT[:, i * NTOK:(i + 1) * NTOK]), "w_o": w_o0, "w_in": fwi, "w_out": fwo,
                    "modv": ca(np.concatenate([_fm(v) for v in cols], axis=1))})
    rd = _run(_get("d1", lambda: build_phase_d1("d1")), ims)
    x2T = [rd[i]["outT"] for i in range(NCORE)]
    if _DBG is not None:
        _DBG['x2T'] = [a.copy() for a in x2T]
    del ims, rd, hT, w_o0, fwi, fwo, xT
    g1p = np.zeros((D, 512), f32); g1p[:, :480] = rw_g1[0]
    g2p = np.zeros((512, D), f32); g2p[:480] = rw_g2[0]
    Wd2 = {"w_r": ca(np.asarray(rw_w_r[0], f32)), "w_k": ca(np.asarray(rw_w_k[0], f32)), "w_v": ca(np.asarray(rw_w_v[0], f32)),
           "w1": ca(np.asarray(rw_w1[0], f32)), "w2": ca(np.asarray(rw_w2[0], f32)), "a1": ca(np.asarray(rw_a1[0], f32)),
           "a2": ca(np.asarray(rw_a2[0], f32)), "g1": g1p, "g2": g2p}
    ims = []
    for i in range(NCORE):
        sh_m, sc_m = mparts(1, i // 4)[0:2]
        first = (i % 4 == 0)
        xprev = np.zeros((128, KC), f32) if first else ca(x2T[i - 1][:, NTOK - 1].reshape(KC, 128).T)
        cols = [_fm(sh_m), _fm(sc_m)] + [_fm(rw_mu[0, jj]) for jj in range(6)] + [np.zeros((128, 6 * KC), f32),
                                                                                  _fm(rw_w0[0]), _fm(rw_a0[0]),
                                                                                  np.full((128, 1), 0.0 if first else 1.0, f32)]
        im = {"xT": x2T[i], "xprev": xprev, "modv": ca(np.concatenate(cols, axis=1))}
        im.update(Wd2)
        ims.append(im)
    r2 = _run(_get("d2", build_phase_d2), ims)
    cat = lambda n: np.concatenate([r2[i][n] for i in range(NCORE)], axis=1)
    rT, kT, vT, wT, aT = cat("rT"), cat("kT"), cat("vT"), cat("wT"), cat("aT")
    gTs = [r2[i]["gT"] for i in range(NCORE)]
    if _DBG is not None:
        _DBG.update(rT=rT, kT=kT, vT=vT, wT=wT, aT=aT, gTs=gTs)
    del ims, r2, Wd2
    ims = []
    for j in range(NCORE):
        sl = slice(j * 512, (j + 1) * 512)
        hv = lambda v: np.asarray(v, f32).reshape(-1)[sl].reshape(8, 64).T
        pv = np.concatenate([hv(rw_k_k[0]), hv(rw_k_a[0]), np.zeros((64, 8), f32), hv(rw_r_k[0]), hv(rw_lnx_g[0]), hv(rw_lnx_b[0])], axis=1)
        ims.append({"rT": ca(rT[sl]), "kT": ca(kT[sl]), "vT": ca(vT[sl]), "wT": ca(wT[sl]), "aT": ca(aT[sl]), "pv": ca(pv)})
    re_ = _run(_get("e", build_phase_e), ims)
    yT = np.concatenate([re_[j]["yT"] for j in range(NCORE)], axis=0)
    if _DBG is not None:
        _DBG['yT'] = yT
    del ims, re_, rT, kT, vT, wT, aT
    w_o1 = ca(np.asarray(rw_w_o[0], f32)); fwi = ca(np.asarray(ffn_w_in[1], f32)); fwo = ca(np.asarray(ffn_w_out[1], f32))
    ims = []
    for i in range(NCORE):
        sh_m, sc_m, gt_m, sh_f, sc_f, gt_f = mparts(1, i // 4)
        cols = [gt_m, mix_ln_g[1], mix_ln_b[1], sh_f, sc_f, gt_f, ffn_ln_g[1], ffn_ln_b[1]]
        ims.append({"xT": x2T[i], "yT": ca(yT[:, i * NTOK:(i + 1) * NTOK]), "gT": gTs[i], "w_o": w_o1, "w_in": fwi, "w_out": fwo,
                    "modv": ca(np.concatenate([_fm(v) for v in cols], axis=1))})
    rf = _run(_get("f", lambda: build_phase_d1("f")), ims)
    out = np.concatenate([rf[i]["outT"].T for i in range(NCORE)], axis=0).reshape(2, ML_S, D)
    return np.ascontiguousarray(out.astype(f32))
```

```python
import numpy as np
import ml_dtypes
from contextlib import ExitStack
import concourse.bass as bass
import concourse.mybir as mybir
from concourse.bass_utils import run_bass_kernel_spmd

F32 = mybir.dt.float32
BF16 = mybir.dt.bfloat16
AF = mybir.ActivationFunctionType
ALU = mybir.AluOpType
AX = mybir.AxisListType
NPBF = ml_dtypes.bfloat16

D = 4096
KC = D // 128
NCORE = 8
NTOK = 1024
TT = 512
NT = NTOK // TT
FF = 11008
ML_IN = 12304
ALPHA = 4 ** 0.25
LN_EPS = 1e-5
ENGS = ("tensor", "vector", "scalar", "gpsimd", "sync")


class Sem:
    def __init__(self, h, name):
        self.h = h
        self.name = name
        self.n = 0


class Prog:
    def __init__(self, nc):
        self.nc = nc
        self.q = {e: [] for e in ENGS}
        self.waited = {e: {} for e in ENGS}
        self.stack = ExitStack()
        self.serial = set()
        self.last = {}
        self.chain_sem = {}

    def sem(self, name):
        return Sem(self.stack.enter_context(self.nc.semaphore(name)), name)

    def sbuf(self, name, shape, dt):
        return self.stack.enter_context(self.nc.sbuf_tensor(name, shape, dt))

    def psum(self, name, shape, dt):
        return self.stack.enter_context(self.nc.psum_tensor(name, shape, dt))

    def wait(self, eng, sem, val):
        if val <= 0:
            return
        w = self.waited[eng]
        if w.get(sem.name, 0) >= val:
            return
        w[sem.name] = val
        self.q[eng].append(lambda e, s=sem.h, v=val: e.wait_ge(s, v))

    def op(self, eng, fn, waits=(), inc=None, dma=False, selfwait=True):
        for s, v in waits:
            self.wait(eng, s, v)
        if eng in self.serial and not dma:
            if eng in self.last and selfwait:
                self.wait(eng, *self.last[eng])
            if inc is None:
                if eng not in self.chain_sem:
                    self.chain_sem[eng] = self.sem("chain_" + eng)
                inc = self.chain_sem[eng]
            self.last[eng] = (inc, inc.n + 1)
        if inc is None:
            self.q[eng].append(lambda e, f=fn: f(e))
            return None
        k = 16 if dma else 1
        inc.n += k
        self.q[eng].append(lambda e, f=fn, sh=inc.h, kk=k: f(e).then_inc(sh, kk))
        return inc.n

    def tok(self, eng):
        return self.last[eng]

    def run(self):
        with self.nc.Block() as block:
            for ename in ENGS:
                lst = self.q[ename]

                def body(e, lst=lst):
                    for f in lst:
                        f(e)

                getattr(block, ename)(body)
        self.stack.close()


class Ring:
    def __init__(self, P, name, n, shape=None, dt=None, tiles=None):
        self.n = n
        self.tiles = tiles if tiles is not None else [P.sbuf(f"{name}{i}", shape, dt) for i in range(n)]
        self.filled = [P.sem(f"{name}_f{i}") for i in range(n)]
        self.freed = [P.sem(f"{name}_e{i}") for i in range(n)]
        self.i = 0

    def next(self):
        s = self.i % self.n
        self.i += 1
        return s

    def free_wait(self, s):
        return (self.freed[s], self.freed[s].n)

    def fill_wait(self, s):
        return (self.filled[s], self.filled[s].n)


class Slots:
    def __init__(self, P, name, n, shape, dt):
        self.n = n
        self.tiles = [P.sbuf(f"{name}{i}", shape, dt) for i in range(n)]
        self.rel = [[] for _ in range(n)]
        self.sem_in = [P.sem(f"{name}_i{i}") for i in range(n)]
        self.sem_out = [P.sem(f"{name}_o{i}") for i in range(n)]
        self.i = 0

    def get(self):
        s = self.i % self.n
        self.i += 1
        return s, self.tiles[s], list(self.rel[s])

    def release(self, s, toks):
        self.rel[s] = [t for t in toks if t is not None]


class Tok:
    WB_ELEMS = 8192
    NBUF = 3
    KG = 16

    def __init__(self, nc, P, hid_chunks=86, rings=True, kg=None):
        self.nc, self.P = nc, P
        if kg is not None:
            self.KG = kg
            self.WB_ELEMS = kg * 512
        self.wb = [P.sbuf(f"wb{i}", [128, self.WB_ELEMS], BF16) for i in range(self.NBUF)]
        self.act = P.sbuf("act", [128, KC, TT], BF16)
        self.hid = P.sbuf("hid", [128, hid_chunks, TT], BF16) if hid_chunks else None
        self.ps = [P.psum(f"ps{i}", [128, 512], F32) for i in range(8)]
        self.pbank = Ring(P, "pb", 8, tiles=self.ps)
        self.gi = 0
        self.mi = 0
        self.s_wl = [P.sem(f"s_wl{i}") for i in range(self.NBUF)]
        self.s_wf = P.sem("s_wf")
        self.wcount = 0
        self.wrel = {}
        if rings:
            self.ostf = Ring(P, "ostf", 2, [128, TT], F32)
            self.ostb = Ring(P, "ostb", 2, [128, TT], BF16)
            self.xs = Ring(P, "xs", 3, [128, TT], F32)
        self.act_rel = []
        self.hid_rel = []
        self.evi = 0

    def gbank(self):
        b = self.gi % 6
        self.gi += 1
        return b

    def mbank(self):
        b = 6 + self.mi % 2
        self.mi += 1
        return b

    def ev_eng(self):
        self.evi += 1
        return "vector" if self.evi % 2 else "scalar"

    def load_w(self, Wv, k0, kn, segs):
        P = self.P
        idx = self.wcount
        self.wcount += 1
        b = idx % self.NBUF
        ncols = sum(n for _, n in segs)
        assert kn * ncols <= self.WB_ELEMS
        view = self.wb[b][:, 0:kn * ncols].rearrange("p (k c) -> p k c", c=ncols)
        waits = []
        if idx - self.NBUF in self.wrel:
            waits.append(self.wrel[idx - self.NBUF])
        off = 0
        val = None
        for (c0, n) in segs:
            src = Wv[k0 * 128:(k0 + kn) * 128, c0:c0 + n].rearrange("(k p) n -> p k n", p=128)
            dst = view[:, :, off:off + n]
            val = P.op("gpsimd", lambda e, dst=dst, src=src: e.dma_start(out=dst, in_=src),
                       waits=waits, inc=self.s_wl[b], dma=True)
            waits = []
            off += n
        return idx, view, (self.s_wl[b], val)

    def gemm(self, Wv, K, blocks, ep, act=None, m_last=128, act_waits=()):
        P = self.P
        act = self.act if act is None else act
        kct = K // 128
        kgs = []
        k0 = 0
        while k0 < kct:
            kn = min(self.KG, kct - k0)
            kgs.append((k0, kn))
            k0 += kn
        loads = [(bi, gi) for bi in range(len(blocks)) for gi in range(len(kgs))]
        pending = {}

        def issue(li):
            bi, gi = loads[li]
            k0, kn = kgs[gi]
            pending[li] = self.load_w(Wv, k0, kn, blocks[bi])

        issue(0)
        if len(loads) > 1:
            issue(1)
        li = 0
        last_full = None
        for bi, segs in enumerate(blocks):
            ncols = sum(n for _, n in segs)
            nch = (ncols + 127) // 128
            banks = [self.gbank() for _ in range(nch)]
            for gi, (k0, kn) in enumerate(kgs):
                if li + 2 < len(loads):
                    issue(li + 2)
                idx, view, wwait = pending.pop(li)
                li += 1
                for oc in range(nch):
                    bank = banks[oc]
                    M = min(128, ncols - oc * 128)
                    for kk in range(kn):
                        first = gi == 0 and kk == 0
                        last = gi == len(kgs) - 1 and kk == kn - 1
                        waits = []
                        if kk == 0:
                            waits.append(wwait)
                            waits.extend(act_waits)
                            if first:
                                waits.append(self.pbank.free_wait(bank))
                        fn = lambda e, bank=bank, M=M, view=view, kk=kk, oc=oc, k0=k0, first=first, last=last: e.matmul(
                            self.ps[bank][0:M, 0:TT], view[:, kk, oc * 128:oc * 128 + M], act[:, k0 + kk, :],
                            start=first, stop=last)
                        if last:
                            P.op("tensor", fn, waits=waits, inc=self.pbank.filled[bank])
                            if oc == nch - 1:
                                self.wrel[idx] = self.pbank.fill_wait(bank)
                        elif kk == kn - 1 and oc == nch - 1:
                            v = P.op("tensor", fn, waits=waits, inc=self.s_wf)
                            self.wrel[idx] = (self.s_wf, v)
                        else:
                            P.op("tensor", fn, waits=waits)
            last_full = self.pbank.fill_wait(banks[-1])
            for oc in range(nch):
                bank = banks[oc]
                ep(bi, oc, bank, self.pbank.fill_wait(bank))
        return last_full

    def gemm_multi(self, Wv, K, blocks, ep, acts, act_waits=()):
        P = self.P
        kct = K // 128
        assert kct <= self.KG
        pending = {}

        def issue(li):
            pending[li] = self.load_w(Wv, 0, kct, blocks[li])

        issue(0)
        if len(blocks) > 1:
            issue(1)
        last_full = None
        for bi, segs in enumerate(blocks):
            if bi + 2 < len(blocks):
                issue(bi + 2)
            idx, view, wwait = pending.pop(bi)
            ncols = sum(n for _, n in segs)
            nch = (ncols + 127) // 128
            for ti, act in enumerate(acts):
                banks = [self.gbank() for _ in range(nch)]
                for oc in range(nch):
                    bank = banks[oc]
                    M = min(128, ncols - oc * 128)
                    for kk in range(kct):
                        waits = []
                        if kk == 0:
                            waits = [wwait] + list(act_waits) + [self.pbank.free_wait(bank)]
                        fn = lambda e, bank=bank, M=M, view=view, kk=kk, oc=oc, act=act: e.matmul(
                            self.ps[bank][0:M, 0:TT], view[:, kk, oc * 128:oc * 128 + M], act[:, kk, :],
                            start=(kk == 0), stop=(kk == kct - 1))
                        if kk == kct - 1:
                            P.op("tensor", fn, waits=waits, inc=self.pbank.filled[bank])
                        else:
                            P.op("tensor", fn, waits=waits)
                last_full = self.pbank.fill_wait(banks[-1])
                if ti == len(acts) - 1:
                    self.wrel[idx] = last_full
                for oc in range(nch):
                    ep(bi, oc, banks[oc], self.pbank.fill_wait(banks[oc]), ti)
        return last_full


def _dram(nc, name, shape, dt, kind):
    return nc.dram_tensor(name, list(shape), dt, kind=kind).ap()


def _finish(P, out_waits):
    for s, v in out_waits:
        P.wait("sync", s, v)


class OutTracker:
    def __init__(self):
        self.sems = {}

    def add(self, sem):
        self.sems[sem.name] = sem

    def waits(self):
        return [(s, s.n) for s in self.sems.values()]


def modulate(T, xT, t, onepsc, sh, extra_waits=(), act=None):
    P = T.P
    act = T.act if act is None else act
    for c in range(KC):
        s = T.xs.next()
        xt = T.xs.tiles[s]
        P.op("sync", lambda e, xt=xt, c=c: e.dma_start(out=xt[:], in_=xT[c * 128:(c + 1) * 128, t * TT:(t + 1) * TT]),
             waits=[T.xs.free_wait(s)], inc=T.xs.filled[s], dma=True)
        waits = [T.xs.fill_wait(s)]
        if c == 0:
            waits += list(T.act_rel) + list(extra_waits)
        P.op("scalar", lambda e, xt=xt, c=c, act=act: e.activation(out=act[:, c, :], in_=xt[:], func=AF.Identity,
                                                          bias=sh[:, c:c + 1], scale=onepsc[:, c:c + 1]),
             waits=waits, inc=T.xs.freed[s])
    T.act_ready = (T.xs.freed[s], T.xs.freed[s].n)


def build_phase_b():
    nc = bass.Bass("TRN2", target_bir_lowering=False)
    xT = _dram(nc, "xT", [D, NTOK], F32, "ExternalInput")
    W = _dram(nc, "w_in", [D, ML_IN], F32, "ExternalInput")
    modv = _dram(nc, "modv", [128, 2 * KC], F32, "ExternalInput")
    qkvT = _dram(nc, "qkvT", [8192, NTOK], BF16, "ExternalOutput")
    soT = _dram(nc, "soT", [D, NTOK], F32, "ExternalOutput")
    gT = _dram(nc, "gT", [16, NTOK], F32, "ExternalOutput")
    P = Prog(nc)
    T = Tok(nc, P, hid_chunks=0, kg=32)
    act1 = P.sbuf("act1", [128, KC, TT], BF16)
    acts = [T.act, act1]
    mod_sb = P.sbuf("mod_sb", [128, 2 * KC], F32)
    onepsc = P.sbuf("onepsc", [128, KC], F32)
    s_c = P.sem("s_const")
    v = P.op("sync", lambda e: e.dma_start(out=mod_sb[:], in_=modv), inc=s_c, dma=True)
    s_c2 = P.sem("s_const2")
    v2 = P.op("vector", lambda e: e.tensor_scalar_add(onepsc[:], mod_sb[:, KC:2 * KC], 1.0), waits=[(s_c, v)], inc=s_c2)
    sh = mod_sb
    outs = OutTracker()
    for t in range(NT):
        modulate(T, xT, t, onepsc, sh, extra_waits=[(s_c2, v2)], act=acts[t])
    blocks = [[(c0, 512)] for c0 in range(0, 12288, 512)] + [[(12288, 16)]]

    def ep(bi, oc, bank, fw, t):
        col = bi * 512 + oc * 128
        ps = T.ps[bank]
        eng = T.ev_eng()
        if bi < 16:
            ring = T.ostb
            s = ring.next()
            st = ring.tiles[s]
            if bi < 4 or bi >= 8:
                if eng == "vector":
                    fn = lambda e: e.tensor_copy(st[:], ps[:, 0:TT])
                else:
                    fn = lambda e: e.copy(st[:], ps[:, 0:TT])
            else:
                if eng == "vector":
                    fn = lambda e: e.tensor_scalar_mul(st[:], ps[:, 0:TT], 0.0625)
                else:
                    fn = lambda e: e.mul(st[:], ps[:, 0:TT], 0.0625)
            P.op(eng, fn, waits=[fw, ring.free_wait(s)], inc=T.pbank.freed[bank])
            P.op("sync", lambda e: e.dma_start(out=qkvT[col:col + 128, t * TT:(t + 1) * TT], in_=st[:]),
                 waits=[(T.pbank.freed[bank], T.pbank.freed[bank].n)], inc=ring.freed[s], dma=True)
            outs.add(ring.freed[s])
        elif bi < 24:
            ring = T.ostf
            s = ring.next()
            st = ring.tiles[s]
            P.op("scalar", lambda e: e.activation(out=st[:], in_=ps[:, 0:TT], func=AF.Sigmoid),
                 waits=[fw, ring.free_wait(s)], inc=T.pbank.freed[bank])
            c2 = col - 8192
            P.op("sync", lambda e: e.dma_start(out=soT[c2:c2 + 128, t * TT:(t + 1) * TT], in_=st[:]),
                 waits=[(T.pbank.freed[bank], T.pbank.freed[bank].n)], inc=ring.freed[s], dma=True)
            outs.add(ring.freed[s])
        else:
            ring = T.ostf
            s = ring.next()
            st = ring.tiles[s]
            P.op("vector", lambda e: e.tensor_copy(st[0:16, :], ps[0:16, 0:TT]),
                 waits=[fw, ring.free_wait(s)], inc=T.pbank.freed[bank])
            P.op("sync", lambda e: e.dma_start(out=gT[:, t * TT:(t + 1) * TT], in_=st[0:16, :]),
                 waits=[(T.pbank.freed[bank], T.pbank.freed[bank].n)], inc=ring.freed[s], dma=True)
            outs.add(ring.freed[s])

    T.gemm_multi(W, D, blocks, ep, acts, act_waits=[T.act_ready])
    _finish(P, outs.waits())
    P.run()
    return nc


ML_S = 4096
ML_NCH = ML_S // 128
ML_EPS = 1e-6


def build_phase_c(debug=False):
    nc = bass.Bass("TRN2", target_bir_lowering=False)
    if debug:
        dbg1 = _dram(nc, "dbg1", [128, 96], F32, "ExternalOutput")
        dbg2 = _dram(nc, "dbg2", [128, 512], F32, "ExternalOutput")
        dbg3 = _dram(nc, "dbg3", [128, 512], F32, "ExternalOutput")
        dbg4 = _dram(nc, "dbg4", [128, 128], BF16, "ExternalOutput")
        dbg5 = _dram(nc, "dbg5", [128, 8], F32, "ExternalOutput")
        dbg6 = _dram(nc, "dbg6", [1, ML_S], F32, "ExternalOutput")
    qT = _dram(nc, "qT", [2, 256, ML_S], BF16, "ExternalInput")
    kT = _dram(nc, "kT", [2, 256, ML_S], BF16, "ExternalInput")
    ktm = _dram(nc, "ktm", [2, ML_S, 256], BF16, "ExternalInput")
    vtm = _dram(nc, "vtm", [2, ML_S, 512], BF16, "ExternalInput")
    sotm = _dram(nc, "sotm", [2, ML_S, 512], F32, "ExternalInput")
    gi_d = _dram(nc, "gi", [2, ML_S], F32, "ExternalInput")
    gf_d = _dram(nc, "gf", [2, ML_S], F32, "ExternalInput")
    gb_d = _dram(nc, "gb", [1, 2], F32, "ExternalInput")
    ng_d = _dram(nc, "ng", [128, 512], F32, "ExternalInput")
    hT = _dram(nc, "hT", [2, 512, ML_S], BF16, "ExternalOutput")
    P = Prog(nc)
    P.serial = {"vector", "scalar", "gpsimd"}
    qs = P.sbuf("qs", [128, 2, ML_S], BF16)
    ks = P.sbuf("ks", [128, 2, ML_S], BF16)
    ktm_s = P.sbuf("ktm_s", [128, ML_NCH, 256], BF16)
    va = P.sbuf("va", [128, ML_NCH, 514], BF16)
    hts_r = [P.sbuf(f"hts{i}", [128, 4, 1024], BF16) for i in range(2)]
    ng = P.sbuf("ng_s", [128, 512], F32)
    gb = P.sbuf("gb_s", [1, 2], F32)
    mask = P.sbuf("mask", [128, 128], F32)
    ident = P.sbuf("ident", [128, 128], F32)
    ones_row = P.sbuf("ones_row", [1, 128], F32)
    one11 = P.sbuf("one11", [1, 1], F32)
    g_in = [P.sbuf(f"grow{i}", [1, ML_S], F32) for i in range(2)]
    t2 = g_in[1]
    cF = P.sbuf("g_cF", [1, ML_S], F32)
    bv = g_in[0]
    Mr = P.sbuf("g_M", [1, ML_S], F32)
    wr = bv
    fr = cF
    ones_bc = one11[0:1, 0:1].to_broadcast([1, ML_S])
    rcr = P.sbuf("g_rc", [1, ML_NCH], F32)
    wcol = P.sbuf("wcol", [128, ML_NCH], F32)
    fcol = P.sbuf("fcol", [128, ML_NCH], F32)
    rcb = P.sbuf("rcb", [128, ML_NCH], F32)
    Cs = P.sbuf("Cs", [128, 2, 514], F32)
    Cb = P.sbuf("Cb", [128, 2, 514], BF16)
    PT = P.sbuf("PT", [128, 128], BF16)
    kw = P.sbuf("kw", [128, 256], BF16)
    junk = P.sbuf("junk", [128, 512], F32)
    hn = P.sbuf("hn", [128, 512], F32)
    hg = P.sbuf("hg", [128, 512], F32)
    sm = P.sbuf("sm", [128, 8], F32)
    so_ring = Ring(P, "so", 3, [128, 512], F32)
    psA = P.psum("psA", [128, 512], F32)
    psB = P.psum("psB", [128, 512], F32)
    psC = P.psum("psC", [128, 512], F32)
    psD = [P.psum(f"psD{i}", [128, 512], F32) for i in range(2)]
    psT = P.psum("psT", [128, 4, 128], F32)
    psG = P.psum("psG", [128, 512], F32)

    S = {n: P.sem("m_" + n) for n in ("ld", "cst", "g", "gp", "gc", "st", "pt", "rs", "cb", "nd", "kw", "dc", "cs",
                                      "ss", "hn", "hg", "tr", "cp", "out", "sq1", "sq2")}
    P.op("gpsimd", lambda e: e.memset(mask[:], 1.0))
    P.op("gpsimd", lambda e: e.affine_select(out=mask[:], in_=mask[:], pattern=[[1, 128]], compare_op=ALU.is_ge,
                                             fill=0.0, base=0, channel_multiplier=-1))
    P.op("gpsimd", lambda e: e.memset(ident[:], 1.0))
    P.op("gpsimd", lambda e: e.affine_select(out=ident[:], in_=ident[:], pattern=[[1, 128]], compare_op=ALU.is_equal,
                                             fill=0.0, base=0, channel_multiplier=-1))
    P.op("gpsimd", lambda e: e.memset(ones_row[:], 1.0))
    P.op("gpsimd", lambda e: e.memset(one11[:], 1.0))
    P.op("gpsimd", lambda e: e.memset(va[:, :, 512:514], 1.0), inc=S["cst"])
    v_cst = S["cst"].n
    P.op("sync", lambda e: e.dma_start(out=ng[:], in_=ng_d), inc=S["ld"], dma=True)
    P.op("sync", lambda e: e.dma_start(out=gb[:], in_=gb_d), inc=S["ld"], dma=True)
    P.op("vector", lambda e: e.tensor_scalar_mul(gb[:, 0:1], gb[:, 0:1], 1.0 / 15.0), waits=[(S["ld"], S["ld"].n)])
    P.op("vector", lambda e: e.tensor_scalar_mul(gb[:, 1:2], gb[:, 1:2], -1.0))
    t_gb = P.tok("vector")

    n = 0
    for b in range(2):
        wprev = [(S["cp"], S["cp"].n), (S["cs"], S["cs"].n), (S["out"], S["out"].n)] if b > 0 else []
        P.op("sync", lambda e, b=b: e.dma_start(out=qs[:], in_=qT[b].rearrange("(c p) s -> p c s", p=128)),
             waits=wprev, inc=S["ld"], dma=True)
        P.op("sync", lambda e, b=b: e.dma_start(out=ks[:], in_=kT[b].rearrange("(c p) s -> p c s", p=128)),
             inc=S["ld"], dma=True)
        P.op("sync", lambda e, b=b: e.dma_start(out=ktm_s[:], in_=ktm[b].rearrange("(c p) d -> p c d", p=128)),
             inc=S["ld"], dma=True)
        P.op("sync", lambda e, b=b: e.dma_start(out=va[:, :, 0:512], in_=vtm[b].rearrange("(c p) d -> p c d", p=128)),
             inc=S["ld"], dma=True)
        P.op("sync", lambda e, b=b: e.dma_start(out=g_in[0][:], in_=gi_d[b:b + 1, :]), inc=S["ld"], dma=True)
        P.op("sync", lambda e, b=b: e.dma_start(out=g_in[1][:], in_=gf_d[b:b + 1, :]), inc=S["ld"], dma=True)
        v_ld = S["ld"].n
        gi_t, gf_t = g_in
        P.op("scalar", lambda e: e.activation(out=t2[:], in_=gf_t[:], func=AF.Exp, bias=gb[:, 1:2], scale=-1.0),
             waits=[(S["ld"], v_ld), (S["cs"], S["cs"].n), (S["hn"], S["hn"].n), t_gb])
        P.op("scalar", lambda e: e.activation(out=t2[:], in_=t2[:], func=AF.Ln, bias=1.0, scale=1.0))
        P.op("scalar", lambda e: e.activation(out=gi_t[:], in_=gi_t[:], func=AF.Tanh, bias=gb[:, 0:1], scale=1.0 / 15.0),
             inc=S["g"])
        vg = S["g"].n
        P.op("vector", lambda e: e.tensor_tensor_scan(out=cF[:], data0=ones_bc, data1=t2[:], initial=0.0,
                                                      op0=ALU.mult, op1=ALU.add), waits=[(S["g"], vg), (S["cst"], v_cst)])
        P.op("vector", lambda e: e.scalar_tensor_tensor(out=bv[:], in0=gi_t[:], scalar=15.0, in1=cF[:],
                                                        op0=ALU.mult, op1=ALU.add))
        P.op("vector", lambda e: e.tensor_tensor_scan(out=Mr[:], data0=ones_bc, data1=bv[:], initial=0.0,
                                                      op0=ALU.mult, op1=ALU.max))
        Mv = Mr[:].rearrange("p (c j) -> p c j", j=128)
        Mend = Mv[:, :, 127:128]
        P.op("vector", lambda e: e.tensor_tensor(out=wr[:].rearrange("p (c j) -> p c j", j=128),
                                                 in0=bv[:].rearrange("p (c j) -> p c j", j=128),
                                                 in1=Mend.to_broadcast([1, ML_NCH, 128]), op=ALU.subtract))
        P.op("vector", lambda e: e.tensor_tensor(out=fr[:].rearrange("p (c j) -> p c j", j=128),
                                                 in0=cF[:].rearrange("p (c j) -> p c j", j=128),
                                                 in1=Mend.to_broadcast([1, ML_NCH, 128]), op=ALU.subtract))
        P.op("vector", lambda e: e.memset(rcr[:], 0.0))
        P.op("vector", lambda e: e.tensor_tensor(out=rcr[:, 1:ML_NCH], in0=Mr[:, 127:ML_S - 128:128],
                                                 in1=Mr[:, 255:ML_S:128], op=ALU.subtract), inc=S["gp"])
        vgp = S["gp"].n
        P.op("scalar", lambda e: e.activation(out=wr[:], in_=wr[:], func=AF.Exp), waits=[(S["gp"], vgp)])
        P.op("scalar", lambda e: e.activation(out=fr[:], in_=fr[:], func=AF.Exp))
        P.op("scalar", lambda e: e.activation(out=rcr[:], in_=rcr[:], func=AF.Exp), inc=S["g"])
        vg2 = S["g"].n
        for c in range(ML_NCH):
            P.op("tensor", lambda e, c=c: e.matmul(psG[:, c:c + 1], wr[0:1, c * 128:(c + 1) * 128], one11[0:1, 0:1],
                                                   start=True, stop=True),
                 waits=[(S["g"], vg2), (S["gc"], S["gc"].n)] if c == 0 else ())
            P.op("tensor", lambda e, c=c: e.matmul(psG[:, 32 + c:33 + c], fr[0:1, c * 128:(c + 1) * 128], one11[0:1, 0:1],
                                                   start=True, stop=True))
        P.op("tensor", lambda e: e.matmul(psG[:, 64:96], ones_row[0:1, 0:128], rcr[0:1, 0:ML_NCH], start=True, stop=True),
             inc=S["gp"])
        vgp2 = S["gp"].n
        P.op("vector", lambda e: e.tensor_copy(wcol[:], psG[:, 0:32]), waits=[(S["gp"], vgp2)])
        P.op("vector", lambda e: e.tensor_copy(fcol[:], psG[:, 32:64]))
        P.op("vector", lambda e: e.tensor_copy(rcb[:], psG[:, 64:96]))
        P.op("vector", lambda e: e.memset(Cs[:], 0.0), inc=S["gc"])
        v_gc = S["gc"].n
        for c in range(ML_NCH):
            cs_ = slice(c * 128, (c + 1) * 128)
            sl = so_ring.next()
            sot = so_ring.tiles[sl]
            P.op("sync", lambda e, b=b, cs_=cs_, sot=sot: e.dma_start(out=sot[:], in_=sotm[b, cs_, :]),
                 waits=[(S["hg"], n - 2)], inc=so_ring.filled[sl], dma=True)
            for dc in range(2):
                P.op("tensor", lambda e, dc=dc, cs_=cs_: e.matmul(psA[:, 0:128], ks[:, dc, cs_], qs[:, dc, cs_],
                                                                  start=(dc == 0), stop=(dc == 1)),
                     waits=[(S["ld"], v_ld), (S["pt"], n)] if dc == 0 else (),
                     inc=S["st"] if dc == 1 else None)
            P.op("vector", lambda e, c=c: e.tensor_scalar_mul(Cs[:], Cs[:], rcb[:, c:c + 1]),
                 waits=[(S["gc"], v_gc)], inc=S["rs"])
            P.op("vector", lambda e, c=c: e.scalar_tensor_tensor(out=PT[:], in0=psA[:, 0:128], scalar=wcol[:, c:c + 1],
                                                                 in1=mask[:], op0=ALU.mult, op1=ALU.mult),
                 waits=[(S["st"], n + 1), (S["nd"], n), (S["dc"], n)], inc=S["pt"])
            P.op("scalar", lambda e: e.copy(Cb[:], Cs[:]), waits=[(S["rs"], n + 1), (S["nd"], n)], inc=S["cb"])
            P.op("scalar", lambda e, c=c: e.activation(out=kw[:], in_=ktm_s[:, c, :], func=AF.Copy, scale=wcol[:, c:c + 1]),
                 waits=[(S["ld"], v_ld), (S["gc"], v_gc), (S["dc"], n)], inc=S["kw"])
            P.op("tensor", lambda e, c=c: e.matmul(psB[:, :], PT[:], va[:, c, 0:512], start=True, stop=False),
                 waits=[(S["pt"], n + 1), (S["cb"], n + 1), (S["hn"], n), (S["ss"], n), (S["cst"], v_cst)])
            for dc in range(2):
                P.op("tensor", lambda e, dc=dc, cs_=cs_: e.matmul(psB[:, :], qs[:, dc, cs_], Cb[:, dc, 0:512],
                                                                  start=False, stop=(dc == 1)))
            P.op("tensor", lambda e, c=c: e.matmul(psC[:, 0:1], PT[:], va[:, c, 512:513], start=True, stop=False))
            for dc in range(2):
                P.op("tensor", lambda e, dc=dc, cs_=cs_: e.matmul(psC[:, 0:1], qs[:, dc, cs_], Cb[:, dc, 512:513],
                                                                  start=False, stop=(dc == 1)),
                     inc=S["nd"] if dc == 1 else None)
            for dc in range(2):
                P.op("tensor", lambda e, dc=dc, c=c: e.matmul(psD[dc][:, :], kw[:, dc * 128:(dc + 1) * 128], va[:, c, 0:512],
                                                              start=True, stop=True),
                     waits=[(S["kw"], n + 1), (S["cs"], n)] if dc == 0 else ())
            for dc in range(2):
                P.op("tensor", lambda e, dc=dc, c=c: e.matmul(psC[:, 8 + dc:9 + dc], kw[:, dc * 128:(dc + 1) * 128],
                                                              va[:, c, 512:513], start=True, stop=True),
                     inc=S["dc"] if dc == 1 else None)
            P.op("scalar", lambda e: e.memzero(sm[:, 0:1]))
            P.op("scalar", lambda e: e.activation(out=junk[:], in_=psB[:, :], func=AF.Square, accum_out=sm[:, 0:1]),
                 waits=[(S["nd"], n + 1), (S["hn"], n)], inc=S["ss"])
            P.op("vector", lambda e: e.tensor_scalar_mul(sm[:, 6:7], psC[:, 0:1], -1.0), waits=[(S["nd"], n + 1)])
            P.op("vector", lambda e: e.tensor_tensor(out=sm[:, 1:2], in0=sm[:, 6:7], in1=psC[:, 0:1], op=ALU.max))
            P.op("vector", lambda e, c=c: e.tensor_tensor(out=sm[:, 1:2], in0=sm[:, 1:2], in1=fcol[:, c:c + 1], op=ALU.max))
            P.op("vector", lambda e: e.reciprocal(sm[:, 2:3], sm[:, 1:2]))
            P.op("vector", lambda e: e.tensor_tensor(out=sm[:, 3:4], in0=sm[:, 2:3], in1=sm[:, 2:3], op=ALU.mult),
                 waits=[(S["ss"], n + 1)])
            P.op("vector", lambda e: e.tensor_tensor(out=sm[:, 3:4], in0=sm[:, 3:4], in1=sm[:, 0:1], op=ALU.mult))
            P.op("vector", lambda e: e.tensor_scalar(out=sm[:, 3:4], in0=sm[:, 3:4], scalar1=1.0 / 512.0, scalar2=ML_EPS,
                                                     op0=ALU.mult, op1=ALU.add))
            vq = P.op("vector", lambda e: e.tensor_copy(sm[:, 7:8], sm[:, 3:4]), inc=S["sq1"])
            vq2 = P.op("scalar", lambda e: e.sqrt(sm[:, 7:8], sm[:, 7:8]), waits=[(S["sq1"], vq)], inc=S["sq2"])
            P.op("vector", lambda e: e.reciprocal(sm[:, 4:5], sm[:, 7:8]), waits=[(S["sq2"], vq2)])
            P.op("vector", lambda e: e.tensor_tensor(out=sm[:, 5:6], in0=sm[:, 4:5], in1=sm[:, 2:3], op=ALU.mult))
            P.op("vector", lambda e: e.scalar_tensor_tensor(out=hn[:], in0=psB[:, :], scalar=sm[:, 5:6], in1=ng[:],
                                                            op0=ALU.mult, op1=ALU.mult),
                 waits=[(S["hg"], n)], inc=S["hn"])
            for dc in range(2):
                P.op("vector", lambda e, dc=dc: e.tensor_tensor(out=Cs[:, dc, 0:512], in0=Cs[:, dc, 0:512], in1=psD[dc][:, :],
                                                                op=ALU.add),
                     waits=[(S["dc"], n + 1)] if dc == 0 else ())
            P.op("vector", lambda e: e.tensor_tensor(out=Cs[:, :, 512], in0=Cs[:, :, 512], in1=psC[:, 8:10], op=ALU.add),
                 inc=S["cs"])
            P.op("gpsimd", lambda e, sot=sot: e.tensor_tensor(out=hg[:], in0=hn[:], in1=sot[:], op=ALU.mult),
                 waits=[(S["hn"], n + 1), so_ring.fill_wait(sl), (S["tr"], n)], inc=S["hg"])
            for j in range(4):
                P.op("tensor", lambda e, j=j: e.transpose(psT[:, j, :], hg[:, j * 128:(j + 1) * 128], ident[:]),
                     waits=[(S["hg"], n + 1), (S["cp"], n)] if j == 0 else (), inc=S["tr"] if j == 3 else None)
            grp = (n // 8)
            hts = hts_r[grp % 2]
            lc = slice((c % 8) * 128, (c % 8 + 1) * 128)
            wl = [(S["tr"], n + 1)]
            if c % 8 == 0 and grp >= 2:
                wl.append((S["out"], 16 * (grp - 1)))
            P.op("scalar", lambda e, lc=lc, hts=hts: e.copy(hts[:, :, lc], psT[:, :, :]), waits=wl, inc=S["cp"])
            n += 1
            if c % 8 == 7:
                g0 = (c // 8) * 1024
                P.op("sync", lambda e, b=b, hts=hts, g0=g0: e.dma_start(
                    out=hT[b, :, g0:g0 + 1024].rearrange("(j p) s -> p j s", p=128), in_=hts[:]),
                     waits=[(S["cp"], n)], inc=S["out"], dma=True)
    if debug:
        dw = [(S["cp"], S["cp"].n), (S["cs"], S["cs"].n), (S["hg"], S["hg"].n)]
        P.op("sync", lambda e: e.dma_start(out=dbg1[:, 0:32], in_=wcol[:]), waits=dw, inc=S["out"], dma=True)
        P.op("sync", lambda e: e.dma_start(out=dbg1[:, 32:64], in_=fcol[:]), inc=S["out"], dma=True)
        P.op("sync", lambda e: e.dma_start(out=dbg1[:, 64:96], in_=rcb[:]), inc=S["out"], dma=True)
        P.op("sync", lambda e: e.dma_start(out=dbg2, in_=hn[:]), inc=S["out"], dma=True)
        P.op("sync", lambda e: e.dma_start(out=dbg3, in_=hg[:]), inc=S["out"], dma=True)
        P.op("sync", lambda e: e.dma_start(out=dbg4, in_=PT[:]), inc=S["out"], dma=True)
        P.op("sync", lambda e: e.dma_start(out=dbg5, in_=sm[:]), inc=S["out"], dma=True)
        P.op("sync", lambda e: e.dma_start(out=dbg6, in_=Mr[:]), inc=S["out"], dma=True)
    P.wait("sync", S["out"], S["out"].n)
    P.run()
    return nc


LN_EPS_P = LN_EPS / (ALPHA * ALPHA)


class LNCtx:
    def __init__(self, T):
        P = T.P
        self.T = T
        self.xs = Slots(P, "lx", 2, [128, TT], F32)
        self.zs = Slots(P, "lz", 2, [128, TT], F32)
        self.sq = Slots(P, "lq", 2, [128, TT], F32)
        self.ones_col = P.sbuf("ones_col", [128, 1], F32)
        self.ones_row = P.sbuf("ones_row1", [1, 128], F32)
        self.rows = P.sbuf("ln_rows", [1, 3, TT], F32)
        self.rstd_b = P.sbuf("rstd_b", [128, TT], F32)
        self.nmr_b = P.sbuf("nmr_b", [128, TT], F32)
        self.s_stat = P.sem("s_stat")
        P.op("gpsimd", lambda e: e.memset(self.ones_col[:], 1.0))
        P.op("gpsimd", lambda e: e.memset(self.ones_row[:], 1.0))
        self.t_init = P.tok("gpsimd")
        self.zwrite = {}
        self.stat_rel = []
        self.bc_rel = []
        self.rows_rel = []
        self.last_stat = None
        self.out_toks = []


def residual_ep(T, L, x_src, xwaits, zd, t, gA):
    P = T.P
    L.zwrite = {}

    def ep(bi, oc, bank, fw):
        ch = bi * 4 + oc
        ps = T.ps[bank]
        xs, xt, xrel = L.xs.get()
        v = P.op("sync", lambda e: e.dma_start(out=xt[:], in_=x_src[ch * 128:(ch + 1) * 128, t * TT:(t + 1) * TT]),
                 waits=xrel + list(xwaits(ch)), inc=L.xs.sem_in[xs], dma=True)
        tx = (L.xs.sem_in[xs], v)
        zs, zt, zrel = L.zs.get()
        v = P.op("vector", lambda e: e.scalar_tensor_tensor(out=zt[:], in0=ps[:, 0:TT], scalar=gA[:, ch:ch + 1], in1=xt[:],
                                                            op0=ALU.mult, op1=ALU.add),
                 waits=[fw, tx] + zrel, inc=T.pbank.freed[bank])
        tz = (T.pbank.freed[bank], v)
        L.xs.release(xs, [tz])
        sq, sqt, sqrel = L.sq.get()
        P.op("scalar", lambda e: e.activation(out=sqt[:], in_=zt[:], func=AF.Square), waits=[tz] + sqrel)
        tsq = P.tok("scalar")
        v = P.op("sync", lambda e: e.dma_start(out=zd[ch * 128:(ch + 1) * 128, :], in_=zt[:]), waits=[tz],
                 inc=L.zs.sem_out[zs], dma=True)
        tdma = (L.zs.sem_out[zs], v)
        L.zwrite[ch] = tdma
        first, last = ch == 0, ch == KC - 1
        w1 = [tz, L.t_init] + (L.stat_rel if first else [])
        P.op("tensor", lambda e: e.matmul(T.ps[6][0:1, 0:TT], L.ones_col[:, 0:1], zt[:], start=first, stop=last), waits=w1)
        v = P.op("tensor", lambda e: e.matmul(T.ps[7][0:1, 0:TT], L.ones_col[:, 0:1], sqt[:], start=first, stop=last),
                 waits=[tsq], inc=L.s_stat)
        tstat = (L.s_stat, v)
        L.last_stat = tstat
        L.zs.release(zs, [tdma, tstat])
        L.sq.release(sq, [tstat])

    return ep


def ln_finish(T, L, zd, t, lng, lnb, x_dst, nxt=None, final=False):
    P = T.P
    rows = L.rows
    P.op("vector", lambda e: e.tensor_scalar_mul(rows[:, 0, :], T.ps[6][0:1, 0:TT], 1.0 / D), waits=[L.last_stat] + L.rows_rel)
    P.op("vector", lambda e: e.tensor_scalar_mul(rows[:, 1, :], T.ps[7][0:1, 0:TT], 1.0 / D))
    tcopy = P.tok("vector")
    P.op("vector", lambda e: e.tensor_tensor(out=rows[:, 2, :], in0=rows[:, 0, :], in1=rows[:, 0, :], op=ALU.mult))
    P.op("vector", lambda e: e.tensor_tensor(out=rows[:, 1, :], in0=rows[:, 1, :], in1=rows[:, 2, :], op=ALU.subtract))
    P.op("vector", lambda e: e.tensor_scalar_add(rows[:, 1, :], rows[:, 1, :], LN_EPS_P))
    t1 = P.tok("vector")
    P.op("scalar", lambda e: e.sqrt(rows[:, 2, :], rows[:, 1, :]), waits=[t1])
    t2 = P.tok("scalar")
    P.op("vector", lambda e: e.reciprocal(rows[:, 1, :], rows[:, 2, :]), waits=[t2])
    P.op("vector", lambda e: e.scalar_tensor_tensor(out=rows[:, 2, :], in0=rows[:, 0, :], scalar=-1.0, in1=rows[:, 1, :],
                                                    op0=ALU.mult, op1=ALU.mult))
    t3 = P.tok("vector")
    P.op("tensor", lambda e: e.matmul(T.ps[6][:, 0:TT], L.ones_row[0:1, 0:128], rows[0:1, 1, :], start=True, stop=True),
         waits=[t3, tcopy])
    v = P.op("tensor", lambda e: e.matmul(T.ps[7][:, 0:TT], L.ones_row[0:1, 0:128], rows[0:1, 2, :], start=True, stop=True),
             inc=L.s_stat)
    tb = (L.s_stat, v)
    P.op("vector", lambda e: e.tensor_copy(L.rstd_b[:], T.ps[6][:, 0:TT]), waits=[tb] + L.bc_rel)
    P.op("vector", lambda e: e.tensor_copy(L.nmr_b[:], T.ps[7][:, 0:TT]))
    tbc = P.tok("vector")
    L.stat_rel = [tbc]
    L.rows_rel = [tb]
    xw = {}
    tn = None
    for c in range(KC):
        xs, xt, xrel = L.xs.get()
        v = P.op("sync", lambda e, xt=xt, c=c: e.dma_start(out=xt[:], in_=zd[c * 128:(c + 1) * 128, :]),
                 waits=xrel + [L.zwrite[c]], inc=L.xs.sem_in[xs], dma=True)
        tin = (L.xs.sem_in[xs], v)
        P.op("vector", lambda e, xt=xt: e.tensor_tensor(out=xt[:], in0=xt[:], in1=L.rstd_b[:], op=ALU.mult), waits=[tin, tbc])
        P.op("vector", lambda e, xt=xt: e.tensor_tensor(out=xt[:], in0=xt[:], in1=L.nmr_b[:], op=ALU.add))
        tv = P.tok("vector")
        zs, zt, zrel = L.zs.get()
        P.op("scalar", lambda e, xt=xt, zt=zt, c=c: e.activation(out=zt[:], in_=xt[:], func=AF.Identity,
                                                                 bias=lnb[:, c:c + 1], scale=lng[:, c:c + 1]),
             waits=[tv] + zrel)
        tx = P.tok("scalar")
        L.xs.release(xs, [tx])
        v = P.op("sync", lambda e, zt=zt, c=c: e.dma_start(out=x_dst[c * 128:(c + 1) * 128, t * TT:(t + 1) * TT], in_=zt[:]),
                 waits=[tx], inc=L.zs.sem_out[zs], dma=True)
        tout = (L.zs.sem_out[zs], v)
        xw[c] = tout
        if final:
            L.out_toks.append(tout)
        rel = [tout]
        if nxt is not None:
            act, onepsc, sh, relw = nxt
            P.op("vector", lambda e, zt=zt, c=c, act=act, onepsc=onepsc, sh=sh: e.tensor_scalar(
                out=act[:, c, :], in0=zt[:], scalar1=onepsc[:, c:c + 1], scalar2=sh[:, c:c + 1], op0=ALU.mult, op1=ALU.add),
                waits=[tx] + (list(relw) if c == 0 else []))
            tn = P.tok("vector")
            rel.append(tn)
        L.zs.release(zs, rel)
    L.bc_rel = [P.tok("vector")]
    return xw, tn


def load_mod(P, modv_d, ncols):
    t = P.sbuf("modv_sb", [128, ncols], F32)
    s = P.sem("s_modv")
    v = P.op("sync", lambda e: e.dma_start(out=t[:], in_=modv_d), inc=s, dma=True)
    return t, (s, v)


def ffn(T, L, w_in, w_out, x_src, xwaits, zd, t, gA, act_ready, sg):
    P = T.P
    blocks = [[(j * 512, 512)] for j in range(FF // 256)]

    sg_state = {}

    def ep_in(bi, oc, bank, fw):
        ps = T.ps[bank]
        if oc < 2:
            s, st, rel = sg.get()
            P.op("scalar", lambda e: e.activation(out=st[:], in_=ps[:, 0:TT], func=AF.Silu), waits=[fw] + rel,
                 inc=T.pbank.freed[bank])
            sg_state[oc] = (s, st, P.tok("scalar"))
        else:
            s, st, tk = sg_state[oc - 2]
            hc = bi * 2 + (oc - 2)
            w = [fw, tk] + (list(T.hid_rel) if (bi == 0 and oc == 2) else [])
            P.op("vector", lambda e: e.tensor_tensor(out=T.hid[:, hc, :], in0=st[:], in1=ps[:, 0:TT], op=ALU.mult),
                 waits=w, inc=T.pbank.freed[bank])
            sg.release(s, [P.tok("vector")])

    lf = T.gemm(w_in, D, blocks, ep_in, act_waits=[act_ready])
    T.act_rel = [lf]
    hid_ready = P.tok("vector")
    blocks2 = [[(c0, 512)] for c0 in range(0, D, 512)]
    lf2 = T.gemm(w_out, FF, blocks2, residual_ep(T, L, x_src, xwaits, zd, t, gA), act=T.hid, act_waits=[hid_ready])
    T.hid_rel = [lf2]


def build_phase_d1(mode="d1"):
    nc = bass.Bass("TRN2", target_bir_lowering=False)
    xT = _dram(nc, "xT", [D, NTOK], F32, "ExternalInput")
    if mode == "d1":
        hT = _dram(nc, "hT", [D, NTOK], BF16, "ExternalInput")
    else:
        yT = _dram(nc, "yT", [D, NTOK], F32, "ExternalInput")
        gT = _dram(nc, "gT", [D, NTOK], BF16, "ExternalInput")
    w_o = _dram(nc, "w_o", [D, D], F32, "ExternalInput")
    w_in = _dram(nc, "w_in", [D, 2 * FF], F32, "ExternalInput")
    w_out = _dram(nc, "w_out", [FF, D], F32, "ExternalInput")
    modv_d = _dram(nc, "modv", [128, 8 * KC], F32, "ExternalInput")
    outT = _dram(nc, "outT", [D, NTOK], F32, "ExternalOutput")
    x1T = nc.dram_tensor("x1T", [D, NTOK], F32, kind="Internal").ap()
    zd = nc.dram_tensor("zscr", [D, TT], F32, kind="Internal").ap()
    P = Prog(nc)
    P.serial = {"vector", "scalar", "gpsimd"}
    T = Tok(nc, P, hid_chunks=86, rings=False)
    L = LNCtx(T)
    sg = L.sq
    mv, tmod = load_mod(P, modv_d, 8 * KC)
    col = lambda i: mv[:, i * KC:(i + 1) * KC]
    onepsc = P.sbuf("onepsc", [128, KC], F32)
    P.op("vector", lambda e: e.tensor_scalar_add(onepsc[:], col(4), 1.0), waits=[tmod])
    P.op("vector", lambda e: e.tensor_scalar_mul(col(0), col(0), 1.0 / ALPHA))
    P.op("vector", lambda e: e.tensor_scalar_mul(col(5), col(5), 1.0 / ALPHA))
    tconst = P.tok("vector")
    s_act = P.sem("s_actin")
    if mode != "d1":
        yin = Slots(P, "yin", 2, [128, TT], F32)
        gin = Slots(P, "gin", 2, [128, TT], BF16)
    for t in range(NT):
        if mode == "d1":
            v = P.op("sync", lambda e, t=t: e.dma_start(out=T.act[:], in_=hT[:, t * TT:(t + 1) * TT].rearrange("(c p) s -> p c s", p=128)),
                     waits=list(T.act_rel), inc=s_act, dma=True)
            act_ready = (s_act, v)
        else:
            for c in range(KC):
                ys, yt, yrel = yin.get()
                gs, gt, grel = gin.get()
                v1 = P.op("sync", lambda e, yt=yt, c=c, t=t: e.dma_start(out=yt[:], in_=yT[c * 128:(c + 1) * 128, t * TT:(t + 1) * TT]),
                          waits=yrel, inc=yin.sem_in[ys], dma=True)
                v2 = P.op("sync", lambda e, gt=gt, c=c, t=t: e.dma_start(out=gt[:], in_=gT[c * 128:(c + 1) * 128, t * TT:(t + 1) * TT]),
                          waits=grel, inc=gin.sem_in[gs], dma=True)
                P.op("vector", lambda e, yt=yt, gt=gt, c=c: e.tensor_tensor(out=T.act[:, c, :], in0=yt[:], in1=gt[:], op=ALU.mult),
                     waits=[(yin.sem_in[ys], v1), (gin.sem_in[gs], v2)] + (list(T.act_rel) if c == 0 else []))
                tk = P.tok("vector")
                yin.release(ys, [tk])
                gin.release(gs, [tk])
            act_ready = P.tok("vector")
        blocks = [[(c0, 512)] for c0 in range(0, D, 512)]
        lf = T.gemm(w_o, D, blocks, residual_ep(T, L, xT, lambda ch: [], zd, t, col(0)), act_waits=[act_ready, tconst])
        T.act_rel = [lf]
        xw, tn = ln_finish(T, L, zd, t, col(1), col(2), x1T, nxt=(T.act, onepsc, col(3), T.act_rel))
        ffn(T, L, w_in, w_out, x1T, lambda ch, xw=xw: [xw[ch]], zd, t, col(5), tn, sg)
        ln_finish(T, L, zd, t, col(6), col(7), outT, nxt=None, final=True)
    for tk in L.out_toks:
        P.wait("sync", *tk)
    P.run()
    return nc


class Buf:
    def __init__(self, name, dma_sem=None):
        self.name = name
        self.w = None
        self.r = []
        self.dma_sem = dma_sem


class Dep:
    def __init__(self, P):
        self.P = P
        self.s_pe = P.sem("dep_pe")
        self.n = 0
        self.selfwait = True

    def buf(self, name, dma=False):
        return Buf(name, self.P.sem("d_" + name) if dma else None)

    def _waits(self, reads, writes):
        ws = []
        for b in reads:
            if b.w is not None:
                ws.append(b.w)
        for b in writes:
            if b.w is not None:
                ws.append(b.w)
            ws.extend(b.r)
        return ws

    def _commit(self, tok, reads, writes):
        for b in reads:
            if b not in writes:
                b.r.append(tok)
        for b in writes:
            b.w = tok
            b.r = []

    def op(self, eng, fn, reads=(), writes=()):
        P = self.P
        P.op(eng, fn, waits=self._waits(reads, writes), selfwait=self.selfwait)
        tok = P.tok(eng)
        self._commit(tok, reads, writes)
        return tok

    def pe(self, fns, reads=(), writes=()):
        P = self.P
        ws = self._waits(reads, writes)
        for i, fn in enumerate(fns):
            if i == len(fns) - 1:
                v = P.op("tensor", fn, waits=ws if i == 0 else (), inc=self.s_pe)
            else:
                P.op("tensor", fn, waits=ws if i == 0 else ())
        tok = (self.s_pe, v)
        self._commit(tok, reads, writes)
        return tok

    def dma(self, eng, fn, sem_buf, reads=(), writes=()):
        P = self.P
        v = P.op(eng, fn, waits=self._waits(reads, writes), inc=sem_buf.dma_sem, dma=True)
        tok = (sem_buf.dma_sem, v)
        self._commit(tok, reads, writes)
        return tok


RW_TOK = 8192
RW_S = 4096
RW_C = 64
RW_WN = 64
RW_NCW = RW_WN // RW_C
GN_EPS = 64e-5
DEC = -0.6065306597126334


def build_phase_e(ntok=RW_TOK, seq=RW_S, stage=99):
    nc = bass.Bass("TRN2", target_bir_lowering=False)
    rT = _dram(nc, "rT", [512, ntok], BF16, "ExternalInput")
    kT = _dram(nc, "kT", [512, ntok], BF16, "ExternalInput")
    vT = _dram(nc, "vT", [512, ntok], BF16, "ExternalInput")
    wT = _dram(nc, "wT", [512, ntok], F32, "ExternalInput")
    aT = _dram(nc, "aT", [512, ntok], F32, "ExternalInput")
    pv_d = _dram(nc, "pv", [64, 48], F32, "ExternalInput")
    yT = _dram(nc, "yT", [512, ntok], F32, "ExternalOutput")
    P = Prog(nc)
    P.serial = {"vector", "scalar", "gpsimd"}
    dp = Dep(P)
    dp.selfwait = False
    W, NC_, C = RW_WN, RW_NCW, RW_C
    sb = lambda n, sh, dt: P.sbuf(n, sh, dt)
    V, S_, g = "vector", "scalar", "gpsimd"
    ones64 = sb("ones64", [64, 64], F32)
    mask2 = sb("mask2", [64, 1, 128], F32)
    maskLT = sb("maskLT", [64, 1, 64], F32)
    blkm = sb("blkm", [64, 1, 64], F32)
    ET = sb("ET", [4, 64], F32)
    identf = sb("identf", [64, 64], F32)
    I64 = sb("I64", [64, 1, 64], F32)
    segm = sb("segm", [64, 8, W], F32)
    pv = sb("pv_s", [64, 48], F32)
    P.op(g, lambda e: e.memset(ones64[:], 1.0))
    P.op(g, lambda e: e.memset(mask2[:], 1.0))
    P.op(g, lambda e: e.affine_select(out=mask2[:, 0, 0:64], in_=mask2[:, 0, 0:64], pattern=[[1, 64]], compare_op=ALU.is_ge,
                                      fill=0.0, base=-1, channel_multiplier=-1))
    P.op(g, lambda e: e.affine_select(out=mask2[:, 0, 64:128], in_=mask2[:, 0, 64:128], pattern=[[1, 64]], compare_op=ALU.is_ge,
                                      fill=0.0, base=0, channel_multiplier=-1))
    P.op(g, lambda e: e.memset(maskLT[:], 1.0))
    P.op(g, lambda e: e.affine_select(out=maskLT[:, 0, :], in_=maskLT[:, 0, :], pattern=[[-1, 64]], compare_op=ALU.is_ge,
                                      fill=0.0, base=-1, channel_multiplier=1))
    P.op(g, lambda e: e.memset(identf[:], 1.0))
    P.op(g, lambda e: e.affine_select(out=identf[:], in_=identf[:], pattern=[[1, 64]], compare_op=ALU.is_equal,
                                      fill=0.0, base=0, channel_multiplier=-1))
    P.op(g, lambda e: e.memset(ET[:], 1.0))
    P.op(g, lambda e: e.affine_select(out=ET[:], in_=ET[:], pattern=[[1, 64]], compare_op=ALU.is_ge, fill=0.0, base=0, channel_multiplier=-16))
    P.op(g, lambda e: e.affine_select(out=ET[:], in_=ET[:], pattern=[[-1, 64]], compare_op=ALU.is_ge, fill=0.0, base=15, channel_multiplier=16))
    P.op(g, lambda e: e.tensor_copy(I64[:, 0, :], identf[:]))
    P.op(g, lambda e: e.memset(segm[:], 1.0))
    P.op(g, lambda e: e.memset(segm[:].rearrange("p h (c j) -> p (h c) j", j=C)[:, :, 0:1], 0.0))
    cb = dp.buf("const")
    cb.w = P.tok(g)
    Q_tmp = None
    pvb = dp.buf("pv", dma=True)
    dp.dma("sync", lambda e: e.dma_start(out=pv[:], in_=pv_d), pvb, writes=[pvb])
    dp.op(V, lambda e: e.tensor_scalar(out=pv[:, 16:24], in0=pv[:, 8:16], scalar1=-1.0, scalar2=1.0, op0=ALU.mult, op1=ALU.add),
          reads=[pvb], writes=[pvb])
    pbc = lambda q: pv[:, q * 8:(q + 1) * 8].unsqueeze(2).to_broadcast([64, 8, W])

    WT = []
    for i_ in range(3):
        sfx = str(i_)
        WT.append(dict(
            r_w=sb("r_w" + sfx, [64, 8, W], BF16), k_w=sb("k_w" + sfx, [64, 8, W], BF16), v_w=sb("v_w" + sfx, [64, 8, W], BF16),
            w_w=sb("w_w" + sfx, [64, 8, W], F32), a_w=sb("a_w" + sfx, [64, 8, W], F32), v_w32=sb("v_w32" + sfx, [64, 8, W], F32),
            AR=sb("AR" + sfx, [64, 8, NC_, 2, C], F32), BK=sb("BK" + sfx, [64, 8, NC_, 2, C], F32), BKh=sb("BKh" + sfx, [64, 8, 2, W], F32),
            gC=sb("gC" + sfx, [64, 8, NC_], F32), bonus=sb("bonus" + sfx, [64, 8, W], F32),
            Vt=sb("Vt" + sfx, [64, NC_, 512], F32), Bt=sb("Bt" + sfx, [64, NC_, 512], F32), Kt=sb("Kt" + sfx, [64, NC_, 512], F32),
            B_in=dp.buf("in" + sfx, dma=True), B_v32=dp.buf("v32" + sfx), B_AR=dp.buf("AR" + sfx), B_BK=dp.buf("BK" + sfx),
            B_BKh=dp.buf("BKh" + sfx), B_gC=dp.buf("gC" + sfx), B_bonus=dp.buf("bonus" + sfx), B_tm=dp.buf("tokmajor" + sfx),
            A1s=sb("A1s" + sfx, [64, 8, 128], F32), A2s=sb("A2s" + sfx, [64, 8, 128], F32), Gb=sb("Gb" + sfx, [64, 8, 64], F32),
            B_A1s=dp.buf("A1s" + sfx), B_A2s=dp.buf("A2s" + sfx), B_G=dp.buf("G" + sfx)))
    YW = 256
    ystage_ = [sb(f"ystage{i_}", [64, 8, YW], F32) for i_ in range(2)]
    B_ystage_ = [dp.buf(f"ystage{i_}", dma=True) for i_ in range(2)]
    UNP = ("r_w, k_w, v_w, w_w, a_w, v_w32, AR, BK, BKh, gC, bonus, Vt, Bt, Kt, B_in, B_v32, B_AR, B_BK, B_BKh, B_gC, B_bonus, B_tm, "
           "A1s, A2s, Gb, B_A1s, B_A2s, B_G")
    unp = lambda pb: tuple(WT[pb][n_.strip()] for n_ in UNP.split(","))
    tn = ("sg", "lg", "lgp", "d", "egi", "kk", "sq", "k2", "bv")
    tmp = {n: sb("t_" + n, [64, 8, W], F32) for n in tn}
    B_t = {n: dp.buf("t_" + n) for n in tn}
    Xs = [sb(f"Xs{i}", [64, 8, 64], F32) for i in range(2)]
    Ys = [sb(f"Ys{i}", [64, 8, 64], F32) for i in range(2)]
    Hb_ = sb("Hb_", [64, 8, 64], F32)
    Lo = sb("Lo", [64, 8, 64], F32); LoT = sb("LoT", [64, 8, 64], F32)
    B_Lo = dp.buf("Lo"); B_LoT = dp.buf("LoT")
    X1s = sb("X1s", [64, 512], F32); Us = sb("Us", [64, 512], F32)
    Hf = sb("Hf", [64, 8, 64], F32); Hbf = Hf
    ysq = sb("ysq", [64, 512], F32); yn = sb("yn", [64, 512], F32)
    st = sb("st", [64, 6, 8], F32)
    B_Xs = [dp.buf("Xs0"), dp.buf("Xs1")]; B_Ys = [dp.buf("Ys0"), dp.buf("Ys1")]
    B_H = dp.buf("H"); B_X1s = dp.buf("X1s"); B_Us = dp.buf("Us")
    B_Hf = dp.buf("Hf"); B_Hf = dp.buf("Hbf"); B_ysq = dp.buf("ysq"); B_yn = dp.buf("yn"); B_st = dp.buf("st")
    Q = [P.psum(f"Q{i}", [128, 512], F32) for i in range(8)]
    B_Q = [dp.buf(f"Q{i}") for i in range(8)]
    h3 = lambda q, j: q[0:64, :].rearrange("p (h j) -> p h j", j=j)
    fl = lambda t: t[:].rearrange("p h j -> p (h j)")
    B_blk = dp.buf("blk")
    dp.pe([lambda e: e.matmul(Q[0][0:64, 0:64], ET[:], ET[:], start=True, stop=True)], reads=[cb], writes=[B_Q[0]])
    dp.op(V, lambda e: e.tensor_copy(blkm[:, 0, :], Q[0][0:64, 0:64]), reads=[B_Q[0]], writes=[cb])

    def prep(t0, pb):
        (r_w, k_w, v_w, w_w, a_w, v_w32, AR, BK, BKh, gC, bonus, Vt, Bt, Kt, B_in, B_v32, B_AR, B_BK, B_BKh, B_gC, B_bonus, B_tm,
         A1s, A2s, Gb, B_A1s, B_A2s, B_G) = unp(pb)
        for (dst, src) in ((r_w, rT), (k_w, kT), (v_w, vT), (w_w, wT), (a_w, aT)):
            dp.dma("sync", lambda e, dst=dst, src=src: e.dma_start(out=dst[:], in_=src[:, t0:t0 + W].rearrange("(h n) s -> n h s", n=64)),
                   B_in, writes=[B_in])
        dp.op(S_, lambda e: e.copy(v_w32[:], v_w[:]), reads=[B_in], writes=[B_v32])
        T_ = tmp
        c4 = lambda ap: ap.rearrange("p h (c j) -> p h c j", j=C)
        Bi = B_in
        dp.op(S_, lambda e: e.activation(out=T_["sg"][:], in_=w_w[:], func=AF.Sigmoid), reads=[Bi], writes=[B_t["sg"]])
        dp.op(V, lambda e: e.tensor_scalar_mul(T_["sg"][:], T_["sg"][:], DEC), writes=[B_t["sg"]])
        dp.op(V, lambda e: e.tensor_tensor_scan(out=fl(T_["lg"]), data0=fl(segm), data1=fl(T_["sg"]), initial=0.0,
                                                op0=ALU.mult, op1=ALU.add), reads=[B_t["sg"], cb], writes=[B_t["lg"]])
        dp.op("gpsimd", lambda e: e.tensor_tensor(out=T_["lgp"][:], in0=T_["lg"][:], in1=T_["sg"][:], op=ALU.subtract),
              reads=[B_t["lg"], B_t["sg"]], writes=[B_t["lgp"]])
        for h in range(8):
            dp.op(V, lambda e, h=h: e.tensor_tensor(out=c4(T_["d"][:])[:, h], in0=c4(T_["lg"][:])[:, h, :, C - 1:C].to_broadcast([64, NC_, C]),
                                                    in1=c4(T_["lg"][:])[:, h], op=ALU.subtract), reads=[B_t["lg"]], writes=[B_t["d"]])
        yield
        dp.op(S_, lambda e: e.activation(out=gC[:], in_=c4(T_["lg"][:])[:, :, :, C - 1], func=AF.Exp), reads=[B_t["lg"]], writes=[B_gC])
        dp.op(S_, lambda e: e.activation(out=T_["egi"][:], in_=T_["lg"][:], func=AF.Exp, scale=-1.0), reads=[B_t["lg"]], writes=[B_t["egi"]])
        dp.op(S_, lambda e: e.activation(out=T_["lg"][:], in_=T_["lg"][:], func=AF.Exp), writes=[B_t["lg"]])
        dp.op(S_, lambda e: e.activation(out=T_["lgp"][:], in_=T_["lgp"][:], func=AF.Exp), writes=[B_t["lgp"]])
        dp.op(S_, lambda e: e.activation(out=T_["d"][:], in_=T_["d"][:], func=AF.Exp), writes=[B_t["d"]])
        yield
        dp.op("gpsimd", lambda e: e.tensor_tensor(out=T_["kk"][:], in0=k_w[:], in1=pbc(0), op=ALU.mult), reads=[Bi, pvb], writes=[B_t["kk"]])
        dp.op(S_, lambda e: e.activation(out=T_["sq"][:], in_=T_["kk"][:], func=AF.Square), reads=[B_t["kk"]], writes=[B_t["sq"]])
        for i in range(8 * W // 512):
            dp.pe([lambda e, i=i: e.matmul(Q[6][0:64, :], ones64[:], fl(T_["sq"])[:, i * 512:(i + 1) * 512], start=True, stop=True)],
                  reads=[B_t["sq"], cb], writes=[B_Q[6]])
            dp.op(S_, lambda e, i=i: e.sqrt(fl(T_["sq"])[:, i * 512:(i + 1) * 512], Q[6][0:64, :]), reads=[B_Q[6]], writes=[B_t["sq"]])
        dp.op(V, lambda e: e.tensor_scalar_max(T_["sq"][:], T_["sq"][:], 1e-12), writes=[B_t["sq"]])
        dp.op(V, lambda e: e.reciprocal(T_["sq"][:], T_["sq"][:]), writes=[B_t["sq"]])
        dp.op(V, lambda e: e.tensor_tensor(out=T_["kk"][:], in0=T_["kk"][:], in1=T_["sq"][:], op=ALU.mult),
              reads=[B_t["sq"]], writes=[B_t["kk"]])
        yield
        dp.op("gpsimd", lambda e: e.tensor_tensor(out=T_["k2"][:], in0=a_w[:], in1=pbc(1), op=ALU.mult), reads=[Bi, pvb], writes=[B_t["k2"]])
        dp.op("gpsimd", lambda e: e.tensor_tensor(out=T_["k2"][:], in0=T_["k2"][:], in1=pbc(2), op=ALU.add), writes=[B_t["k2"]])
        dp.op("gpsimd", lambda e: e.tensor_tensor(out=T_["k2"][:], in0=T_["k2"][:], in1=k_w[:], op=ALU.mult), reads=[Bi], writes=[B_t["k2"]])
        dp.op(V, lambda e: e.tensor_tensor(out=T_["bv"][:], in0=T_["kk"][:], in1=a_w[:], op=ALU.mult),
              reads=[Bi, B_t["kk"]], writes=[B_t["bv"]])
        yield
        dp.op("gpsimd", lambda e: e.tensor_tensor(out=AR[:, :, :, 1, :], in0=c4(r_w[:]), in1=c4(T_["lg"][:]), op=ALU.mult),
              reads=[Bi, B_t["lg"]], writes=[B_AR])
        dp.op(V, lambda e: e.scalar_tensor_tensor(out=AR[:, :, :, 0, :], in0=c4(T_["kk"][:]), scalar=-1.0, in1=c4(T_["lgp"][:]),
                                                  op0=ALU.mult, op1=ALU.mult), reads=[B_t["kk"], B_t["lgp"]], writes=[B_AR])
        dp.op(V, lambda e: e.tensor_tensor(out=BK[:, :, :, 0, :], in0=c4(T_["bv"][:]), in1=c4(T_["egi"][:]), op=ALU.mult),
              reads=[B_t["bv"], B_t["egi"]], writes=[B_BK])
        dp.op("gpsimd", lambda e: e.tensor_tensor(out=BK[:, :, :, 1, :], in0=c4(T_["k2"][:]), in1=c4(T_["egi"][:]), op=ALU.mult),
              reads=[B_t["k2"], B_t["egi"]], writes=[B_BK])
        dp.op(V, lambda e: e.tensor_tensor(out=BKh[:, :, 0, :], in0=T_["bv"][:], in1=T_["d"][:], op=ALU.mult),
              reads=[B_t["bv"], B_t["d"]], writes=[B_BKh])
        dp.op("gpsimd", lambda e: e.tensor_tensor(out=BKh[:, :, 1, :], in0=T_["k2"][:], in1=T_["d"][:], op=ALU.mult),
              reads=[B_t["k2"], B_t["d"]], writes=[B_BKh])
        yield
        dp.op("gpsimd", lambda e: e.tensor_tensor(out=T_["sq"][:], in0=r_w[:], in1=pbc(3), op=ALU.mult), reads=[Bi, pvb], writes=[B_t["sq"]])
        dp.op("gpsimd", lambda e: e.tensor_tensor(out=T_["sq"][:], in0=T_["sq"][:], in1=T_["k2"][:], op=ALU.mult), reads=[B_t["k2"]], writes=[B_t["sq"]])
        for i in range(8 * W // 512):
            dp.pe([lambda e, i=i: e.matmul(Q[6][0:64, :], ones64[:], fl(T_["sq"])[:, i * 512:(i + 1) * 512], start=True, stop=True)],
                  reads=[B_t["sq"], cb], writes=[B_Q[6]])
            dp.op(V, lambda e, i=i: e.tensor_tensor(out=fl(bonus)[:, i * 512:(i + 1) * 512], in0=Q[6][0:64, :],
                                                    in1=fl(v_w)[:, i * 512:(i + 1) * 512], op=ALU.mult), reads=[B_Q[6], Bi], writes=[B_bonus])


    def tm_chunk(c, pb):
        (r_w, k_w, v_w, w_w, a_w, v_w32, AR, BK, BKh, gC, bonus, Vt, Bt, Kt, B_in, B_v32, B_AR, B_BK, B_BKh, B_gC, B_bonus, B_tm,
         A1s, A2s, Gb, B_A1s, B_A2s, B_G) = unp(pb)
        cs_ = slice(c * C, (c + 1) * C)
        for ti, (dst, srcf, rb) in enumerate(((Vt, lambda h: v_w32[:, h, cs_], B_v32), (Bt, lambda h: BKh[:, h, 0, cs_], B_BKh),
                                               (Kt, lambda h: BKh[:, h, 1, cs_], B_BKh))):
            dp.pe([lambda e, h=h, srcf=srcf: e.matmul(Q[6][0:64, h * 64:(h + 1) * 64], srcf(h), identf[:], start=True, stop=True)
                   for h in range(8)], reads=[rb, cb], writes=[B_Q[6]])
            if ti == 1:
                dp.op(S_, lambda e, dst=dst: e.copy(dst[:, c, :], Q[6][0:64, :]), reads=[B_Q[6]], writes=[B_tm])
            else:
                dp.op(V, lambda e, dst=dst: e.tensor_copy(dst[:, c, :], Q[6][0:64, :]), reads=[B_Q[6]], writes=[B_tm])


    def pre(c, pb):
        (r_w, k_w, v_w, w_w, a_w, v_w32, AR, BK, BKh, gC, bonus, Vt, Bt, Kt, B_in, B_v32, B_AR, B_BK, B_BKh, B_gC, B_bonus, B_tm,
         A1s, A2s, Gb, B_A1s, B_A2s, B_G) = unp(pb)
        hs = lambda h: slice(h * 64, (h + 1) * 64)
        dp.pe([lambda e, h=h: e.matmul(Q[h // 4][0:64, (h % 4) * 128:(h % 4 + 1) * 128], BK[:, h, c, 0, :], AR[:, h, c, :, :],
                                       start=True, stop=True) for h in range(8)], reads=[B_AR, B_BK], writes=[B_Q[0], B_Q[1]])
        dp.pe([lambda e, h=h: e.matmul(Q[2 + h // 4][0:64, (h % 4) * 128:(h % 4 + 1) * 128], BK[:, h, c, 1, :], AR[:, h, c, :, :],
                                       start=True, stop=True) for h in range(8)], reads=[B_AR, B_BK], writes=[B_Q[2], B_Q[3]])
        dp.pe([lambda e, h=h: e.matmul(Q[4][0:64, hs(h)], AR[:, h, c, 0, :], BK[:, h, c, 0, :], start=True, stop=True)
               for h in range(8)], reads=[B_AR, B_BK], writes=[B_Q[4]])
        yield
        m2b = mask2[:].to_broadcast([64, 4, 128])
        dp.op(V, lambda e: e.tensor_tensor(out=A1s[:, 0:4, :], in0=h3(Q[0], 128), in1=m2b, op=ALU.mult), reads=[B_Q[0], cb], writes=[B_A1s])
        dp.op(V, lambda e: e.tensor_tensor(out=A1s[:, 4:8, :], in0=h3(Q[1], 128), in1=m2b, op=ALU.mult), reads=[B_Q[1], cb], writes=[B_A1s])
        dp.op(V, lambda e: e.tensor_tensor(out=A2s[:, 0:4, :], in0=h3(Q[2], 128), in1=m2b, op=ALU.mult), reads=[B_Q[2], cb], writes=[B_A2s])
        dp.op(V, lambda e: e.tensor_tensor(out=A2s[:, 4:8, :], in0=h3(Q[3], 128), in1=m2b, op=ALU.mult), reads=[B_Q[3], cb], writes=[B_A2s])
        bm8 = blkm[:].to_broadcast([64, 8, 64])
        I8 = I64[:].to_broadcast([64, 8, 64])
        dp.op(V, lambda e: e.tensor_tensor(out=LoT[:], in0=h3(Q[4], 64), in1=maskLT[:].to_broadcast([64, 8, 64]), op=ALU.mult),
              reads=[B_Q[4], cb], writes=[B_LoT])
        dp.op(V, lambda e: e.tensor_tensor(out=Ys[0][:], in0=LoT[:], in1=bm8, op=ALU.mult), reads=[B_LoT, cb], writes=[B_Ys[0]])
        dp.op(V, lambda e: e.tensor_tensor(out=Xs[0][:], in0=A1s[:, :, 0:64], in1=bm8, op=ALU.mult), reads=[B_A1s, cb], writes=[B_Xs[0]])
        dp.op(V, lambda e: e.tensor_tensor(out=Lo[:], in0=A1s[:, :, 0:64], in1=Xs[0][:], op=ALU.subtract),
              reads=[B_A1s, B_Xs[0]], writes=[B_Lo])
        yield
        dp.op(V, lambda e: e.tensor_tensor(out=Gb[:], in0=Xs[0][:], in1=I8, op=ALU.add), reads=[B_Xs[0], cb], writes=[B_G])
        cur = 0
        for lvl in range(1, 4):
            nxt = 1 - cur
            if lvl < 3:
                dp.pe([lambda e, h=h, cur=cur: e.matmul(Q[0][0:64, hs(h)], Ys[cur][:, h, :], Xs[cur][:, h, :], start=True, stop=True)
                       for h in range(8)], reads=[B_Xs[cur], B_Ys[cur]], writes=[B_Q[0]])
            dp.pe([lambda e, h=h, cur=cur: e.matmul(Q[1][0:64, hs(h)], Xs[cur][:, h, :], Ys[cur][:, h, :], start=True, stop=True)
                   for h in range(8)], reads=[B_Xs[cur], B_Ys[cur]], writes=[B_Q[1]])
            if lvl < 3:
                dp.op(S_, lambda e, nxt=nxt: e.copy(fl(Xs[nxt]), Q[0][0:64, :]), reads=[B_Q[0]], writes=[B_Xs[nxt]])
            dp.op(V, lambda e, nxt=nxt: e.tensor_copy(fl(Ys[nxt]), Q[1][0:64, :]), reads=[B_Q[1]], writes=[B_Ys[nxt]])
            dp.pe([lambda e, h=h, nxt=nxt: e.matmul(Q[2][0:64, hs(h)], Ys[nxt][:, h, :], Gb[:, h, :], start=True, stop=True)
                   for h in range(8)], reads=[B_G, B_Ys[nxt]], writes=[B_Q[2]])
            dp.op(V, lambda e: e.tensor_tensor(out=fl(Gb), in0=fl(Gb), in1=Q[2][0:64, :], op=ALU.add), reads=[B_Q[2]], writes=[B_G])
            cur = nxt
            yield
        dp.pe([lambda e, h=h: e.transpose(Q[3][0:64, hs(h)], Gb[:, h, :], identf[:]) for h in range(8)], reads=[B_G, cb], writes=[B_Q[3]])
        dp.op(S_, lambda e: e.copy(fl(Hb_), Q[3][0:64, :]), reads=[B_Q[3]], writes=[B_H])
        ZTs, BZT = Ys[0], B_Ys[0]
        R1, BR1, R2, BR2 = Xs[0], B_Xs[0], Xs[1], B_Xs[1]
        dp.pe([lambda e, h=h: e.matmul(Q[1][0:64, hs(h)], Lo[:, h, :], Hb_[:, h, :], start=True, stop=True) for h in range(8)],
              reads=[B_H, B_Lo], writes=[B_Q[1]])
        dp.op(S_, lambda e: e.copy(fl(ZTs), Q[1][0:64, :]), reads=[B_Q[1]], writes=[BZT])
        yield
        dp.pe([lambda e, h=h: e.matmul(Q[0][0:64, hs(h)], ZTs[:, h, :], Gb[:, h, :], start=True, stop=True) for h in range(8)],
              reads=[BZT, B_G], writes=[B_Q[0]])
        dp.op(V, lambda e: e.tensor_tensor(out=fl(R1), in0=fl(Gb), in1=Q[0][0:64, :], op=ALU.add), reads=[B_Q[0], B_G], writes=[BR1])
        yield
        dp.pe([lambda e, h=h: e.matmul(Q[2][0:64, hs(h)], ZTs[:, h, :], R1[:, h, :], start=True, stop=True) for h in range(8)],
              reads=[BZT, BR1], writes=[B_Q[2]])
        dp.op(V, lambda e: e.tensor_tensor(out=fl(R2), in0=fl(Gb), in1=Q[2][0:64, :], op=ALU.add), reads=[B_Q[2], B_G], writes=[BR2])
        yield
        dp.pe([lambda e, h=h: e.matmul(Q[0][0:64, hs(h)], ZTs[:, h, :], R2[:, h, :], start=True, stop=True) for h in range(8)],
              reads=[BZT, BR2], writes=[B_Q[0]])
        dp.op(V, lambda e: e.tensor_tensor(out=fl(Gb), in0=fl(Gb), in1=Q[0][0:64, :], op=ALU.add), reads=[B_Q[0]], writes=[B_G])

    def seqg(c, pb, k):
        hs = lambda h: slice(h * 64, (h + 1) * 64)
        (r_w, k_w, v_w, w_w, a_w, v_w32, AR, BK, BKh, gC, bonus, Vt, Bt, Kt, B_in, B_v32, B_AR, B_BK, B_BKh, B_gC, B_bonus, B_tm,
         A1s, A2s, Gb, B_A1s, B_A2s, B_G) = unp(pb)
        ystage = ystage_[(k // 4) % 2]; B_ystage = B_ystage_[(k // 4) % 2]; yoff = (k % 4) * C
        fX = []
        for h in range(8):
            fX.append(lambda e, h=h: e.matmul(Q[7][0:64, hs(h)], AR[:, h, c, 0, :], Hbf[:, h, :], start=True, stop=False))
            fX.append(lambda e, h=h: e.matmul(Q[7][0:64, hs(h)], A2s[:, h, 0:64], Vt[:, c, hs(h)], start=False, stop=True))
        dp.pe(fX, reads=[B_AR, B_Hf, B_A2s, B_tm], writes=[B_Q[7]])
        dp.op(S_, lambda e: e.copy(X1s[:], Q[7][0:64, :]), reads=[B_Q[7]], writes=[B_X1s])
        yield
        dp.pe([lambda e, h=h: e.matmul(Q[7][0:64, hs(h)], Gb[:, h, :], X1s[:, hs(h)], start=True, stop=True) for h in range(8)],
              reads=[B_G, B_X1s], writes=[B_Q[7]])
        dp.op(S_, lambda e: e.copy(Us[:], Q[7][0:64, :]), reads=[B_Q[7]], writes=[B_Us])
        yield
        fY = []
        for h in range(8):
            fY.append(lambda e, h=h: e.matmul(Q[5][0:64, hs(h)], AR[:, h, c, 1, :], Hbf[:, h, :], start=True, stop=False))
            fY.append(lambda e, h=h: e.matmul(Q[5][0:64, hs(h)], A1s[:, h, 64:128], Us[:, hs(h)], start=False, stop=False))
            fY.append(lambda e, h=h: e.matmul(Q[5][0:64, hs(h)], A2s[:, h, 64:128], Vt[:, c, hs(h)], start=False, stop=True))
        dp.pe(fY, reads=[B_AR, B_Hf, B_A1s, B_A2s, B_Us, B_tm], writes=[B_Q[5]])
        yield
        fH = []
        for h in range(8):
            fH.append(lambda e, h=h: e.matmul(Q[6][0:64, hs(h)], Bt[:, c, hs(h)], Us[:, hs(h)], start=True, stop=False))
            fH.append(lambda e, h=h: e.matmul(Q[6][0:64, hs(h)], Kt[:, c, hs(h)], Vt[:, c, hs(h)], start=False, stop=True))
        dp.pe(fH, reads=[B_Us, B_tm], writes=[B_Q[6]])
        dp.op(V, lambda e: e.tensor_tensor(out=Hf[:], in0=Hf[:], in1=gC[:, :, c:c + 1].to_broadcast([64, 8, 64]), op=ALU.mult),
              reads=[B_gC], writes=[B_Hf])
        dp.op(V, lambda e: e.tensor_tensor(out=fl(Hf), in0=fl(Hf), in1=Q[6][0:64, :], op=ALU.add), reads=[B_Q[6]], writes=[B_Hf])
        yield
        y3 = h3(Q[5], 64)
        yn3 = yn[:].rearrange("p (h j) -> p h j", j=64)
        dp.op(V, lambda e: e.tensor_reduce(out=st[:, 0, :], in_=y3, axis=AX.X, op=ALU.add), reads=[B_Q[5]], writes=[B_st])
        dp.op(S_, lambda e: e.activation(out=ysq[:], in_=Q[5][0:64, :], func=AF.Square), reads=[B_Q[5]], writes=[B_ysq])
        dp.op(V, lambda e: e.tensor_reduce(out=st[:, 1, :], in_=ysq[:].rearrange("p (h j) -> p h j", j=64), axis=AX.X, op=ALU.add),
              reads=[B_ysq], writes=[B_st])
        yield
        dp.op(V, lambda e: e.tensor_scalar_mul(st[:, 0, :], st[:, 0, :], 1.0 / 64), writes=[B_st])
        dp.op(V, lambda e: e.tensor_tensor(out=st[:, 2, :], in0=st[:, 0, :], in1=st[:, 0, :], op=ALU.mult), writes=[B_st])
        dp.op(V, lambda e: e.scalar_tensor_tensor(out=st[:, 1, :], in0=st[:, 1, :], scalar=1.0 / 64, in1=st[:, 2, :],
                                                  op0=ALU.mult, op1=ALU.subtract), writes=[B_st])
        dp.op(V, lambda e: e.tensor_scalar_add(st[:, 1, :], st[:, 1, :], GN_EPS), writes=[B_st])
        dp.op(S_, lambda e: e.sqrt(st[:, 3, :], st[:, 1, :]), reads=[B_st], writes=[B_st])
        dp.op(V, lambda e: e.reciprocal(st[:, 4, :], st[:, 3, :]), reads=[B_st], writes=[B_st])
        dp.op(V, lambda e: e.tensor_tensor(out=yn3, in0=y3, in1=st[:, 0, :].unsqueeze(2).to_broadcast([64, 8, 64]), op=ALU.subtract),
              reads=[B_Q[5], B_st], writes=[B_yn])
        dp.op(V, lambda e: e.tensor_tensor(out=yn3, in0=yn3, in1=st[:, 4, :].unsqueeze(2).to_broadcast([64, 8, 64]), op=ALU.mult),
              reads=[B_st], writes=[B_yn])
        yield
        dp.pe([lambda e, h=h: e.transpose(Q[5][0:64, hs(h)], yn[:, hs(h)], identf[:]) for h in range(8)],
              reads=[B_yn, cb], writes=[B_Q[5]])
        yo = ystage[:, :, yoff:yoff + C]
        gcol = pv[:, 32:40].unsqueeze(2).to_broadcast([64, 8, 64])
        bcol = pv[:, 40:48].unsqueeze(2).to_broadcast([64, 8, 64])
        dp.op(V, lambda e: e.tensor_tensor(out=yo, in0=h3(Q[5], 64), in1=gcol, op=ALU.mult), reads=[B_Q[5], pvb], writes=[B_ystage])
        dp.op(V, lambda e: e.tensor_tensor(out=yo, in0=yo, in1=bcol, op=ALU.add), reads=[pvb], writes=[B_ystage])
        dp.op(V, lambda e: e.tensor_tensor(out=yo, in0=yo, in1=bonus[:, :, c * C:(c + 1) * C], op=ALU.add),
              reads=[B_bonus], writes=[B_ystage])

        yield

    def stageA1(k):
        pb = k % 3
        yield from prep(k * C, pb)
        tm_chunk(0, pb)
        yield

    def stageA2(k):
        yield from pre(0, k % 3)

    def stageB(k):
        if k % (seq // C) == 0:
            dp.op(V, lambda e: e.memset(Hf[:], 0.0), writes=[B_Hf])
        yield from seqg(0, k % 3, k)
        if k % 4 == 3:
            t0 = (k - 3) * C
            yb = (k // 4) % 2
            dp.dma("sync", lambda e, t0=t0, yb=yb: e.dma_start(out=yT[:, t0:t0 + YW].rearrange("(h n) s -> n h s", n=64), in_=ystage_[yb][:]),
                   B_ystage_[yb], reads=[B_ystage_[yb]])

    def drain(*gens):
        gens = list(gens)
        while gens:
            for g_ in list(gens):
                try:
                    next(g_)
                except StopIteration:
                    gens.remove(g_)

    nchunks = ntok // C
    drain(stageA1(0))
    if nchunks > 1:
        drain(stageA2(0), stageA1(1))
    else:
        drain(stageA2(0))
    for k in range(nchunks):
        gens = [stageB(k)]
        if k + 1 < nchunks:
            gens.append(stageA2(k + 1))
        if k + 2 < nchunks:
            gens.append(stageA1(k + 2))
        drain(*gens)
    for yb in range(2):
        P.wait("sync", B_ystage_[yb].dma_sem, B_ystage_[yb].dma_sem.n)
    P.run()
    return nc


def build_phase_d2():
    nc = bass.Bass("TRN2", target_bir_lowering=False)
    xT = _dram(nc, "xT", [D, NTOK], F32, "ExternalInput")
    xprev_d = _dram(nc, "xprev", [128, KC], F32, "ExternalInput")
    modv_d = _dram(nc, "modv", [128, 16 * KC + 1], F32, "ExternalInput")
    Wd = {n: _dram(nc, n, sh, F32, "ExternalInput") for n, sh in (
        ("w_r", [D, D]), ("w_k", [D, D]), ("w_v", [D, D]), ("w1", [D, 128]), ("w2", [128, D]),
        ("a1", [D, 128]), ("a2", [128, D]), ("g1", [D, 512]), ("g2", [512, D]))}
    outs = {n: _dram(nc, n, [D, NTOK], dt, "ExternalOutput") for n, dt in (
        ("rT", BF16), ("kT", BF16), ("vT", BF16), ("gT", BF16), ("wT", F32), ("aT", F32))}
    P = Prog(nc)
    P.serial = {"vector", "scalar", "gpsimd"}
    T = Tok(nc, P, hid_chunks=0, rings=False)
    act2 = P.sbuf("act2", [128, KC, TT], BF16)
    acts = [T.act, act2]
    U = P.sbuf("U", [128, KC, TT + 1], F32)
    h1 = P.sbuf("h1", [128, 4, TT], BF16)
    lastcol = P.sbuf("lastcol", [128, KC], F32)
    xs = Slots(P, "dx", 2, [128, TT], F32)
    tmps = Slots(P, "dtmp", 2, [128, TT], F32)
    ostf = Slots(P, "dof", 2, [128, TT], F32)
    ostb = Slots(P, "dob", 2, [128, TT], BF16)
    mv, tmod = load_mod(P, modv_d, 16 * KC + 1)
    col = lambda i: mv[:, i * KC:(i + 1) * KC]
    flag = mv[:, 16 * KC:16 * KC + 1]
    onepsc = P.sbuf("onepsc", [128, KC], F32)
    xp = P.sbuf("xp", [128, KC], F32)
    s_xp = P.sem("s_xp")
    v = P.op("sync", lambda e: e.dma_start(out=xp[:], in_=xprev_d), inc=s_xp, dma=True)
    P.op("vector", lambda e: e.tensor_scalar_add(onepsc[:], col(1), 1.0), waits=[tmod, (s_xp, v)])
    P.op("vector", lambda e: e.tensor_scalar(out=mv[:, 8 * KC:14 * KC], in0=mv[:, 2 * KC:8 * KC], scalar1=-1.0, scalar2=1.0,
                                             op0=ALU.mult, op1=ALU.add))
    P.op("vector", lambda e: e.tensor_tensor(out=xp[:], in0=xp[:], in1=onepsc[:], op=ALU.mult))
    P.op("vector", lambda e: e.tensor_tensor(out=xp[:], in0=xp[:], in1=col(0), op=ALU.add))
    P.op("vector", lambda e: e.tensor_scalar_mul(lastcol[:], xp[:], flag))
    tconst = P.tok("vector")
    out_toks = []
    act_rel = [[], []]
    h1_rel = []
    U_rel = []
    ai = 0
    for t in range(NT):
        P.op("vector", lambda e: e.tensor_copy(U[:, :, 0], lastcol[:]), waits=[tconst] + U_rel)
        for c in range(KC):
            s, xt, rel = xs.get()
            v = P.op("sync", lambda e, xt=xt, c=c, t=t: e.dma_start(out=xt[:], in_=xT[c * 128:(c + 1) * 128, t * TT:(t + 1) * TT]),
                     waits=rel, inc=xs.sem_in[s], dma=True)
            P.op("scalar", lambda e, xt=xt, c=c: e.activation(out=U[:, c, 1:TT + 1], in_=xt[:], func=AF.Identity,
                                                              bias=col(0)[:, c:c + 1], scale=onepsc[:, c:c + 1]),
                 waits=[(xs.sem_in[s], v), tconst] + (U_rel if c == 0 else []))
            xs.release(s, [P.tok("scalar")])
        tU = P.tok("scalar")
        P.op("vector", lambda e: e.tensor_copy(lastcol[:], U[:, :, TT]), waits=[tU])
        tUv = P.tok("vector")
        U_readers = []

        def build_act(j):
            nonlocal ai
            a = acts[ai % 2]
            rel = act_rel[ai % 2]
            for c in range(KC):
                s, tt, trel = tmps.get()
                P.op("scalar", lambda e, tt=tt, c=c, j=j: e.activation(out=tt[:], in_=U[:, c, 1:TT + 1], func=AF.Copy,
                                                                       scale=col(8 + j)[:, c:c + 1]), waits=[tU, tUv] + trel)
                tk = P.tok("scalar")
                P.op("vector", lambda e, tt=tt, c=c, j=j, a=a: e.scalar_tensor_tensor(out=a[:, c, :], in0=U[:, c, 0:TT], scalar=col(2 + j)[:, c:c + 1],
                                                                                      in1=tt[:], op0=ALU.mult, op1=ALU.add),
                     waits=[tk, tU, tUv] + (list(rel) if c == 0 else []))
                tmps.release(s, [P.tok("vector")])
            U_readers.append(P.tok("vector"))
            U_readers.append(P.tok("scalar"))
            idx = ai % 2
            ai += 1
            return a, idx, P.tok("vector")

        def ep_store(dst, bf, func=None, bias=None, t=t):
            def ep(bi, oc, bank, fw):
                ch = bi * 4 + oc
                ps = T.ps[bank]
                pool = ostb if bf else ostf
                s, st, rel = pool.get()
                if func is None and bias is None:
                    eng = T.ev_eng()
                    if eng == "vector":
                        P.op(eng, lambda e: e.tensor_copy(st[:], ps[:, 0:TT]), waits=[fw] + rel, inc=T.pbank.freed[bank])
                    else:
                        P.op(eng, lambda e: e.copy(st[:], ps[:, 0:TT]), waits=[fw] + rel, inc=T.pbank.freed[bank])
                else:
                    P.op("scalar", lambda e: e.activation(out=st[:], in_=ps[:, 0:TT], func=func or AF.Identity,
                                                          bias=(bias[:, ch:ch + 1] if bias is not None else 0.0)),
                         waits=[fw] + rel, inc=T.pbank.freed[bank])
                tk = (T.pbank.freed[bank], T.pbank.freed[bank].n)
                v = P.op("sync", lambda e: e.dma_start(out=dst[ch * 128:(ch + 1) * 128, t * TT:(t + 1) * TT], in_=st[:]),
                         waits=[tk], inc=pool.sem_out[s], dma=True)
                tko = (pool.sem_out[s], v)
                pool.release(s, [tko])
                out_toks.append(tko)
            return ep

        def ep_h1(func, nch):
            def ep(bi, oc, bank, fw):
                ps = T.ps[bank]
                P.op("scalar", lambda e: e.activation(out=h1[:, oc, :], in_=ps[:, 0:TT], func=func),
                     waits=[fw] + (list(h1_rel) if oc == 0 else []), inc=T.pbank.freed[bank])
            return ep

        full = [[(c0, 512)] for c0 in range(0, D, 512)]
        plan = [(0, "w_r", "rT", None), (1, "w1", None, "w"), (2, "w_k", "kT", None), (3, "w_v", "vT", None),
                (4, "a1", None, "a"), (5, "g1", None, "g")]
        for (j, wname, oname, lora) in plan:
            a, idx, ta = build_act(j)
            if lora is None:
                lf = T.gemm(Wd[wname], D, full, ep_store(outs[oname], True), act=a, act_waits=[ta])
                act_rel[idx] = [lf]
            elif lora == "w":
                lf = T.gemm(Wd["w1"], D, [[(0, 128)]], ep_h1(AF.Tanh, 1), act=a, act_waits=[ta])
                act_rel[idx] = [lf]
                th = P.tok("scalar")
                lf2 = T.gemm(Wd["w2"], 128, full, ep_store(outs["wT"], False, AF.Identity, col(14)), act=h1, act_waits=[th])
                h1_rel[:] = [lf2]
            elif lora == "a":
                lf = T.gemm(Wd["a1"], D, [[(0, 128)]], ep_h1(AF.Copy, 1), act=a, act_waits=[ta])
                act_rel[idx] = [lf]
                th = P.tok("scalar")
                lf2 = T.gemm(Wd["a2"], 128, full, ep_store(outs["aT"], False, AF.Sigmoid, col(15)), act=h1, act_waits=[th])
                h1_rel[:] = [lf2]
            else:
                lf = T.gemm(Wd["g1"], D, [[(0, 512)]], ep_h1(AF.Sigmoid, 4), act=a, act_waits=[ta])
                act_rel[idx] = [lf]
                th = P.tok("scalar")
                lf2 = T.gemm(Wd["g2"], 512, full, ep_store(outs["gT"], True), act=h1, act_waits=[th])
                h1_rel[:] = [lf2]
        U_rel = list(U_readers[-4:])
    for tk in out_toks[-8:]:
        P.wait("sync", *tk)
    for pool in (ostf, ostb):
        for s in range(pool.n):
            P.wait("sync", pool.sem_out[s], pool.sem_out[s].n)
    P.run()
    return nc


A_NCH = 24


def build_phase_a():
    nc = bass.Bass("TRN2", target_bir_lowering=False)
    cT_d = _dram(nc, "cT", [128, KC, 2], F32, "ExternalInput")
    W_d = _dram(nc, "ada_w", [2, D, A_NCH * 128], F32, "ExternalInput")
    b_d = _dram(nc, "ada_b", [128, 2 * A_NCH], F32, "ExternalInput")
    o_d = _dram(nc, "modT", [128, 2 * A_NCH, 2], F32, "ExternalOutput")
    P = Prog(nc)
    P.serial = {"vector", "scalar", "gpsimd"}
    dp = Dep(P)
    cT = P.sbuf("cT_s", [128, KC, 2], F32)
    sg = P.sbuf("sg_s", [128, KC, 2], F32)
    bs = P.sbuf("b_s", [128, 2 * A_NCH], F32)
    osb = P.sbuf("o_s", [128, 2 * A_NCH, 2], F32)
    wb = [P.sbuf(f"aw{i}", [128, KC, 256], F32) for i in range(2)]
    Bc = dp.buf("c", dma=True); Bb = dp.buf("b", dma=True); Bo = dp.buf("o", dma=True)
    Bw = [dp.buf("w0", dma=True), dp.buf("w1", dma=True)]
    ps = [P.psum(f"aps{i}", [128, 512], F32) for i in range(2)]
    Bp = [dp.buf("p0"), dp.buf("p1")]
    dp.dma("sync", lambda e: e.dma_start(out=cT[:], in_=cT_d), Bc, writes=[Bc])
    dp.dma("sync", lambda e: e.dma_start(out=bs[:], in_=b_d), Bb, writes=[Bb])
    dp.op("scalar", lambda e: e.activation(out=sg[:], in_=cT[:], func=AF.Sigmoid), reads=[Bc], writes=[Bc])
    dp.op("vector", lambda e: e.tensor_tensor(out=cT[:], in0=cT[:], in1=sg[:], op=ALU.mult), writes=[Bc])
    nblk = 2 * A_NCH // 2
    for blk in range(nblk):
        l = blk // (A_NCH // 2)
        c0 = (blk % (A_NCH // 2)) * 256
        i = blk % 2
        dp.dma("sync" if i == 0 else "gpsimd", lambda e, l=l, c0=c0, i=i: e.dma_start(
            out=wb[i][:], in_=W_d[l, :, c0:c0 + 256].rearrange("(k p) n -> p k n", p=128)), Bw[i], writes=[Bw[i]])
        for oc in range(2):
            ch = blk * 2 + oc
            dp.pe([lambda e, kk=kk, oc=oc, i=i: e.matmul(ps[oc][:, 0:2], wb[i][:, kk, oc * 128:(oc + 1) * 128], cT[:, kk, :],
                                                         start=(kk == 0), stop=(kk == KC - 1)) for kk in range(KC)],
                  reads=[Bw[i], Bc], writes=[Bp[oc]])
            dp.op("vector", lambda e, ch=ch, oc=oc: e.tensor_tensor(out=osb[:, ch, :], in0=ps[oc][:, 0:2],
                                                                    in1=bs[:, ch:ch + 1].to_broadcast([128, 2]), op=ALU.add),
                  reads=[Bp[oc], Bb], writes=[Bo])
    dp.dma("sync", lambda e: e.dma_start(out=o_d, in_=osb[:]), Bo, reads=[Bo])
    P.wait("sync", Bo.dma_sem, Bo.dma_sem.n)
    P.run()
    return nc


_NC = {}
_DBG = None


def _get(name, fn):
    if name not in _NC:
        _NC[name] = fn()
    return _NC[name]


def _fm(v):
    return np.ascontiguousarray(np.asarray(v, np.float32).reshape(-1, 128).T)


def _ilv(w):
    w = np.asarray(w, np.float32)
    g = w[:, :FF].reshape(D, FF // 256, 256)
    u = w[:, FF:].reshape(D, FF // 256, 256)
    return np.ascontiguousarray(np.concatenate([g, u], axis=2).reshape(D, 2 * FF))


def _run(nc, in_maps):
    res = run_bass_kernel_spmd(nc, in_maps, core_ids=list(range(len(in_maps))))
    return res.results


def kernel(x, c, ada_w, ada_b, mix_ln_g, mix_ln_b, ffn_ln_g, ffn_ln_b, ffn_w_in, ffn_w_out,
           ml_w_in, ml_b_i, ml_b_f, ml_norm_g, ml_w_out,
           rw_mu, rw_w_r, rw_w_k, rw_w_v, rw_w0, rw_w1, rw_w2, rw_a0, rw_a1, rw_a2, rw_g1, rw_g2,
           rw_k_k, rw_k_a, rw_r_k, rw_lnx_g, rw_lnx_b, rw_w_o):
    f32 = np.float32
    x = np.asarray(x, f32)
    xf = x.reshape(8192, D)
    ca = np.ascontiguousarray
    cT = ca(np.asarray(c, f32).T.reshape(KC, 128, 2).transpose(1, 0, 2))
    ims = []
    for j in range(NCORE):
        sl = slice(j * 3072, (j + 1) * 3072)
        bj = np.concatenate([np.asarray(ada_b[l, sl], f32).reshape(A_NCH, 128).T for l in range(2)], axis=1)
        ims.append({"cT": cT, "ada_w": ca(np.asarray(ada_w[:, :, sl], f32)), "ada_b": ca(bj)})
    ra = _run(_get("a", build_phase_a), ims)
    mod = np.zeros((2, 2, 6 * D), f32)
    for j in range(NCORE):
        o = ra[j]["modT"]
        for l in range(2):
            mod[l, :, j * 3072:(j + 1) * 3072] = o[:, l * A_NCH:(l + 1) * A_NCH, :].transpose(2, 1, 0).reshape(2, 3072)
    del ims, ra
    if _DBG is not None:
        _DBG['mod'] = mod.copy()
    mparts = lambda l, b: [mod[l, b, i * D:(i + 1) * D] for i in range(6)]
    xT = [ca(xf[i * NTOK:(i + 1) * NTOK].T) for i in range(NCORE)]
    w_in0 = ca(np.asarray(ml_w_in[0], f32))
    ims = []
    for i in range(NCORE):
        sh_m, sc_m = mparts(0, i // 4)[0:2]
        ims.append({"xT": xT[i], "w_in": w_in0, "modv": ca(np.concatenate([_fm(sh_m), _fm(sc_m)], axis=1))})
    rb = _run(_get("b", build_phase_b), ims)
    qkvT = np.concatenate([rb[i]["qkvT"] for i in range(NCORE)], axis=1)
    soT = np.concatenate([rb[i]["soT"] for i in range(NCORE)], axis=1)
    gT = np.concatenate([rb[i]["gT"] for i in range(NCORE)], axis=1)
    del ims, rb, w_in0
    ims = []
    for j in range(NCORE):
        q = qkvT[j * 256:(j + 1) * 256].reshape(256, 2, ML_S).transpose(1, 0, 2)
        k = qkvT[2048 + j * 256:2048 + (j + 1) * 256].reshape(256, 2, ML_S).transpose(1, 0, 2)
        v = qkvT[4096 + j * 512:4096 + (j + 1) * 512].reshape(512, 2, ML_S).transpose(1, 2, 0)
        so = soT[j * 512:(j + 1) * 512].reshape(512, 2, ML_S).transpose(1, 2, 0)
        ims.append({"qT": ca(q), "kT": ca(k), "ktm": ca(k.transpose(0, 2, 1)), "vtm": ca(v), "sotm": ca(so),
                    "gi": ca(gT[j].reshape(2, ML_S)), "gf": ca(gT[8 + j].reshape(2, ML_S)),
                    "gb": np.array([[ml_b_i[0, j], ml_b_f[0, j]]], f32),
                    "ng": ca(np.tile(np.asarray(ml_norm_g[0, j * 512:(j + 1) * 512], f32)[None, :], (128, 1)))})
    rc = _run(_get("c", build_phase_c), ims)
    hT = np.concatenate([rc[j]["hT"].transpose(1, 0, 2).reshape(512, 8192) for j in range(NCORE)], axis=0)
    if _DBG is not None:
        _DBG['qkvT'] = qkvT; _DBG['soT'] = soT; _DBG['gT'] = gT; _DBG['hT'] = hT
    del ims, rc, qkvT, soT, gT
    w_o0 = ca(np.asarray(ml_w_out[0], f32)); fwi = _ilv(ffn_w_in[0]); fwo = ca(np.asarray(ffn_w_out[0], f32))
    ims = []
    for i in range(NCORE):
        sh_m, sc_m, gt_m, sh_f, sc_f, gt_f = mparts(0, i // 4)
        cols = [gt_m, mix_ln_g[0], mix_ln_b[0], sh_f, sc_f, gt_f, ffn_ln_g[0], ffn_ln_b[0]]
        ims.append({"xT": xT[i], "hT": ca(hT[:, i * NTOK:(i + 1) * NTOK]), "w_o": w_o0, "w_in": fwi, "w_out": fwo,
                    "modv": ca(np.concatenate([_fm(v) for v in cols], axis=1))})
    rd = _run(_get("d1", lambda: build_phase_d1("d1")), ims)
    x2T = [rd[i]["outT"] for i in range(NCORE)]
    if _DBG is not None:
        _DBG['x2T'] = [a.copy() for a in x2T]
    del ims, rd, hT, w_o0, fwi, fwo, xT
    g1p = np.zeros((D, 512), f32); g1p[:, :480] = rw_g1[0]
    g2p = np.zeros((512, D), f32); g2p[:480] = rw_g2[0]
    Wd2 = {"w_r": ca(np.asarray(rw_w_r[0], f32)), "w_k": ca(np.asarray(rw_w_k[0], f32)), "w_v": ca(np.asarray(rw_w_v[0], f32)),
           "w1": ca(np.asarray(rw_w1[0], f32)), "w2": ca(np.asarray(rw_w2[0], f32)), "a1": ca(np.asarray(rw_a1[0], f32)),
           "a2": ca(np.asarray(rw_a2[0], f32)), "g1": g1p, "g2": g2p}
    ims = []
    for i in range(NCORE):
        sh_m, sc_m = mparts(1, i // 4)[0:2]
        first = (i % 4 == 0)
        xprev = np.zeros((128, KC), f32) if first else ca(x2T[i - 1][:, NTOK - 1].reshape(KC, 128).T)
        cols = [_fm(sh_m), _fm(sc_m)] + [_fm(rw_mu[0, jj]) for jj in range(6)] + [np.zeros((128, 6 * KC), f32),
                                                                                  _fm(rw_w0[0]), _fm(rw_a0[0]),
                                                                                  np.full((128, 1), 0.0 if first else 1.0, f32)]
        im = {"xT": x2T[i], "xprev": xprev, "modv": ca(np.concatenate(cols, axis=1))}
        im.update(Wd2)
        ims.append(im)
    r2 = _run(_get("d2", build_phase_d2), ims)
    cat = lambda n: np.concatenate([r2[i][n] for i in range(NCORE)], axis=1)
    rT, kT, vT, wT, aT = cat("rT"), cat("kT"), cat("vT"), cat("wT"), cat("aT")
    gTs = [r2[i]["gT"] for i in range(NCORE)]
    if _DBG is not None:
        _DBG.update(rT=rT, kT=kT, vT=vT, wT=wT, aT=aT, gTs=gTs)
    del ims, r2, Wd2
    ims = []
    for j in range(NCORE):
        sl = slice(j * 512, (j + 1) * 512)
        hv = lambda v: np.asarray(v, f32).reshape(-1)[sl].reshape(8, 64).T
        pv = np.concatenate([hv(rw_k_k[0]), hv(rw_k_a[0]), np.zeros((64, 8), f32), hv(rw_r_k[0]), hv(rw_lnx_g[0]), hv(rw_lnx_b[0])], axis=1)
        ims.append({"rT": ca(rT[sl]), "kT": ca(kT[sl]), "vT": ca(vT[sl]), "wT": ca(wT[sl]), "aT": ca(aT[sl]), "pv": ca(pv)})
    re_ = _run(_get("e", build_phase_e), ims)
    yT = np.concatenate([re_[j]["yT"] for j in range(NCORE)], axis=0)
    if _DBG is not None:
        _DBG['yT'] = yT
    del ims, re_, rT, kT, vT, wT, aT
    w_o1 = ca(np.asarray(rw_w_o[0], f32)); fwi = _ilv(ffn_w_in[1]); fwo = ca(np.asarray(ffn_w_out[1], f32))
    ims = []
    for i in range(NCORE):
        sh_m, sc_m, gt_m, sh_f, sc_f, gt_f = mparts(1, i // 4)
        cols = [gt_m, mix_ln_g[1], mix_ln_b[1], sh_f, sc_f, gt_f, ffn_ln_g[1], ffn_ln_b[1]]
        ims.append({"xT": x2T[i], "yT": ca(yT[:, i * NTOK:(i + 1) * NTOK]), "gT": gTs[i], "w_o": w_o1, "w_in": fwi, "w_out": fwo,
                    "modv": ca(np.concatenate([_fm(v) for v in cols], axis=1))})
    rf = _run(_get("f", lambda: build_phase_d1("f")), ims)
    out = np.concatenate([rf[i]["outT"].T for i in range(NCORE)], axis=0).reshape(2, ML_S, D)
    return np.ascontiguousarray(out.astype(f32))
```
